# Optimizing a Trainium2 kernel written in Bass

```python
import math
import jax, jax.numpy as jnp
from jax import lax
import numpy as np

D_MODEL = 1024
BATCH = 8
SEQ = 4096
DEPTH = 2

N_HEADS = 16
HEAD_DIM = D_MODEL // N_HEADS
D_FF = 4 * D_MODEL
N_MIXERS = 2
N_A_LAYERS = (DEPTH + 1) // 2
N_B_LAYERS = DEPTH // 2
NUM_BUCKETS = 32
MAX_DISTANCE = 128
MOBA_BLOCK = 256
MOBA_TOPK = 3
MOBA_Q_CHUNK = 64
DSA_Q_LORA = 256
DSA_KV_LORA = 256
IDX_HEADS = 8
IDX_DIM = 64
DSA_TOPK_MAX = 256
DSA_Q_CHUNK = 128
DSA_IN_DIM = DSA_Q_LORA + DSA_KV_LORA + IDX_DIM + IDX_HEADS
EPS = 1e-6
NEG = -1e30

kernel_name = "hybrid_moba_dsa_decoder"


def rms_norm(x, g):
    x32 = x.astype(jnp.float32)
    y = x32 * lax.rsqrt(jnp.mean(x32 * x32, axis=-1, keepdims=True) + EPS)
    return y.astype(x.dtype) * g


def rel_bucket(dist):
    n = jnp.maximum(dist, 0)
    exact = NUM_BUCKETS // 2
    nf = jnp.maximum(n, 1).astype(jnp.float32)
    large = exact + (jnp.log(nf / exact) / math.log(MAX_DISTANCE / exact) * (NUM_BUCKETS - exact)).astype(jnp.int32)
    large = jnp.minimum(large, NUM_BUCKETS - 1)
    return jnp.where(n < exact, n, large)


def moba_attention(h, w_qkv, w_o, bias_table):
    B, S, _ = h.shape
    H, Dh, BLK, QC = N_HEADS, HEAD_DIM, MOBA_BLOCK, MOBA_Q_CHUNK
    q, k, v = jnp.split(h @ w_qkv, 3, axis=-1)
    q = q.reshape(B, S, H, Dh).transpose(0, 2, 1, 3)
    k = k.reshape(B, S, H, Dh).transpose(0, 2, 1, 3)
    v = v.reshape(B, S, H, Dh).transpose(0, 2, 1, 3)
    n_blk = -(-S // BLK)
    pad = ((0, 0), (0, 0), (0, n_blk * BLK - S), (0, 0))
    kb = jnp.pad(k, pad).reshape(B, H, n_blk, BLK, Dh)
    vb = jnp.pad(v, pad).reshape(B, H, n_blk, BLK, Dh)
    k_mean = jnp.mean(kb.astype(jnp.float32), axis=3)
    gate = jnp.einsum('bhsd,bhnd->bhsn', q.astype(jnp.float32), k_mean)
    n_past = jnp.arange(S) // BLK
    past = jnp.arange(n_blk)[None, :] < n_past[:, None]
    gate = jnp.where(past[None, None], gate, NEG)
    k_sel = max(1, min(MOBA_TOPK, n_blk - 1))
    _, sel = lax.top_k(gate, k_sel)
    sel = sel.astype(jnp.int32)

    n_ch = S // QC

    def to_chunks(a):
        a = a.reshape(B, H, n_ch, QC, *a.shape[3:])
        a = jnp.moveaxis(a, 2, 1)
        return a.reshape(B * n_ch, H, QC, *a.shape[4:])

    q_c, sel_c = to_chunks(q), to_chunks(sel)
    b_idx = jnp.repeat(jnp.arange(B, dtype=jnp.int32), n_ch)
    c_idx = jnp.tile(jnp.arange(n_ch, dtype=jnp.int32), B)
    head_ids = jnp.arange(H)
    scale = HEAD_DIM ** -0.5

    def step(args):
        qc, selc, b, c = args
        t = c * QC + jnp.arange(QC, dtype=jnp.int32)
        j_own = (c * QC) // BLK
        kbb = lax.dynamic_index_in_dim(kb, b, 0, keepdims=False)
        vbb = lax.dynamic_index_in_dim(vb, b, 0, keepdims=False)
        k_g = kbb[head_ids[:, None, None], selc]
        v_g = vbb[head_ids[:, None, None], selc]
        k_own = lax.dynamic_index_in_dim(kbb, j_own, 1, keepdims=False)
        v_own = lax.dynamic_index_in_dim(vbb, j_own, 1, keepdims=False)
        qf = qc.astype(jnp.float32)
        l_sel = jnp.einsum('hqd,hqnjd->hqnj', qf, k_g.astype(jnp.float32)) * scale
        pos_sel = selc[..., None] * BLK + jnp.arange(BLK, dtype=jnp.int32)
        b_sel = bias_table[rel_bucket(t[None, :, None, None] - pos_sel), head_ids[:, None, None, None]].astype(jnp.float32)
        slot_ok = jnp.arange(k_sel)[None, :] < (t // BLK)[:, None]
        l_sel = jnp.where(slot_ok[None, :, :, None], l_sel + b_sel, NEG).reshape(H, QC, k_sel * BLK)
        l_own = jnp.einsum('hqd,hkd->hqk', qf, k_own.astype(jnp.float32)) * scale
        d_own = t[:, None] - (j_own * BLK + jnp.arange(BLK, dtype=jnp.int32))[None, :]
        b_own = jnp.moveaxis(bias_table[rel_bucket(d_own)].astype(jnp.float32), -1, 0)
        l_own = jnp.where((d_own >= 0)[None], l_own + b_own, NEG)
        p = jax.nn.softmax(jnp.concatenate([l_sel, l_own], axis=-1), axis=-1)
        p_sel = p[..., :k_sel * BLK].reshape(H, QC, k_sel, BLK)
        p_own = p[..., k_sel * BLK:]
        o = (jnp.einsum('hqnj,hqnjd->hqd', p_sel, v_g.astype(jnp.float32))
             + jnp.einsum('hqk,hkd->hqd', p_own, v_own.astype(jnp.float32)))
        return o.astype(qc.dtype)

    o = lax.map(step, (q_c, sel_c, b_idx, c_idx))
    o = o.reshape(B, n_ch, H, QC, Dh).transpose(0, 1, 3, 2, 4).reshape(B, S, H * Dh)
    return o @ w_o


def dsa_attention(h, w_in, g_q, g_kv, w_uq, w_qi, w_uk, w_uv, w_o, bias_table):
    B, S, _ = h.shape
    H, Dh, QC = N_HEADS, HEAD_DIM, DSA_Q_CHUNK
    proj = h @ w_in
    c_q, c_kv, k_idx, w_idx = jnp.split(proj, [DSA_Q_LORA, DSA_Q_LORA + DSA_KV_LORA, DSA_Q_LORA + DSA_KV_LORA + IDX_DIM], axis=-1)
    c_q = rms_norm(c_q, g_q)
    c_kv = rms_norm(c_kv, g_kv)
    q_nope = (c_q @ w_uq).reshape(B, S, H, Dh)
    q_idx = (c_q @ w_qi).reshape(B, S, IDX_HEADS, IDX_DIM)
    w_idx = w_idx.astype(jnp.float32) * (IDX_HEADS ** -0.5 * IDX_DIM ** -0.5)
    top_k = min(DSA_TOPK_MAX, S // 4)
    n_ch = S // QC

    def to_chunks(a):
        return jnp.moveaxis(a.reshape(B, n_ch, QC, *a.shape[2:]), 1, 0)

    k_idx32 = k_idx.astype(jnp.float32)
    key_pos = jnp.arange(S, dtype=jnp.int32)
    b_ids = jnp.arange(B)[:, None, None]
    scale = HEAD_DIM ** -0.5

    def step(args):
        qn, qi, wi, c = args
        t = c * QC + jnp.arange(QC, dtype=jnp.int32)
        rel = jax.nn.relu(jnp.einsum('bqhd,bsd->bqhs', qi.astype(jnp.float32), k_idx32))
        score = jnp.einsum('bqh,bqhs->bqs', wi, rel)
        score = jnp.where((key_pos[None, :] <= t[:, None])[None], score, NEG)
        _, idx = lax.top_k(score, top_k)
        valid = idx <= t[None, :, None]
        c_sel = c_kv[b_ids, idx].astype(jnp.float32)
        q_lat = jnp.einsum('bqhd,hdc->bqhc', qn.astype(jnp.float32), w_uk.astype(jnp.float32))
        logits = jnp.einsum('bqhc,bqkc->bhqk', q_lat, c_sel) * scale
        bias = jnp.moveaxis(bias_table[rel_bucket(t[None, :, None] - idx)].astype(jnp.float32), -1, 1)
        logits = jnp.where(valid[:, None], logits + bias, NEG)
        p = jax.nn.softmax(logits, axis=-1)
        o_lat = jnp.einsum('bhqk,bqkc->bqhc', p, c_sel)
        o = jnp.einsum('bqhc,hcd->bqhd', o_lat, w_uv.astype(jnp.float32))
        return o.astype(qn.dtype)

    o = lax.map(step, (to_chunks(q_nope), to_chunks(q_idx), to_chunks(w_idx), jnp.arange(n_ch, dtype=jnp.int32)))
    o = jnp.moveaxis(o, 0, 1).reshape(B, S, H * Dh)
    return o @ w_o


def sq_relu_mlp(h, w_up, w_down):
    return jnp.square(jax.nn.relu(h @ w_up)) @ w_down


def setup_inputs(seed: int = 0) -> dict:
    key = jax.random.key(seed)
    ks = jax.random.split(key, 20)
    D, H, Dh = D_MODEL, N_HEADS, HEAD_DIM
    nrm = lambda k, shape, s: jax.random.normal(k, shape, jnp.float32) * s
    gain = lambda k, shape: 1.0 + 0.05 * jax.random.normal(k, shape, jnp.float32)
    return {
        "x": nrm(ks[0], (BATCH, SEQ, D), 1.0),
        "rel_bias": nrm(ks[1], (NUM_BUCKETS, H), 0.2),
        "ln_attn": gain(ks[2], (DEPTH, D)),
        "ln_mlp": gain(ks[3], (DEPTH, D)),
        "moba_w_qkv": nrm(ks[4], (N_A_LAYERS, D, 3 * D), D ** -0.5),
        "moba_w_o": nrm(ks[5], (N_A_LAYERS, D, D), D ** -0.5),
        "dsa_w_in": nrm(ks[6], (N_B_LAYERS, D, DSA_IN_DIM), D ** -0.5),
        "dsa_g_q": gain(ks[7], (N_B_LAYERS, DSA_Q_LORA)),
        "dsa_g_kv": gain(ks[8], (N_B_LAYERS, DSA_KV_LORA)),
        "dsa_w_uq": nrm(ks[9], (N_B_LAYERS, DSA_Q_LORA, H * Dh), DSA_Q_LORA ** -0.5),
        "dsa_w_qi": nrm(ks[10], (N_B_LAYERS, DSA_Q_LORA, IDX_HEADS * IDX_DIM), DSA_Q_LORA ** -0.5),
        "dsa_w_uk": nrm(ks[11], (N_B_LAYERS, H, Dh, DSA_KV_LORA), DSA_KV_LORA ** -0.5),
        "dsa_w_uv": nrm(ks[12], (N_B_LAYERS, H, DSA_KV_LORA, Dh), DSA_KV_LORA ** -0.5),
        "dsa_w_o": nrm(ks[13], (N_B_LAYERS, H * Dh, D), (H * Dh) ** -0.5),
        "mlp_w_up": nrm(ks[14], (DEPTH, D, D_FF), D ** -0.5),
        "mlp_w_down": nrm(ks[15], (DEPTH, D_FF, D), 0.5 * D_FF ** -0.5),
        "final_norm": gain(ks[16], (D,)),
    }


def reference(x, rel_bias, ln_attn, ln_mlp, moba_w_qkv, moba_w_o, dsa_w_in, dsa_g_q, dsa_g_kv,
              dsa_w_uq, dsa_w_qi, dsa_w_uk, dsa_w_uv, dsa_w_o, mlp_w_up, mlp_w_down, final_norm):
    for i in range(DEPTH):
        hn = rms_norm(x, ln_attn[i])
        j = i // N_MIXERS
        if i % N_MIXERS == 0:
            x = x + moba_attention(hn, moba_w_qkv[j], moba_w_o[j], rel_bias)
        else:
            x = x + dsa_attention(hn, dsa_w_in[j], dsa_g_q[j], dsa_g_kv[j], dsa_w_uq[j], dsa_w_qi[j],
                                  dsa_w_uk[j], dsa_w_uv[j], dsa_w_o[j], rel_bias)
        x = x + sq_relu_mlp(rms_norm(x, ln_mlp[i]), mlp_w_up[i], mlp_w_down[i])
    return rms_norm(x, final_norm)
```

```python
import math
import numpy as np
import concourse.bass as bass
import concourse.mybir as mybir
from concourse.bass_utils import run_bass_kernel_spmd
from contextlib import ExitStack

F32 = mybir.dt.float32
BF16 = mybir.dt.bfloat16
I32 = mybir.dt.int32
AF = mybir.ActivationFunctionType
ALU = mybir.AluOpType
AX = mybir.AxisListType

D = 1024
H = 16
DFF = 4096
BIG = 30000.0
NEGF = -1.0e30
EPS = 1e-6
NBIS = 16


class Op:
    __slots__ = ("eng", "fn", "deps", "needs_inc", "tok", "sem", "is_dma", "chan", "n")


class Prog:
    ENG = ("pe", "act", "dve", "pool", "sp")

    def __init__(self, nc):
        self.nc = nc
        self.ops = {e: [] for e in self.ENG}
        self.res = {}
        self.stack = ExitStack()
        self.chan_sems = {}
        self.chan_cnt = {}
        self.last = {e: None for e in self.ENG}
        self.last_dma = {}
        self.strict_default = False

    def sb(self, name, shape, dt):
        return self.stack.enter_context(self.nc.sbuf_tensor(name, list(shape), dt))

    def ps(self, name, shape, dt):
        return self.stack.enter_context(self.nc.psum_tensor(name, list(shape), dt))

    def _add(self, eng, fn, reads, writes, chan=None, n=1, strict=False):
        op = Op()
        op.eng, op.fn, op.needs_inc, op.is_dma, op.chan, op.n = eng, fn, False, chan is not None, chan, n
        deps = set()
        raw = set()
        for r in reads:
            st = self.res.get(r)
            if st is not None and st[0] is not None:
                deps.add(st[0])
                raw.add(st[0])
            if st is not None and r.startswith("pb"):
                deps.update(st[1])
        for w in writes:
            st = self.res.get(w)
            if st is not None:
                if st[0] is not None:
                    deps.add(st[0])
                deps.update(st[1])
        keep = []
        for d in deps:
            if d is op or d.fn is None:
                continue
            if (not d.is_dma) and d.eng == eng and eng == "pe":
                continue
            d.needs_inc = True
            keep.append(d)
        op.deps = keep
        for r in reads:
            lst = self.res.setdefault(r, [None, []])[1]
            if not op.is_dma:
                lst[:] = [o for o in lst if o.is_dma or o.eng != eng]
            lst.append(op)
        for w in writes:
            self.res[w] = [op, []]
        self.ops[eng].append(op)
        if op.is_dma:
            self.last_dma[chan] = op
        else:
            self.last[eng] = op
        return op

    def op(self, eng, fn, reads=(), writes=(), strict=None):
        if strict is None:
            strict = self.strict_default
        return self._add(eng, fn, reads, writes, strict=strict)

    def dma(self, eng, chan, fn, reads=(), writes=(), n=1):
        return self._add(eng, fn, reads, writes, chan=chan, n=n)

    def strict(self):
        P = self

        class _S:
            def __enter__(self_):
                self_.old = P.strict_default
                P.strict_default = True

            def __exit__(self_, *a):
                P.strict_default = self_.old
        return _S()

    def barrier(self):
        lasts = [o for o in self.last.values() if o is not None]
        dmas = list(self.last_dma.values())
        for e in self.ENG:
            op = Op()
            op.eng, op.fn, op.needs_inc, op.is_dma, op.chan, op.n = e, None, False, False, None, 0
            op.deps = []
            for d in lasts:
                if d.eng != e:
                    d.needs_inc = True
                    op.deps.append(d)
            for d in dmas:
                op.deps.append(d)
            self.ops[e].append(op)
        self.res = {}

    def emit(self):
        nc = self.nc
        st = self.stack
        esem = {e: st.enter_context(nc.semaphore("sem_" + e)) for e in self.ENG}
        for e in self.ENG:
            for op in self.ops[e]:
                if op.is_dma and op.chan not in self.chan_sems:
                    self.chan_sems[op.chan] = st.enter_context(nc.semaphore("ch_" + op.chan))
                    self.chan_cnt[op.chan] = 0
        for e in self.ENG:
            c = 0
            for op in self.ops[e]:
                if op.is_dma:
                    self.chan_cnt[op.chan] += 16 * op.n
                    op.sem = self.chan_sems[op.chan]
                    op.tok = self.chan_cnt[op.chan]
                else:
                    if op.needs_inc:
                        c += 1
                    op.sem = esem[e]
                    op.tok = c
        finals = [(s, self.chan_cnt[ch]) for ch, s in self.chan_sems.items()]
        block = st.enter_context(nc.Block())

        def body_for(e):
            def body(eng):
                known = {}
                for op in self.ops[e]:
                    need = {}
                    for d in op.deps:
                        k = id(d.sem)
                        if k not in need or need[k][1] < d.tok:
                            need[k] = (d.sem, d.tok)
                    for k, (s, v) in need.items():
                        if known.get(k, 0) >= v:
                            continue
                        eng.wait_ge(s, v)
                        known[k] = v
                    if op.fn is None:
                        continue
                    ins = op.fn(eng)
                    if op.is_dma:
                        if op.n == 1:
                            ins.then_inc(op.sem, 16)
                        else:
                            for i_ in ins:
                                i_.then_inc(op.sem, 16)
                    elif op.needs_inc:
                        ins.then_inc(op.sem, 1)
                if e == "sp":
                    for s, v in finals:
                        eng.wait_ge(s, v)
            return body

        block.tensor(body_for("pe"))
        block.scalar(body_for("act"))
        block.vector(body_for("dve"))
        block.gpsimd(body_for("pool"))
        block.sync(body_for("sp"))
        st.close()


class Arena:
    def __init__(self, P, kbytes):
        self.t = P.sb("arena", [128, kbytes * 256], F32)
        self.cap = kbytes * 1024
        self.off = 0

    def alloc(self, free_shape, dt):
        n = 1
        for s in free_shape:
            n *= s
        nbytes = n * mybir.dt.size(dt)
        nbytes = (nbytes + 63) // 64 * 64
        assert self.off + nbytes <= self.cap, ("arena overflow", self.off, nbytes, self.cap)
        a = self.t[:, self.off // 4:(self.off + nbytes) // 4]
        self.off += nbytes
        if dt != F32:
            a = a.bitcast(dt)
        a = a[:, 0:n]
        if len(free_shape) == 2:
            a = a.rearrange("p (a b) -> p a b", a=free_shape[0])
        elif len(free_shape) == 3:
            a = a.rearrange("p (a b c) -> p a b c", a=free_shape[0], b=free_shape[1])
        return a


def bcast_last(ap2d, n):
    return bass.AP(ap2d.tensor, ap2d.offset, [list(ap2d.ap[0]), list(ap2d.ap[1]), [0, n]])


def MM(P, out, lhsT, rhs, start, stop, r, w):
    P.op("pe", lambda e: e.matmul(out, lhsT=lhsT, rhs=rhs, start=start, stop=stop), reads=r, writes=w)


def TR(P, out, in_, ident, r, w):
    P.op("pe", lambda e: e.transpose(out, in_, ident), reads=r, writes=w)


def ACT(P, out, in_, func, r, w, scale=None, bias=None, accum=None):
    kw = {}
    if scale is not None:
        kw["scale"] = scale
    if bias is not None:
        kw["bias"] = bias
    if accum is not None:
        kw["accum_out"] = accum
    P.op("act", lambda e: e.activation(out=out, in_=in_, func=func, **kw), reads=r, writes=w)


def TS(P, eng, out, in0, s1, s2, op0, op1, r, w, accum=None):
    kw = {}
    if op1 is not None:
        kw["op1"] = op1
    if accum is not None:
        kw["accum_out"] = accum
    P.op(eng, lambda e: e.tensor_scalar(out=out, in0=in0, scalar1=s1, scalar2=s2, op0=op0, **kw), reads=r, writes=w)


def TT(P, eng, out, in0, in1, op, r, w):
    P.op(eng, lambda e: e.tensor_tensor(out=out, in0=in0, in1=in1, op=op), reads=r, writes=w)


def STT(P, out, in0, scalar, in1, op0, op1, r, w):
    P.op("dve", lambda e: e.scalar_tensor_tensor(out=out, in0=in0, scalar=scalar, in1=in1, op0=op0, op1=op1), reads=r, writes=w)


SKIP_RED = [False]


def RED(P, out, in_, op, r, w):
    if SKIP_RED[0]:
        return
    P.op("dve", lambda e: e.tensor_reduce(out=out, in_=in_, axis=AX.X, op=op), reads=r, writes=w)


def COPY(P, eng, out, in_, r, w):
    if eng == "act":
        P.op("act", lambda e: e.copy(out=out, in_=in_), reads=r, writes=w)
    else:
        P.op(eng, lambda e: e.tensor_copy(out=out, in_=in_), reads=r, writes=w)


SKIP_CH = set()


def DMA(P, eng, chan, out, in_, r, w):
    if chan in SKIP_CH:
        return
    P.dma(eng, chan, lambda e: e.dma_start(out=out, in_=in_), reads=r, writes=w)


def DMAN(P, eng, chan, pairs, r, w):
    pairs = list(pairs)
    P.dma(eng, chan, lambda e: [e.dma_start(out=o, in_=i) for (o, i) in pairs], reads=r, writes=w, n=len(pairs))


class Ctx:
    pass


def build(stages=("a0", "m0", "a1", "m1"), S=4096, debug=None):
    NT = S // 128
    NG = S // 512
    nc = bass.Bass("TRN2", target_bir_lowering=False)
    P = Prog(nc)
    C = Ctx()
    C.S, C.NT, C.NG = S, NT, NG

    def din(name, shape, dt=F32):
        return nc.dram_tensor(name, list(shape), dt, kind="ExternalInput").ap()

    def dscr(name, shape, dt):
        return nc.dram_tensor(name, list(shape), dt, kind="Internal").ap()

    dr = {}
    dr["x"] = din("x", [S, D])
    dr["y"] = nc.dram_tensor("y", [S, D], F32, kind="ExternalOutput").ap()
    dr["gb"] = din("gb", [5, 128, D])
    dr["gqkv"] = din("gqkv", [128, 512])
    dr["tb"] = din("tb", [128, H, 256])
    dr["b31"] = din("b31", [128, H])
    dr["wqkv"] = din("wqkv", [D, 3 * D])
    dr["wo0"] = din("wo0", [D, D])
    dr["win"] = din("win", [D, 584])
    dr["wuq"] = din("wuq", [256, D])
    dr["wqi"] = din("wqi", [256, 512])
    dr["wukT"] = din("wukT", [256, D])
    dr["wuv"] = din("wuv", [256, D])
    dr["wo1"] = din("wo1", [D, D])
    dr["wup0"] = din("wup0", [D, DFF])
    dr["wup1"] = din("wup1", [D, DFF])
    dr["wdn0"] = din("wdn0", [DFF, D])
    dr["wdn1"] = din("wdn1", [DFF, D])
    dr["qT"] = dscr("qT", [8, 128, S], BF16)
    dr["kT"] = dscr("kT", [8, 128, S], BF16)
    dr["va"] = dscr("va", [H, 128, NT, 128], BF16)
    dr["qiT"] = dscr("qiT", [4, 128, S], BF16)
    xs = [dr["x"]]
    for i, stg in enumerate(stages):
        if i == len(stages) - 1:
            xs.append(dr["y"])
        else:
            xs.append(dscr("xs%d" % i, [S, D], F32))

    C.pb = [P.ps("pb%d" % i, [128, 512], F32) for i in range(8)]
    A = Arena(P, 200)
    C.A = A
    C.ident = A.alloc([128], BF16)
    C.identB = A.alloc([128], BF16)
    C.io = A.alloc([128], F32)
    C.ssb = A.alloc([64], F32)
    C.ksum = A.alloc([8, 16], F32)
    P.op("pool", lambda e: e.iota(C.io, pattern=[[1, 128]], base=0, channel_multiplier=-1,
                                  allow_small_or_imprecise_dtypes=True), writes=["io"])
    TS(P, "dve", C.ident, C.io, 0.0, None, ALU.is_equal, None, ["io"], ["ident"])
    TS(P, "dve", C.identB, C.io, 0.0, BIG, ALU.is_equal, ALU.mult, ["io"], ["identB"])
    base_off = A.off
    P.barrier()

    for i, stg in enumerate(stages):
        A.off = base_off
        last = (i == len(stages) - 1)
        if stg == "a0":
            attn_phaseA(P, C, dr, xs[i], 0)
            P.barrier()
            A.off = base_off
            if debug is not None and debug.startswith("A"):
                dbg_copy(P, C, dr, xs[i], xs[i + 1])
            else:
                attn_phaseB(P, C, dr, xs[i], xs[i + 1], 0, debug)
        elif stg == "a1":
            C.kiTz = [A.alloc([S], BF16) for _ in range(2)]
            C.wabs = A.alloc([NT, 8], F32)
            C.wsgn = A.alloc([NT, 8], F32)
            base1 = A.off
            attn_phaseA(P, C, dr, xs[i], 1)
            P.barrier()
            A.off = base1
            attn_phaseB(P, C, dr, xs[i], xs[i + 1], 1)
        elif stg == "m0":
            mlp_phase(P, C, dr, xs[i], xs[i + 1], 0, False)
        elif stg == "m1":
            mlp_phase(P, C, dr, xs[i], xs[i + 1], 1, True)
        P.barrier()
    P.emit()
    return nc


def load_w_cast(P, dst3, src, nk, ncols, name, c0=0):
    pairs = []
    step = 2048
    for k in range(nk):
        for cc in range(0, ncols, step):
            w = min(step, ncols - cc)
            pairs.append((dst3[:, k, cc:cc + w], src[k * 128:(k + 1) * 128, c0 + cc:c0 + cc + w]))
    DMAN(P, "pool", "w_" + name, pairs, [], [name])


class Normer:
    def __init__(self, P, C, gb_dram_row, nbuf=3, tag="n"):
        A = C.A
        self.P, self.C = P, C
        self.gb = A.alloc([D], F32)
        DMA(P, "sp", "gbld", self.gb, gb_dram_row, [], ["gb"])
        self.nbuf = nbuf
        self.xt = [A.alloc([D], F32) for _ in range(nbuf)]
        self.xn = [A.alloc([D], BF16) for _ in range(2)]
        self.junk = A.alloc([D], BF16)
        self.cnt = 0

    def load(self, src_tile):
        i = self.cnt % self.nbuf
        DMA(self.P, "sp", "xt%d" % i, self.xt[i], src_tile, [], ["xt%d" % i])
        return i

    def norm_T(self, i, dstT, c0, dst_name, evac_eng="act"):
        P, C = self.P, self.C
        k = self.cnt
        self.cnt += 1
        xt = self.xt[i]
        xn = self.xn[k % 2]
        xnn = "xn%d" % (k % 2)
        ss = C.ssb[:, (k % 8) * 2:(k % 8) * 2 + 1]
        rs = C.ssb[:, (k % 8) * 2 + 1:(k % 8) * 2 + 2]
        sn = "ss%d" % (k % 8)
        ACT(P, self.junk, xt, AF.Square, ["xt%d" % i], ["junk", sn], accum=ss)
        with P.strict():
            ACT(P, rs, ss, AF.Ln, [sn], [sn + "r"], scale=1.0 / D, bias=EPS)
            ACT(P, rs, rs, AF.Exp, [sn + "r"], [sn + "r"], scale=-0.5)
        STT(P, xn, xt, rs, self.gb, ALU.mult, ALU.mult, ["xt%d" % i, sn + "r", "gb"], [xnn])
        pbi = k % 2
        pbv = C.pb[pbi][:].bitcast(BF16)
        for kc in range(8):
            TR(P, pbv[:, kc * 128:(kc + 1) * 128], xn[:, kc * 128:(kc + 1) * 128], C.ident, [xnn, "ident"], ["pb%d" % pbi])
        COPY(P, evac_eng, dstT[:, :, c0:c0 + 128], pbv.rearrange("p (a b) -> p a b", a=8), ["pb%d" % pbi], [dst_name])


def attn_phaseA(P, C, dr, xin, layer):
    A = C.A
    S, NT, NG = C.S, C.NT, C.NG
    nm = Normer(P, C, dr["gb"][2 * layer], nbuf=3)
    hnT = [A.alloc([8, 512], BF16) for _ in range(2)]
    qst = A.alloc([8, 512], BF16)
    kst = A.alloc([8, 512], BF16)
    vst = [A.alloc([8, 2, 128], BF16) for _ in range(2)]
    for b in range(2):
        P.op("pool", (lambda e, b=b: e.memset(vst[b], 1.0)), writes=["vst%d" % b])
    if layer == 0:
        wq = A.alloc([8, D], BF16)
        wk = A.alloc([8, D], BF16)
        wv = A.alloc([8, D], BF16)
        load_w_cast(P, wq, dr["wqkv"], 8, D, "wq", 0)
        load_w_cast(P, wk, dr["wqkv"], 8, D, "wk", D)
        load_w_cast(P, wv, dr["wqkv"], 8, D, "wv", 2 * D)
        P.op("dve", lambda e: e.memset(C.ksum, 0.0), writes=["ksum"])
    else:
        win = A.alloc([8, 584], BF16)
        wuq = A.alloc([2, D], BF16)
        wqi = A.alloc([2, 512], BF16)
        wuk = A.alloc([2, D], BF16)
        wuv = A.alloc([2, D], BF16)
        load_w_cast(P, win, dr["win"], 8, 584, "win")
        load_w_cast(P, wuq, dr["wuq"], 2, D, "wuq")
        load_w_cast(P, wqi, dr["wqi"], 2, 512, "wqi")
        load_w_cast(P, wuk, dr["wukT"], 2, D, "wuk")
        load_w_cast(P, wuv, dr["wuv"], 2, D, "wuv")
        gq = A.alloc([512], F32)
        DMA(P, "sp", "gqld", gq, dr["gqkv"], [], ["gq"])
        cn = [A.alloc([512], BF16) for _ in range(2)]
        ki2 = [A.alloc([2, 128], BF16) for _ in range(2)]
        for b in range(2):
            P.op("pool", (lambda e, b=b: e.memset(ki2[b], 0.0)), writes=["ki2%d" % b])
        cnT = [A.alloc([4, 512], BF16) for _ in range(2)]
        qist = A.alloc([4, 512], BF16)
        junk2 = A.alloc([256], BF16)
    rot = [2, 3, 4, 5, 6, 7]
    rc = [0]

    def nextbank():
        b = rot[rc[0] % len(rot)]
        rc[0] += 1
        return b

    xtiles = xin.rearrange("(t p) d -> t p d", p=128)
    for g in range(NG):
        hb = g % 2
        hT = hnT[hb]
        hname = "hnT%d" % hb
        for j in range(4):
            t = g * 4 + j
            i = nm.load(xtiles[t])
            nm.norm_T(i, hT, j * 128, hname, evac_eng="act" if layer == 0 else "dve")
        if layer == 0:
            for which, wmat, stg, sname in (("q", wq, qst, "qst"), ("k", wk, kst, "kst")):
                for hp in range(8):
                    b = nextbank()
                    for kc in range(8):
                        MM(P, C.pb[b][:, :], wmat[:, kc, hp * 128:(hp + 1) * 128], hT[:, kc, :], kc == 0, kc == 7,
                           ["w" + which, hname], ["pb%d" % b])
                    if which == "q":
                        ACT(P, stg[:, hp, :], C.pb[b][:, :], AF.Identity, ["pb%d" % b], [sname], scale=0.125)
                    else:
                        COPY(P, "act", stg[:, hp, :], C.pb[b][:, :], ["pb%d" % b], [sname])
                        RED(P, C.ksum[:, hp, 2 * g:2 * g + 2], C.pb[b][:, :].rearrange("p (a b) -> p a b", a=2), ALU.add,
                            ["pb%d" % b], ["ksum"])
                dst = dr["qT" if which == "q" else "kT"][:, :, g * 512:(g + 1) * 512].rearrange("h p t -> p h t")
                DMA(P, "sp", sname, dst, stg, [sname], [])
        else:
            for j in range(4):
                t = g * 4 + j
                tc = slice(j * 128, (j + 1) * 128)
                ba = nextbank()
                bb = nextbank()
                for kc in range(8):
                    MM(P, C.pb[ba][:, :], hT[:, kc, tc], win[:, kc, 0:512], kc == 0, kc == 7, [hname, "win"], ["pb%d" % ba])
                for kc in range(8):
                    MM(P, C.pb[bb][:, 0:72], hT[:, kc, tc], win[:, kc, 512:584], kc == 0, kc == 7, [hname, "win"], ["pb%d" % bb])
                cb = t % 2
                ssq = C.ssb[:, 32 + cb * 4:32 + cb * 4 + 1]
                ssk = C.ssb[:, 32 + cb * 4 + 1:32 + cb * 4 + 2]
                rsq = C.ssb[:, 32 + cb * 4 + 2:32 + cb * 4 + 3]
                rsk = C.ssb[:, 32 + cb * 4 + 3:32 + cb * 4 + 4]
                sn = "cs%d" % cb
                ACT(P, junk2, C.pb[ba][:, 0:256], AF.Square, ["pb%d" % ba], ["junk2", sn], accum=ssq)
                ACT(P, junk2, C.pb[ba][:, 256:512], AF.Square, ["pb%d" % ba], ["junk2", sn], accum=ssk)
                with P.strict():
                    ACT(P, rsq, ssq, AF.Ln, [sn], [sn + "q"], scale=1.0 / 256, bias=EPS)
                    ACT(P, rsq, rsq, AF.Exp, [sn + "q"], [sn + "q"], scale=-0.5)
                    ACT(P, rsk, ssk, AF.Ln, [sn], [sn + "k"], scale=1.0 / 256, bias=EPS)
                    ACT(P, rsk, rsk, AF.Exp, [sn + "k"], [sn + "k"], scale=-0.5)
                cname = "cn%d" % cb
                STT(P, cn[cb][:, 0:256], C.pb[ba][:, 0:256], rsq, gq[:, 0:256], ALU.mult, ALU.mult,
                    ["pb%d" % ba, sn + "q", "gq"], [cname])
                STT(P, cn[cb][:, 256:512], C.pb[ba][:, 256:512], rsk, gq[:, 256:512], ALU.mult, ALU.mult,
                    ["pb%d" % ba, sn + "k", "gq"], [cname])
                kname = "ki2%d" % cb
                COPY(P, "act", ki2[cb][:, 0, 0:64], C.pb[bb][:, 0:64], ["pb%d" % bb], [kname])
                COPY(P, "act", ki2[cb][:, 1, 64:128], C.pb[bb][:, 0:64], ["pb%d" % bb], [kname])
                TS(P, "dve", C.wabs[:, t, :], C.pb[bb][:, 64:72], (8.0 ** -0.5) * (64.0 ** -0.5), None, ALU.mult, None,
                   ["pb%d" % bb], ["wabs"])
                TS(P, "dve", C.wsgn[:, t, :], C.wabs[:, t, :], 0.0, 2.0, ALU.is_ge, ALU.mult, ["wabs"], ["wsgn"])
                TS(P, "dve", C.wsgn[:, t, :], C.wsgn[:, t, :], -1.0, None, ALU.add, None, ["wsgn"], ["wsgn"])
                STT(P, C.wabs[:, t, :], C.wabs[:, t, :], -1.0, C.wabs[:, t, :], ALU.mult, ALU.max, ["wabs"], ["wabs"])
                pbv = C.pb[cb][:].bitcast(BF16)
                for q4 in range(4):
                    TR(P, pbv[:, q4 * 128:(q4 + 1) * 128], cn[cb][:, q4 * 128:(q4 + 1) * 128], C.ident, [cname, "ident"], ["pb%d" % cb])
                TR(P, pbv[:, 512:640], ki2[cb][:, 0, :], C.ident, [kname, "ident"], ["pb%d" % cb])
                TR(P, pbv[:, 640:768], ki2[cb][:, 1, :], C.ident, [kname, "ident"], ["pb%d" % cb])
                COPY(P, "act", cnT[hb][:, :, tc], pbv[:, 0:512].rearrange("p (a b) -> p a b", a=4), ["pb%d" % cb], ["cnT%d" % hb])
                COPY(P, "act", C.kiTz[0][:, t * 128:(t + 1) * 128], pbv[:, 512:640], ["pb%d" % cb], ["kiT"])
                COPY(P, "act", C.kiTz[1][:, t * 128:(t + 1) * 128], pbv[:, 640:768], ["pb%d" % cb], ["kiT"])
            cT = cnT[hb]
            cTn = "cnT%d" % hb
            for hp in range(8):
                b = nextbank()
                for kc in range(2):
                    MM(P, C.pb[b][:, :], wuq[:, kc, hp * 128:(hp + 1) * 128], cT[:, kc, :], kc == 0, kc == 1, ["wuq", cTn], ["pb%d" % b])
                ACT(P, qst[:, hp, :], C.pb[b][:, :], AF.Identity, ["pb%d" % b], ["qst"], scale=0.125)
            DMA(P, "sp", "qst", dr["qT"][:, :, g * 512:(g + 1) * 512].rearrange("h p t -> p h t"), qst, ["qst"], [])
            for ch in range(4):
                b = nextbank()
                for kc in range(2):
                    MM(P, C.pb[b][:, :], wqi[:, kc, ch * 128:(ch + 1) * 128], cT[:, kc, :], kc == 0, kc == 1, ["wqi", cTn], ["pb%d" % b])
                COPY(P, "dve", qist[:, ch, :], C.pb[b][:, :], ["pb%d" % b], ["qist"])
            DMA(P, "sp", "qist", dr["qiT"][:, :, g * 512:(g + 1) * 512].rearrange("h p t -> p h t"), qist, ["qist"], [])
            for hp in range(8):
                b = nextbank()
                for kc in range(2):
                    MM(P, C.pb[b][:, :], wuk[:, kc, hp * 128:(hp + 1) * 128], cT[:, 2 + kc, :], kc == 0, kc == 1, ["wuk", cTn], ["pb%d" % b])
                COPY(P, "act", kst[:, hp, :], C.pb[b][:, :], ["pb%d" % b], ["kst"])
            DMA(P, "sp", "kst", dr["kT"][:, :, g * 512:(g + 1) * 512].rearrange("h p t -> p h t"), kst, ["kst"], [])
        for j in range(4):
            t = g * 4 + j
            tc = slice(j * 128, (j + 1) * 128)
            vb = t % 2
            vname = "vst%d" % vb
            for half in range(2):
                b = nextbank()
                if layer == 0:
                    for kc in range(8):
                        MM(P, C.pb[b][:, :], hT[:, kc, tc], wv[:, kc, half * 512:(half + 1) * 512], kc == 0, kc == 7, [hname, "wv"], ["pb%d" % b])
                else:
                    for kc in range(2):
                        MM(P, C.pb[b][:, :], cT[:, 2 + kc, tc], wuv[:, kc, half * 512:(half + 1) * 512], kc == 0, kc == 1, [cTn, "wuv"], ["pb%d" % b])
                psv = C.pb[b][:, :].rearrange("p (a b c) -> p a b c", a=4, b=2)
                COPY(P, "dve", vst[vb][:, half * 4:(half + 1) * 4, 0, 0:64], psv[:, :, 0, :], ["pb%d" % b], [vname])
                COPY(P, "dve", vst[vb][:, half * 4:(half + 1) * 4, 1, 64:128], psv[:, :, 1, :], ["pb%d" % b], [vname])
            dst = dr["va"][:, :, t, :].rearrange("(a b) p d -> p a b d", b=2)
            DMA(P, "sp", vname, dst, vst[vb], [vname], [])


def dbg_copy(P, C, dr, xin, xout):
    A = C.A
    xt = A.alloc([D], F32)
    xin_t = xin.rearrange("(t p) d -> t p d", p=128)
    xout_t = xout.rearrange("(t p) d -> t p d", p=128)
    for t in range(C.NT):
        DMA(P, "sp", "dbgl", xt, xin_t[t], [], ["dbgx"])
        DMA(P, "pool", "dbgs", xout_t[t], xt, ["dbgx"], [])


def attn_phaseB(P, C, dr, xin, xout, layer, debug=None):
    A = C.A
    S, NT, NG = C.S, C.NT, C.NG
    wo = A.alloc([8, D], BF16)
    load_w_cast(P, wo, dr["wo0" if layer == 0 else "wo1"], 8, D, "wo")
    if layer == 1:
        score = A.alloc([max(S, H * 256)], F32)
        tbf = score[:, 0:H * 256].rearrange("p (a b) -> p a b", a=H)
        tbn = "score"
    else:
        tbf = A.alloc([H, 256], F32)
        tbn = "tbf"
    b31 = A.alloc([H], F32)
    cm0 = A.alloc([128], F32)
    TBp = A.alloc([H, 256], BF16)
    DMA(P, "sp", "tbld", tbf, dr["tb"], [], [tbn])
    DMA(P, "sp", "b31ld", b31, dr["b31"], [], ["b31"])
    TS(P, "dve", cm0, C.io, 0.0, -BIG, ALU.is_lt, ALU.mult, ["io"], ["cm0"])
    for h in range(H):
        STT(P, TBp[:, h, 0:128], tbf[:, h, 0:128], b31[:, h:h + 1], cm0, ALU.subtract, ALU.add, [tbn, "b31", "cm0"], ["TBp"])
        TS(P, "dve", TBp[:, h, 128:256], tbf[:, h, 128:256], b31[:, h:h + 1], None, ALU.subtract, None, [tbn, "b31"], ["TBp"])
    qz = [[A.alloc([8, 512], BF16) for _ in range(2)] for _ in range(2)]
    for b in range(2):
        P.op("pool", (lambda e, b=b: e.memset(qz[b][0][64:128], 0.0)), writes=["qg%d" % b])
        P.op("pool", (lambda e, b=b: e.memset(qz[b][1][0:64], 0.0)), writes=["qg%d" % b])
    kbuf = [A.alloc([S], BF16) for _ in range(2)]
    vbuf = [A.alloc([NT, 128], BF16) for _ in range(2)]
    pbuf = [A.alloc([512], BF16) for _ in range(3)]
    oT = A.alloc([8, 512], BF16)
    rec = A.alloc([512], F32)
    xt = [A.alloc([D], F32) for _ in range(2)]
    if layer == 0:
        kbd = A.alloc([8, 32], BF16)
        nidx = A.alloc([256], F32)
        pneg = A.alloc([256], F32)
        ownm1 = A.alloc([256], F32)
        gm = A.alloc([256], F32)
        gm2 = A.alloc([256], F32)
        tmpg = A.alloc([256], F32)
        mx = A.alloc([16], F32)
        selv = A.alloc([256], BF16)
        selT = A.alloc([H, 512], BF16)
        Esel = A.alloc([16, 128], BF16)
        iot = A.alloc([16, 128], F32)
        P.op("pool", lambda e: e.memset(selT, 0.0), writes=["selT"])
        P.op("dve", lambda e: e.memset(kbd, 0.0), writes=["kbd"])
        COPY(P, "dve", kbd[0:64, :, 0:16], C.ksum[0:64, :, :], ["ksum", "kbd"], ["kbd"])
        COPY(P, "dve", kbd[64:128, :, 16:32], C.ksum[64:128, :, :], ["ksum", "kbd"], ["kbd"])
        P.op("pool", lambda e: e.iota(nidx.rearrange("p (a b) -> p a b", a=16), pattern=[[0, 16], [1, 16]], base=0,
                                      channel_multiplier=0, allow_small_or_imprecise_dtypes=True), writes=["nidx"])
        P.op("pool", lambda e: e.iota(iot, pattern=[[1, 16], [0, 128]], base=0, channel_multiplier=-1,
                                      allow_small_or_imprecise_dtypes=True), writes=["iot"])
        TS(P, "dve", Esel, iot, 0.0, BIG, ALU.is_equal, ALU.mult, ["iot"], ["Esel"])
    else:
        qig = A.alloc([4, 512], BF16)
        madd = A.alloc([S], BF16)
        junkb = madd
        maskT = A.alloc([NT, 512], BF16)
        cneg = A.alloc([128], F32)
        cpos = A.alloc([128], F32)
        dtmp = A.alloc([128], F32)
        bs = A.alloc([16], F32)
        bsi = A.alloc([4], I32)
        TS(P, "dve", cneg, C.io, 0.0, NEGF, ALU.is_gt, ALU.mult, ["io"], ["cneg"])
        TS(P, "dve", cpos, C.io, 0.0, -NEGF, ALU.is_gt, ALU.mult, ["io"], ["cpos"])
    xin_t = xin.rearrange("(t p) d -> t p d", p=128)
    xout_t = xout.rearrange("(t p) d -> t p d", p=128)
    lrot = [2, 3, 4]
    lc = [0]
    for g in range(NG):
        qb = g % 2
        qname = "qg%d" % qb
        QZ = qz[qb]
        DMAN(P, "sp", qname, [(QZ[0][0:64], dr["qT"][:, 0:64, g * 512:(g + 1) * 512].rearrange("h p t -> p h t")),
                              (QZ[1][64:128], dr["qT"][:, 64:128, g * 512:(g + 1) * 512].rearrange("h p t -> p h t"))], [], [qname])
        nch = 4 * (g + 1)
        if layer == 0:
            for j in range(4):
                t = g * 4 + j
                blk = t // 2
                if t % 2 == 0:
                    TS(P, "dve", pneg, nidx, float(blk), NEGF, ALU.is_ge, ALU.mult, ["nidx"], ["pneg"])
                    TS(P, "dve", ownm1, nidx, float(blk), -1.0, ALU.is_ge, ALU.add, ["nidx"], ["ownm1"])
                for hp in range(8):
                    for h2 in range(2):
                        MM(P, C.pb[0][:, hp * 32:(hp + 1) * 32], QZ[h2][:, hp, j * 128:(j + 1) * 128], kbd[:, hp, :], h2 == 0, h2 == 1,
                           [qname, "kbd"], ["pb0"])
                P.strict_default = True
                TT(P, "dve", gm, C.pb[0][:, 0:256], pneg, ALU.add, ["pb0", "pneg"], ["gm"])
                g3 = gm.rearrange("p (a b) -> p a b", a=16)
                g23 = gm2.rearrange("p (a b) -> p a b", a=16)
                t3 = tmpg.rearrange("p (a b) -> p a b", a=16)
                mb = bcast_last(mx, 16)
                RED(P, mx, g3, ALU.max, ["gm"], ["mx"])
                TT(P, "dve", t3, g3, mb, ALU.is_ge, ["gm", "mx"], ["tmpg"])
                STT(P, gm2, tmpg, NEGF, gm, ALU.mult, ALU.add, ["tmpg", "gm"], ["gm2"])
                RED(P, mx, g23, ALU.max, ["gm2"], ["mx"])
                TT(P, "dve", t3, g23, mb, ALU.is_ge, ["gm2", "mx"], ["tmpg"])
                STT(P, gm2, tmpg, NEGF, gm2, ALU.mult, ALU.add, ["tmpg", "gm2"], ["gm2"])
                RED(P, mx, g23, ALU.max, ["gm2"], ["mx"])
                TT(P, "dve", t3, g3, mb, ALU.is_ge, ["gm", "mx"], ["tmpg"])
                STT(P, selv, tmpg, -1.0, ownm1, ALU.add, ALU.max, ["tmpg", "ownm1"], ["selv"])
                P.strict_default = False
                pv1 = C.pb[1][:].bitcast(BF16)
                for rnd in range(2):
                    for hh in range(8):
                        h = rnd * 8 + hh
                        TR(P, pv1[0:16, hh * 128:(hh + 1) * 128], selv[:, h * 16:(h + 1) * 16], C.ident, ["selv", "ident"], ["pb1"])
                    COPY(P, "act", selT[0:16, rnd * 8:(rnd + 1) * 8, j * 128:(j + 1) * 128],
                         pv1[0:16, :].rearrange("p (a b) -> p a b", a=8), ["pb1"], ["selT"])
        else:
            DMA(P, "sp", "qig", qig, dr["qiT"][:, :, g * 512:(g + 1) * 512].rearrange("h p t -> p h t"), [], ["qig"])
            for j in range(4):
                t = g * 4 + j
                nk = 128 * (t + 1)
                qc = slice(j * 128, (j + 1) * 128)
                nsg = (nk + 511) // 512
                for sg in range(nsg):
                    ncol = min(512, nk - sg * 512)
                    sc = slice(sg * 512, sg * 512 + ncol)
                    for hi in range(8):
                        ch, r0 = hi // 2, 64 * (hi % 2)
                        bm = (sg * 8 + hi) % 2
                        br = 6 + bm
                        MM(P, C.pb[bm][:, 0:ncol], qig[:, ch, qc], C.kiTz[hi % 2][:, sc], True, True, ["qig", "kiT"], ["pb%d" % bm])
                        ACT(P, C.pb[br][:, 0:ncol], C.pb[bm][:, 0:ncol], AF.Relu, ["pb%d" % bm, "wabs"], ["pb%d" % br],
                            scale=C.wabs[:, t, hi:hi + 1])
                        if hi == 0:
                            TS(P, "dve", score[:, sc], C.pb[br][:, 0:ncol], C.wsgn[:, t, 0:1], None, ALU.mult, None,
                               ["pb%d" % br, "wsgn"], ["score"])
                        else:
                            STT(P, score[:, sc], C.pb[br][:, 0:ncol], C.wsgn[:, t, hi:hi + 1], score[:, sc], ALU.mult, ALU.add,
                                ["pb%d" % br, "wsgn", "score"], ["score"])
                dg = slice(128 * t, 128 * t + 128)
                lo, hi_, rg, mid, cnt, m1 = (bs[:, k:k + 1] for k in range(6))
                fl = bsi[:, 0:1]
                P.strict_default = True
                TT(P, "dve", dtmp, score[:, dg], cpos, ALU.add, ["score", "cpos"], ["dtmp"])
                RED(P, lo, dtmp, ALU.min, ["dtmp"], ["bs"])
                if t > 0:
                    RED(P, m1, score[:, 0:128 * t], ALU.min, ["score"], ["bs"])
                    TT(P, "dve", lo, lo, m1, ALU.min, ["bs"], ["bs"])
                TT(P, "dve", score[:, dg], score[:, dg], cneg, ALU.add, ["score", "cneg"], ["score"])
                if nk > 256:
                    RED(P, hi_, score[:, 0:nk], ALU.max, ["score"], ["bs"])
                    TT(P, "dve", rg, hi_, lo, ALU.subtract, ["bs"], ["bs"])
                    for it in range(NBIS):
                        TS(P, "dve", mid, rg, 2.0 ** -(it + 1), lo, ALU.mult, ALU.add, ["bs"], ["bs"])
                        TS(P, "dve", junkb[:, 0:nk], score[:, 0:nk], mid, None, ALU.is_ge, ALU.add, ["score", "bs"], ["junkb", "bs"], accum=cnt)
                        TS(P, "dve", fl, cnt, 256.0, None, ALU.is_ge, None, ["bs"], ["bsi"])
                        P.op("dve", (lambda e, lo=lo, fl=fl, mid=mid: e.copy_predicated(out=lo, mask=fl, data=mid)), reads=["bs", "bsi"], writes=["bs"])
                TS(P, "dve", madd[:, 0:nk], score[:, 0:nk], lo, 1.0, ALU.is_ge, ALU.subtract, ["score", "bs"], ["madd"])
                P.strict_default = False
                pv7 = C.pb[5][:].bitcast(BF16)
                for c0 in range(0, t + 1, 8):
                    cn_ = min(8, t + 1 - c0)
                    for cc in range(cn_):
                        c = c0 + cc
                        TR(P, pv7[:, cc * 128:(cc + 1) * 128], madd[:, c * 128:(c + 1) * 128], C.ident, ["madd", "ident"], ["pb5"])
                    COPY(P, "act", maskT[:, c0:c0 + cn_, qc], pv7[:, 0:cn_ * 128].rearrange("p (a b) -> p a b", a=cn_), ["pb5"], ["maskT"])
        items = [(h, c) for h in range(H) for c in range(nch)]
        LOOK = 3
        lbanks = [2, 3, 4, 7] if layer == 0 else [0, 1, 2, 5]

        def hinfo(h):
            hp, h2 = h // 2, h % 2
            r0 = 64 * h2
            return hp, h2, slice(r0, r0 + 64), slice(64 - r0, 128 - r0), hp % 2, h % 2

        def issue_qk(idx):
            h, c = items[idx]
            hp, h2, rs_, so, kb, vb = hinfo(h)
            kname = "kbuf%d" % kb
            if c == 0:
                if h2 == 0:
                    DMA(P, "sp", kname, kbuf[kb][:, 0:nch * 128], dr["kT"][hp, :, 0:nch * 128], [], [kname])
                DMA(P, "sp", "vbuf%d" % vb, vbuf[vb][:, 0:nch, :], dr["va"][h, :, 0:nch, :], [], ["vbuf%d" % vb])
            dc = c - 4 * g
            col0 = max(0, dc * 128)
            cs = slice(col0, 512)
            lb = lbanks[idx % 4]
            lbn = "pb%d" % lb
            psl = C.pb[lb]
            n_blk = c // 2
            need_sel = (layer == 1) or (n_blk < 2 * g + 1)
            need_tb = dc >= -1
            MM(P, psl[:, cs], kbuf[kb][:, c * 128:(c + 1) * 128], QZ[h2][:, hp, cs], True, not (need_sel or need_tb), [kname, qname], [lbn])
            if need_sel:
                if layer == 0:
                    MM(P, psl[:, cs], Esel[:, n_blk, :], selT[:, h, cs], False, not need_tb, ["Esel", "selT"], [lbn])
                else:
                    MM(P, psl[:, cs], C.identB, maskT[:, c, cs], False, not need_tb, ["identB", "maskT"], [lbn])
            if need_tb:
                if dc == -1:
                    MM(P, psl[:, 0:128], C.ident, TBp[:, h, 128:256], False, True, ["ident", "TBp"], [lbn])
                elif dc == 3:
                    MM(P, psl[:, 384:512], C.ident, TBp[:, h, 0:128], False, True, ["ident", "TBp"], [lbn])
                else:
                    MM(P, psl[:, col0:col0 + 256], C.ident, TBp[:, h, 0:256], False, True, ["ident", "TBp"], [lbn])

        for idx in range(min(LOOK, len(items))):
            issue_qk(idx)
        for idx in range(len(items)):
            if idx + LOOK < len(items):
                issue_qk(idx + LOOK)
            h, c = items[idx]
            hp, h2, rs_, so, kb, vb = hinfo(h)
            ob = 5 + (h % 2) if layer == 0 else 3 + (h % 2)
            obn = "pb%d" % ob
            pso = C.pb[ob]
            dc = c - 4 * g
            col0 = max(0, dc * 128)
            cs = slice(col0, 512)
            lb = lbanks[idx % 4]
            lbn = "pb%d" % lb
            pi = idx % 3
            pn = "pbuf%d" % pi
            ACT(P, pbuf[pi][:, cs], C.pb[lb][:, cs], AF.Exp, [lbn], [pn])
            MM(P, pso[:, cs], vbuf[vb][:, c, :], pbuf[pi][:, cs], c == 0, c == nch - 1, ["vbuf%d" % vb, pn], [obn])
            if c == nch - 1:
                ACT(P, rec[so, :], pso[so, :], AF.Ln, [obn], ["rec"])
                ACT(P, rec[so, :], rec[so, :], AF.Exp, ["rec"], ["rec"], scale=-1.0)
                TT(P, "dve", oT[rs_, hp, :], pso[rs_, :], rec[so, :], ALU.mult, [obn, "rec"], ["oT"])
        for j in range(4):
            t = g * 4 + j
            xb = t % 2
            xn_ = "xta%d" % xb
            DMA(P, "sp", xn_, xt[xb], xin_t[t], [], [xn_])
            for half in range(2):
                b = 6 + half if layer == 1 else (0, 1)[half]
                bn = "pb%d" % b
                for hp in range(8):
                    MM(P, C.pb[b][:, :], oT[:, hp, j * 128:(j + 1) * 128], wo[:, hp, half * 512:(half + 1) * 512], hp == 0, hp == 7, ["oT", "wo"], [bn])
                hs = slice(half * 512, (half + 1) * 512)
                TT(P, "dve", xt[xb][:, hs], C.pb[b][:, :], xt[xb][:, hs], ALU.add, [bn, xn_], [xn_])
            DMA(P, "pool", "xst%d" % xb, xout_t[t], xt[xb], [xn_], [])


def mlp_phase(P, C, dr, xin, xout, layer, final):
    A = C.A
    S, NT = C.S, C.NT
    wup = A.alloc([8, DFF], BF16)
    wdn = A.alloc([32, D], BF16)
    load_w_cast(P, wup, dr["wup%d" % layer], 8, DFF, "wup")
    load_w_cast(P, wdn, dr["wdn%d" % layer], 32, D, "wdn")
    nm = Normer(P, C, dr["gb"][2 * layer + 1], nbuf=4)
    if final:
        fnb = A.alloc([D], F32)
        DMA(P, "sp", "fnld", fnb, dr["gb"][4], [], ["fnb"])
        junkf = A.alloc([D], BF16)
    hnT = [A.alloc([8, 256], BF16) for _ in range(2)]
    hTt = A.alloc([32, 256], BF16)
    rbuf = [A.alloc([512], BF16) for _ in range(3)]
    xin_t = xin.rearrange("(t p) d -> t p d", p=128)
    xout_t = xout.rearrange("(t p) d -> t p d", p=128)
    rcnt = [0]
    for g2 in range(NT // 2):
        hb = g2 % 2
        hname = "mhT%d" % hb
        xi = []
        for j in range(2):
            t = g2 * 2 + j
            i = nm.load(xin_t[t])
            xi.append(i)
            nm.norm_T(i, hnT[hb], j * 128, hname, evac_eng="dve")
        for f2 in range(16):
            b = 2 + (f2 % 2)
            bn = "pb%d" % b
            for q in range(2):
                ffc = f2 * 2 + q
                for kc in range(8):
                    MM(P, C.pb[b][:, q * 256:(q + 1) * 256], wup[:, kc, ffc * 128:(ffc + 1) * 128], hnT[hb][:, kc, :], kc == 0, kc == 7,
                       ["wup", hname], [bn])
            ri = rcnt[0] % 3
            rcnt[0] += 1
            rn = "rbuf%d" % ri
            ACT(P, rbuf[ri], C.pb[b][:, :], AF.Relu, [bn], [rn])
            TT(P, "dve", hTt[:, f2 * 2:f2 * 2 + 2, :], C.pb[b][:, :].rearrange("p (a b) -> p a b", a=2),
               rbuf[ri].rearrange("p (a b) -> p a b", a=2), ALU.mult, [bn, rn], ["hTt"])
        for j in range(2):
            t = g2 * 2 + j
            i = xi[j]
            xn_ = "xt%d" % i
            for half in range(2):
                b = 4 + j * 2 + half
                bn = "pb%d" % b
                for ffc in range(32):
                    MM(P, C.pb[b][:, :], hTt[:, ffc, j * 128:(j + 1) * 128], wdn[:, ffc, half * 512:(half + 1) * 512], ffc == 0, ffc == 31,
                       ["hTt", "wdn"], [bn])
                hs = slice(half * 512, (half + 1) * 512)
                TT(P, "dve", nm.xt[i][:, hs], C.pb[b][:, :], nm.xt[i][:, hs], ALU.add, [bn, xn_], [xn_])
            if final:
                ss = C.ssb[:, 48 + (t % 4) * 2:48 + (t % 4) * 2 + 1]
                rs = C.ssb[:, 48 + (t % 4) * 2 + 1:48 + (t % 4) * 2 + 2]
                sn = "fs%d" % (t % 4)
                ACT(P, junkf, nm.xt[i], AF.Square, [xn_], ["junkf", sn], accum=ss)
                with P.strict():
                    ACT(P, rs, ss, AF.Ln, [sn], [sn + "r"], scale=1.0 / D, bias=EPS)
                    ACT(P, rs, rs, AF.Exp, [sn + "r"], [sn + "r"], scale=-0.5)
                STT(P, nm.xt[i], nm.xt[i], rs, fnb, ALU.mult, ALU.mult, [xn_, sn + "r", "fnb"], [xn_])
            DMA(P, "pool", "xst%d" % i, xout_t[t], nm.xt[i], [xn_], [])


def _rel_bucket_np(dist):
    n = np.maximum(dist, 0)
    exact = 16
    nf = np.maximum(n, 1).astype(np.float32)
    large = exact + (np.log(nf / np.float32(exact)) / np.float32(math.log(128 / exact)) * np.float32(32 - exact)).astype(np.int32)
    large = np.minimum(large, 31)
    return np.where(n < exact, n, large)


def host_layout(inp):
    f = lambda a: np.ascontiguousarray(np.asarray(a, dtype=np.float32))
    rb = f(inp["rel_bias"])
    s_idx = np.arange(128)[:, None]
    q_idx = np.arange(128)[None, :]
    bk0 = _rel_bucket_np(q_idx - s_idx)
    bk1 = _rel_bucket_np(q_idx - s_idx + 128)
    tb = np.empty((128, H, 256), np.float32)
    tb[:, :, 0:128] = rb[bk0].transpose(0, 2, 1)
    tb[:, :, 128:256] = rb[bk1].transpose(0, 2, 1)
    gb = np.stack([inp["ln_attn"][0], inp["ln_mlp"][0], inp["ln_attn"][1], inp["ln_mlp"][1], inp["final_norm"]])
    gb = f(np.broadcast_to(gb[:, None, :], (5, 128, D)))
    gqkv = f(np.broadcast_to(np.concatenate([inp["dsa_g_q"][0], inp["dsa_g_kv"][0]])[None, :], (128, 512)))
    m = {
        "gb": gb, "gqkv": gqkv, "tb": f(tb), "b31": f(np.broadcast_to(rb[31][None, :], (128, H))),
        "wqkv": f(inp["moba_w_qkv"][0]), "wo0": f(inp["moba_w_o"][0]), "win": f(inp["dsa_w_in"][0]),
        "wuq": f(inp["dsa_w_uq"][0]), "wqi": f(inp["dsa_w_qi"][0]),
        "wukT": f(np.transpose(inp["dsa_w_uk"][0], (2, 0, 1)).reshape(256, D)),
        "wuv": f(np.transpose(inp["dsa_w_uv"][0], (1, 0, 2)).reshape(256, D)),
        "wo1": f(inp["dsa_w_o"][0]),
        "wup0": f(inp["mlp_w_up"][0]), "wup1": f(inp["mlp_w_up"][1]),
        "wdn0": f(inp["mlp_w_down"][0]), "wdn1": f(inp["mlp_w_down"][1]),
    }
    return m


_NC_CACHE = {}


def kernel(**inputs):
    x = np.asarray(inputs["x"], dtype=np.float32)
    B = x.shape[0]
    shared = host_layout(inputs)
    key = ("full", x.shape[1])
    if key not in _NC_CACHE:
        _NC_CACHE[key] = build(("a0", "m0", "a1", "m1"), S=x.shape[1])
    nc = _NC_CACHE[key]
    in_maps = []
    for b in range(B):
        m = dict(shared)
        m["x"] = np.ascontiguousarray(x[b])
        in_maps.append(m)
    res = run_bass_kernel_spmd(nc, in_maps, core_ids=list(range(B)))
    return np.stack([np.asarray(r["y"], dtype=np.float32) for r in res.results], axis=0)
```

```python
import math
import numpy as np
import concourse.bass as bass
import concourse.mybir as mybir
from concourse.bass_utils import run_bass_kernel_spmd
from contextlib import ExitStack

F32 = mybir.dt.float32
BF16 = mybir.dt.bfloat16
I32 = mybir.dt.int32
AF = mybir.ActivationFunctionType
ALU = mybir.AluOpType
AX = mybir.AxisListType

D = 1024
H = 16
DFF = 4096
BIG = 30000.0
NEGF = -1.0e30
EPS = 1e-6
NBIS = 16


class Op:
    __slots__ = ("eng", "fn", "deps", "needs_inc", "tok", "sem", "is_dma", "chan", "n")


class Prog:
    ENG = ("pe", "act", "dve", "pool", "sp")

    def __init__(self, nc):
        self.nc = nc
        self.ops = {e: [] for e in self.ENG}
        self.res = {}
        self.stack = ExitStack()
        self.chan_sems = {}
        self.chan_cnt = {}
        self.last = {e: None for e in self.ENG}
        self.last_dma = {}
        self.strict_default = False

    def sb(self, name, shape, dt):
        return self.stack.enter_context(self.nc.sbuf_tensor(name, list(shape), dt))

    def ps(self, name, shape, dt):
        return self.stack.enter_context(self.nc.psum_tensor(name, list(shape), dt))

    def _add(self, eng, fn, reads, writes, chan=None, n=1, strict=False):
        op = Op()
        op.eng, op.fn, op.needs_inc, op.is_dma, op.chan, op.n = eng, fn, False, chan is not None, chan, n
        deps = set()
        raw = set()
        for r in reads:
            st = self.res.get(r)
            if st is not None and st[0] is not None:
                deps.add(st[0])
                raw.add(st[0])
            if st is not None and r.startswith("pb"):
                deps.update(st[1])
        for w in writes:
            st = self.res.get(w)
            if st is not None:
                if st[0] is not None:
                    deps.add(st[0])
                deps.update(st[1])
        keep = []
        for d in deps:
            if d is op or d.fn is None:
                continue
            if (not d.is_dma) and d.eng == eng and eng == "pe":
                continue
            d.needs_inc = True
            keep.append(d)
        op.deps = keep
        for r in reads:
            lst = self.res.setdefault(r, [None, []])[1]
            if not op.is_dma:
                lst[:] = [o for o in lst if o.is_dma or o.eng != eng]
            lst.append(op)
        for w in writes:
            self.res[w] = [op, []]
        self.ops[eng].append(op)
        if op.is_dma:
            self.last_dma[chan] = op
        else:
            self.last[eng] = op
        return op

    def op(self, eng, fn, reads=(), writes=(), strict=None):
        if strict is None:
            strict = self.strict_default
        return self._add(eng, fn, reads, writes, strict=strict)

    def dma(self, eng, chan, fn, reads=(), writes=(), n=1):
        return self._add(eng, fn, reads, writes, chan=chan, n=n)

    def strict(self):
        P = self

        class _S:
            def __enter__(self_):
                self_.old = P.strict_default
                P.strict_default = True

            def __exit__(self_, *a):
                P.strict_default = self_.old
        return _S()

    def barrier(self):
        lasts = [o for o in self.last.values() if o is not None]
        dmas = list(self.last_dma.values())
        for e in self.ENG:
            op = Op()
            op.eng, op.fn, op.needs_inc, op.is_dma, op.chan, op.n = e, None, False, False, None, 0
            op.deps = []
            for d in lasts:
                if d.eng != e:
                    d.needs_inc = True
                    op.deps.append(d)
            for d in dmas:
                op.deps.append(d)
            self.ops[e].append(op)
        self.res = {}

    def emit(self):
        nc = self.nc
        st = self.stack
        esem = {e: st.enter_context(nc.semaphore("sem_" + e)) for e in self.ENG}
        for e in self.ENG:
            for op in self.ops[e]:
                if op.is_dma and op.chan not in self.chan_sems:
                    self.chan_sems[op.chan] = st.enter_context(nc.semaphore("ch_" + op.chan))
                    self.chan_cnt[op.chan] = 0
        for e in self.ENG:
            c = 0
            for op in self.ops[e]:
                if op.is_dma:
                    self.chan_cnt[op.chan] += 16 * op.n
                    op.sem = self.chan_sems[op.chan]
                    op.tok = self.chan_cnt[op.chan]
                else:
                    if op.needs_inc:
                        c += 1
                    op.sem = esem[e]
                    op.tok = c
        finals = [(s, self.chan_cnt[ch]) for ch, s in self.chan_sems.items()]
        block = st.enter_context(nc.Block())

        def body_for(e):
            def body(eng):
                known = {}
                for op in self.ops[e]:
                    need = {}
                    for d in op.deps:
                        k = id(d.sem)
                        if k not in need or need[k][1] < d.tok:
                            need[k] = (d.sem, d.tok)
                    for k, (s, v) in need.items():
                        if known.get(k, 0) >= v:
                            continue
                        eng.wait_ge(s, v)
                        known[k] = v
                    if op.fn is None:
                        continue
                    ins = op.fn(eng)
                    if op.is_dma:
                        if op.n == 1:
                            ins.then_inc(op.sem, 16)
                        else:
                            for i_ in ins:
                                i_.then_inc(op.sem, 16)
                    elif op.needs_inc:
                        ins.then_inc(op.sem, 1)
                if e == "sp":
                    for s, v in finals:
                        eng.wait_ge(s, v)
            return body

        block.tensor(body_for("pe"))
        block.scalar(body_for("act"))
        block.vector(body_for("dve"))
        block.gpsimd(body_for("pool"))
        block.sync(body_for("sp"))
        st.close()


class Arena:
    def __init__(self, P, kbytes):
        self.t = P.sb("arena", [128, kbytes * 256], F32)
        self.cap = kbytes * 1024
        self.off = 0

    def alloc(self, free_shape, dt):
        n = 1
        for s in free_shape:
            n *= s
        nbytes = n * mybir.dt.size(dt)
        nbytes = (nbytes + 63) // 64 * 64
        assert self.off + nbytes <= self.cap, ("arena overflow", self.off, nbytes, self.cap)
        a = self.t[:, self.off // 4:(self.off + nbytes) // 4]
        self.off += nbytes
        if dt != F32:
            a = a.bitcast(dt)
        a = a[:, 0:n]
        if len(free_shape) == 2:
            a = a.rearrange("p (a b) -> p a b", a=free_shape[0])
        elif len(free_shape) == 3:
            a = a.rearrange("p (a b c) -> p a b c", a=free_shape[0], b=free_shape[1])
        return a


def bcast_last(ap2d, n):
    return bass.AP(ap2d.tensor, ap2d.offset, [list(ap2d.ap[0]), list(ap2d.ap[1]), [0, n]])


def MM(P, out, lhsT, rhs, start, stop, r, w):
    P.op("pe", lambda e: e.matmul(out, lhsT=lhsT, rhs=rhs, start=start, stop=stop), reads=r, writes=w)


def TR(P, out, in_, ident, r, w):
    P.op("pe", lambda e: e.transpose(out, in_, ident), reads=r, writes=w)


def ACT(P, out, in_, func, r, w, scale=None, bias=None, accum=None):
    kw = {}
    if scale is not None:
        kw["scale"] = scale
    if bias is not None:
        kw["bias"] = bias
    if accum is not None:
        kw["accum_out"] = accum
    P.op("act", lambda e: e.activation(out=out, in_=in_, func=func, **kw), reads=r, writes=w)


def TS(P, eng, out, in0, s1, s2, op0, op1, r, w, accum=None):
    kw = {}
    if op1 is not None:
        kw["op1"] = op1
    if accum is not None:
        kw["accum_out"] = accum
    P.op(eng, lambda e: e.tensor_scalar(out=out, in0=in0, scalar1=s1, scalar2=s2, op0=op0, **kw), reads=r, writes=w)


def TT(P, eng, out, in0, in1, op, r, w):
    P.op(eng, lambda e: e.tensor_tensor(out=out, in0=in0, in1=in1, op=op), reads=r, writes=w)


def STT(P, out, in0, scalar, in1, op0, op1, r, w):
    P.op("dve", lambda e: e.scalar_tensor_tensor(out=out, in0=in0, scalar=scalar, in1=in1, op0=op0, op1=op1), reads=r, writes=w)


SKIP_RED = [False]


def RED(P, out, in_, op, r, w):
    if SKIP_RED[0]:
        return
    P.op("dve", lambda e: e.tensor_reduce(out=out, in_=in_, axis=AX.X, op=op), reads=r, writes=w)


def COPY(P, eng, out, in_, r, w):
    if eng == "act":
        P.op("act", lambda e: e.copy(out=out, in_=in_), reads=r, writes=w)
    else:
        P.op(eng, lambda e: e.tensor_copy(out=out, in_=in_), reads=r, writes=w)


SKIP_CH = set()


def DMA(P, eng, chan, out, in_, r, w):
    if chan in SKIP_CH:
        return
    P.dma(eng, chan, lambda e: e.dma_start(out=out, in_=in_), reads=r, writes=w)


def DMAN(P, eng, chan, pairs, r, w):
    pairs = list(pairs)
    P.dma(eng, chan, lambda e: [e.dma_start(out=o, in_=i) for (o, i) in pairs], reads=r, writes=w, n=len(pairs))


class Ctx:
    pass


def build(stages=("a0", "m0", "a1", "m1"), S=4096, debug=None):
    NT = S // 128
    NG = S // 512
    nc = bass.Bass("TRN2", target_bir_lowering=False)
    P = Prog(nc)
    C = Ctx()
    C.S, C.NT, C.NG = S, NT, NG

    def din(name, shape, dt=F32):
        return nc.dram_tensor(name, list(shape), dt, kind="ExternalInput").ap()

    def dscr(name, shape, dt):
        return nc.dram_tensor(name, list(shape), dt, kind="Internal").ap()

    dr = {}
    dr["x"] = din("x", [S, D])
    dr["y"] = nc.dram_tensor("y", [S, D], F32, kind="ExternalOutput").ap()
    dr["gb"] = din("gb", [5, 128, D])
    dr["gqkv"] = din("gqkv", [128, 512])
    dr["tb"] = din("tb", [128, H, 256])
    dr["b31"] = din("b31", [128, H])
    dr["wqkv"] = din("wqkv", [D, 3 * D])
    dr["wo0"] = din("wo0", [D, D])
    dr["win"] = din("win", [D, 584])
    dr["wuq"] = din("wuq", [256, D])
    dr["wqi"] = din("wqi", [256, 512])
    dr["wukT"] = din("wukT", [256, D])
    dr["wuv"] = din("wuv", [256, D])
    dr["wo1"] = din("wo1", [D, D])
    dr["wup0"] = din("wup0", [D, DFF])
    dr["wup1"] = din("wup1", [D, DFF])
    dr["wdn0"] = din("wdn0", [DFF, D])
    dr["wdn1"] = din("wdn1", [DFF, D])
    dr["qT"] = dscr("qT", [8, 128, S], BF16)
    dr["kT"] = dscr("kT", [8, 128, S], BF16)
    dr["va"] = dscr("va", [H, 128, NT, 128], BF16)
    dr["qiT"] = dscr("qiT", [4, 128, S], BF16)
    xs = [dr["x"]]
    for i, stg in enumerate(stages):
        if i == len(stages) - 1:
            xs.append(dr["y"])
        else:
            xs.append(dscr("xs%d" % i, [S, D], F32))

    C.pb = [P.ps("pb%d" % i, [128, 512], F32) for i in range(8)]
    A = Arena(P, 200)
    C.A = A
    C.ident = A.alloc([128], BF16)
    C.identB = A.alloc([128], BF16)
    C.io = A.alloc([128], F32)
    C.ssb = A.alloc([64], F32)
    C.ksum = A.alloc([8, 16], F32)
    P.op("pool", lambda e: e.iota(C.io, pattern=[[1, 128]], base=0, channel_multiplier=-1,
                                  allow_small_or_imprecise_dtypes=True), writes=["io"])
    TS(P, "dve", C.ident, C.io, 0.0, None, ALU.is_equal, None, ["io"], ["ident"])
    TS(P, "dve", C.identB, C.io, 0.0, BIG, ALU.is_equal, ALU.mult, ["io"], ["identB"])
    base_off = A.off
    P.barrier()

    for i, stg in enumerate(stages):
        A.off = base_off
        last = (i == len(stages) - 1)
        if stg == "a0":
            attn_phaseA(P, C, dr, xs[i], 0)
            P.barrier()
            A.off = base_off
            if debug is not None and debug.startswith("A"):
                dbg_copy(P, C, dr, xs[i], xs[i + 1])
            else:
                attn_phaseB(P, C, dr, xs[i], xs[i + 1], 0, debug)
        elif stg == "a1":
            C.kiTz = [A.alloc([S], BF16) for _ in range(2)]
            C.wabs = A.alloc([NT, 8], F32)
            C.wsgn = A.alloc([NT, 8], F32)
            base1 = A.off
            attn_phaseA(P, C, dr, xs[i], 1)
            P.barrier()
            A.off = base1
            attn_phaseB(P, C, dr, xs[i], xs[i + 1], 1)
        elif stg == "m0":
            mlp_phase(P, C, dr, xs[i], xs[i + 1], 0, False)
        elif stg == "m1":
            mlp_phase(P, C, dr, xs[i], xs[i + 1], 1, True)
        P.barrier()
    P.emit()
    return nc


def load_w_cast(P, dst3, src, nk, ncols, name, c0=0):
    pairs = []
    step = 2048
    for k in range(nk):
        for cc in range(0, ncols, step):
            w = min(step, ncols - cc)
            pairs.append((dst3[:, k, cc:cc + w], src[k * 128:(k + 1) * 128, c0 + cc:c0 + cc + w]))
    DMAN(P, "pool", "w_" + name, pairs, [], [name])


class Normer:
    def __init__(self, P, C, gb_dram_row, nbuf=3, tag="n"):
        A = C.A
        self.P, self.C = P, C
        self.gb = A.alloc([D], F32)
        DMA(P, "sp", "gbld", self.gb, gb_dram_row, [], ["gb"])
        self.nbuf = nbuf
        self.xt = [A.alloc([D], F32) for _ in range(nbuf)]
        self.xn = [A.alloc([D], BF16) for _ in range(2)]
        self.junk = A.alloc([D], BF16)
        self.cnt = 0

    def load(self, src_tile):
        i = self.cnt % self.nbuf
        DMA(self.P, "sp", "xt%d" % i, self.xt[i], src_tile, [], ["xt%d" % i])
        return i

    def norm_T(self, i, dstT, c0, dst_name, evac_eng="act"):
        P, C = self.P, self.C
        k = self.cnt
        self.cnt += 1
        xt = self.xt[i]
        xn = self.xn[k % 2]
        xnn = "xn%d" % (k % 2)
        ss = C.ssb[:, (k % 8) * 2:(k % 8) * 2 + 1]
        rs = C.ssb[:, (k % 8) * 2 + 1:(k % 8) * 2 + 2]
        sn = "ss%d" % (k % 8)
        ACT(P, self.junk, xt, AF.Square, ["xt%d" % i], ["junk", sn], accum=ss)
        with P.strict():
            ACT(P, rs, ss, AF.Ln, [sn], [sn + "r"], scale=1.0 / D, bias=EPS)
            ACT(P, rs, rs, AF.Exp, [sn + "r"], [sn + "r"], scale=-0.5)
        STT(P, xn, xt, rs, self.gb, ALU.mult, ALU.mult, ["xt%d" % i, sn + "r", "gb"], [xnn])
        pbi = k % 2
        pbv = C.pb[pbi][:].bitcast(BF16)
        for kc in range(8):
            TR(P, pbv[:, kc * 128:(kc + 1) * 128], xn[:, kc * 128:(kc + 1) * 128], C.ident, [xnn, "ident"], ["pb%d" % pbi])
        COPY(P, evac_eng, dstT[:, :, c0:c0 + 128], pbv.rearrange("p (a b) -> p a b", a=8), ["pb%d" % pbi], [dst_name])


def attn_phaseA(P, C, dr, xin, layer):
    A = C.A
    S, NT, NG = C.S, C.NT, C.NG
    nm = Normer(P, C, dr["gb"][2 * layer], nbuf=3)
    hnT = [A.alloc([8, 512], BF16) for _ in range(2)]
    qst = A.alloc([8, 512], BF16)
    kst = A.alloc([8, 512], BF16)
    vst = [A.alloc([8, 2, 128], BF16) for _ in range(2)]
    for b in range(2):
        P.op("pool", (lambda e, b=b: e.memset(vst[b], 1.0)), writes=["vst%d" % b])
    if layer == 0:
        wq = A.alloc([8, D], BF16)
        wk = A.alloc([8, D], BF16)
        wv = A.alloc([8, D], BF16)
        load_w_cast(P, wq, dr["wqkv"], 8, D, "wq", 0)
        load_w_cast(P, wk, dr["wqkv"], 8, D, "wk", D)
        load_w_cast(P, wv, dr["wqkv"], 8, D, "wv", 2 * D)
        P.op("dve", lambda e: e.memset(C.ksum, 0.0), writes=["ksum"])
    else:
        win = A.alloc([8, 584], BF16)
        wuq = A.alloc([2, D], BF16)
        wqi = A.alloc([2, 512], BF16)
        wuk = A.alloc([2, D], BF16)
        wuv = A.alloc([2, D], BF16)
        load_w_cast(P, win, dr["win"], 8, 584, "win")
        load_w_cast(P, wuq, dr["wuq"], 2, D, "wuq")
        load_w_cast(P, wqi, dr["wqi"], 2, 512, "wqi")
        load_w_cast(P, wuk, dr["wukT"], 2, D, "wuk")
        load_w_cast(P, wuv, dr["wuv"], 2, D, "wuv")
        gq = A.alloc([512], F32)
        DMA(P, "sp", "gqld", gq, dr["gqkv"], [], ["gq"])
        cn = [A.alloc([512], BF16) for _ in range(2)]
        ki2 = [A.alloc([2, 128], BF16) for _ in range(2)]
        for b in range(2):
            P.op("pool", (lambda e, b=b: e.memset(ki2[b], 0.0)), writes=["ki2%d" % b])
        cnT = [A.alloc([4, 512], BF16) for _ in range(2)]
        qist = A.alloc([4, 512], BF16)
        junk2 = A.alloc([256], BF16)
    rot = [2, 3, 4, 5, 6, 7]
    rc = [0]

    def nextbank():
        b = rot[rc[0] % len(rot)]
        rc[0] += 1
        return b

    xtiles = xin.rearrange("(t p) d -> t p d", p=128)
    for g in range(NG):
        hb = g % 2
        hT = hnT[hb]
        hname = "hnT%d" % hb
        for j in range(4):
            t = g * 4 + j
            i = nm.load(xtiles[t])
            nm.norm_T(i, hT, j * 128, hname, evac_eng="act" if layer == 0 else "dve")
        if layer == 0:
            for which, wmat, stg, sname in (("q", wq, qst, "qst"), ("k", wk, kst, "kst")):
                for hp in range(8):
                    b = nextbank()
                    for kc in range(8):
                        MM(P, C.pb[b][:, :], wmat[:, kc, hp * 128:(hp + 1) * 128], hT[:, kc, :], kc == 0, kc == 7,
                           ["w" + which, hname], ["pb%d" % b])
                    if which == "q":
                        ACT(P, stg[:, hp, :], C.pb[b][:, :], AF.Identity, ["pb%d" % b], [sname], scale=0.125)
                    else:
                        COPY(P, "act", stg[:, hp, :], C.pb[b][:, :], ["pb%d" % b], [sname])
                        RED(P, C.ksum[:, hp, 2 * g:2 * g + 2], C.pb[b][:, :].rearrange("p (a b) -> p a b", a=2), ALU.add,
                            ["pb%d" % b], ["ksum"])
                dst = dr["qT" if which == "q" else "kT"][:, :, g * 512:(g + 1) * 512].rearrange("h p t -> p h t")
                DMA(P, "sp", sname, dst, stg, [sname], [])
        else:
            for j in range(4):
                t = g * 4 + j
                tc = slice(j * 128, (j + 1) * 128)
                ba = nextbank()
                bb = nextbank()
                for kc in range(8):
                    MM(P, C.pb[ba][:, :], hT[:, kc, tc], win[:, kc, 0:512], kc == 0, kc == 7, [hname, "win"], ["pb%d" % ba])
                for kc in range(8):
                    MM(P, C.pb[bb][:, 0:72], hT[:, kc, tc], win[:, kc, 512:584], kc == 0, kc == 7, [hname, "win"], ["pb%d" % bb])
                cb = t % 2
                ssq = C.ssb[:, 32 + cb * 4:32 + cb * 4 + 1]
                ssk = C.ssb[:, 32 + cb * 4 + 1:32 + cb * 4 + 2]
                rsq = C.ssb[:, 32 + cb * 4 + 2:32 + cb * 4 + 3]
                rsk = C.ssb[:, 32 + cb * 4 + 3:32 + cb * 4 + 4]
                sn = "cs%d" % cb
                ACT(P, junk2, C.pb[ba][:, 0:256], AF.Square, ["pb%d" % ba], ["junk2", sn], accum=ssq)
                ACT(P, junk2, C.pb[ba][:, 256:512], AF.Square, ["pb%d" % ba], ["junk2", sn], accum=ssk)
                with P.strict():
                    ACT(P, rsq, ssq, AF.Ln, [sn], [sn + "q"], scale=1.0 / 256, bias=EPS)
                    ACT(P, rsq, rsq, AF.Exp, [sn + "q"], [sn + "q"], scale=-0.5)
                    ACT(P, rsk, ssk, AF.Ln, [sn], [sn + "k"], scale=1.0 / 256, bias=EPS)
                    ACT(P, rsk, rsk, AF.Exp, [sn + "k"], [sn + "k"], scale=-0.5)
                cname = "cn%d" % cb
                STT(P, cn[cb][:, 0:256], C.pb[ba][:, 0:256], rsq, gq[:, 0:256], ALU.mult, ALU.mult,
                    ["pb%d" % ba, sn + "q", "gq"], [cname])
                STT(P, cn[cb][:, 256:512], C.pb[ba][:, 256:512], rsk, gq[:, 256:512], ALU.mult, ALU.mult,
                    ["pb%d" % ba, sn + "k", "gq"], [cname])
                kname = "ki2%d" % cb
                COPY(P, "act", ki2[cb][:, 0, 0:64], C.pb[bb][:, 0:64], ["pb%d" % bb], [kname])
                COPY(P, "act", ki2[cb][:, 1, 64:128], C.pb[bb][:, 0:64], ["pb%d" % bb], [kname])
                TS(P, "dve", C.wabs[:, t, :], C.pb[bb][:, 64:72], (8.0 ** -0.5) * (64.0 ** -0.5), None, ALU.mult, None,
                   ["pb%d" % bb], ["wabs"])
                TS(P, "dve", C.wsgn[:, t, :], C.wabs[:, t, :], 0.0, 2.0, ALU.is_ge, ALU.mult, ["wabs"], ["wsgn"])
                TS(P, "dve", C.wsgn[:, t, :], C.wsgn[:, t, :], -1.0, None, ALU.add, None, ["wsgn"], ["wsgn"])
                STT(P, C.wabs[:, t, :], C.wabs[:, t, :], -1.0, C.wabs[:, t, :], ALU.mult, ALU.max, ["wabs"], ["wabs"])
                pbv = C.pb[cb][:].bitcast(BF16)
                for q4 in range(4):
                    TR(P, pbv[:, q4 * 128:(q4 + 1) * 128], cn[cb][:, q4 * 128:(q4 + 1) * 128], C.ident, [cname, "ident"], ["pb%d" % cb])
                TR(P, pbv[:, 512:640], ki2[cb][:, 0, :], C.ident, [kname, "ident"], ["pb%d" % cb])
                TR(P, pbv[:, 640:768], ki2[cb][:, 1, :], C.ident, [kname, "ident"], ["pb%d" % cb])
                COPY(P, "act", cnT[hb][:, :, tc], pbv[:, 0:512].rearrange("p (a b) -> p a b", a=4), ["pb%d" % cb], ["cnT%d" % hb])
                COPY(P, "act", C.kiTz[0][:, t * 128:(t + 1) * 128], pbv[:, 512:640], ["pb%d" % cb], ["kiT"])
                COPY(P, "act", C.kiTz[1][:, t * 128:(t + 1) * 128], pbv[:, 640:768], ["pb%d" % cb], ["kiT"])
            cT = cnT[hb]
            cTn = "cnT%d" % hb
            for hp in range(8):
                b = nextbank()
                for kc in range(2):
                    MM(P, C.pb[b][:, :], wuq[:, kc, hp * 128:(hp + 1) * 128], cT[:, kc, :], kc == 0, kc == 1, ["wuq", cTn], ["pb%d" % b])
                ACT(P, qst[:, hp, :], C.pb[b][:, :], AF.Identity, ["pb%d" % b], ["qst"], scale=0.125)
            DMA(P, "sp", "qst", dr["qT"][:, :, g * 512:(g + 1) * 512].rearrange("h p t -> p h t"), qst, ["qst"], [])
            for ch in range(4):
                b = nextbank()
                for kc in range(2):
                    MM(P, C.pb[b][:, :], wqi[:, kc, ch * 128:(ch + 1) * 128], cT[:, kc, :], kc == 0, kc == 1, ["wqi", cTn], ["pb%d" % b])
                COPY(P, "dve", qist[:, ch, :], C.pb[b][:, :], ["pb%d" % b], ["qist"])
            DMA(P, "sp", "qist", dr["qiT"][:, :, g * 512:(g + 1) * 512].rearrange("h p t -> p h t"), qist, ["qist"], [])
            for hp in range(8):
                b = nextbank()
                for kc in range(2):
                    MM(P, C.pb[b][:, :], wuk[:, kc, hp * 128:(hp + 1) * 128], cT[:, 2 + kc, :], kc == 0, kc == 1, ["wuk", cTn], ["pb%d" % b])
                COPY(P, "act", kst[:, hp, :], C.pb[b][:, :], ["pb%d" % b], ["kst"])
            DMA(P, "sp", "kst", dr["kT"][:, :, g * 512:(g + 1) * 512].rearrange("h p t -> p h t"), kst, ["kst"], [])
        for j in range(4):
            t = g * 4 + j
            tc = slice(j * 128, (j + 1) * 128)
            vb = t % 2
            vname = "vst%d" % vb
            for half in range(2):
                b = nextbank()
                if layer == 0:
                    for kc in range(8):
                        MM(P, C.pb[b][:, :], hT[:, kc, tc], wv[:, kc, half * 512:(half + 1) * 512], kc == 0, kc == 7, [hname, "wv"], ["pb%d" % b])
                else:
                    for kc in range(2):
                        MM(P, C.pb[b][:, :], cT[:, 2 + kc, tc], wuv[:, kc, half * 512:(half + 1) * 512], kc == 0, kc == 1, [cTn, "wuv"], ["pb%d" % b])
                psv = C.pb[b][:, :].rearrange("p (a b c) -> p a b c", a=4, b=2)
                COPY(P, "dve", vst[vb][:, half * 4:(half + 1) * 4, 0, 0:64], psv[:, :, 0, :], ["pb%d" % b], [vname])
                COPY(P, "dve", vst[vb][:, half * 4:(half + 1) * 4, 1, 64:128], psv[:, :, 1, :], ["pb%d" % b], [vname])
            dst = dr["va"][:, :, t, :].rearrange("(a b) p d -> p a b d", b=2)
            DMA(P, "sp", vname, dst, vst[vb], [vname], [])


def dbg_copy(P, C, dr, xin, xout):
    A = C.A
    xt = A.alloc([D], F32)
    xin_t = xin.rearrange("(t p) d -> t p d", p=128)
    xout_t = xout.rearrange("(t p) d -> t p d", p=128)
    for t in range(C.NT):
        DMA(P, "sp", "dbgl", xt, xin_t[t], [], ["dbgx"])
        DMA(P, "pool", "dbgs", xout_t[t], xt, ["dbgx"], [])


SALL = ["score%d" % i_ for i_ in range(8)]


def attn_phaseB(P, C, dr, xin, xout, layer, debug=None):
    A = C.A
    S, NT, NG = C.S, C.NT, C.NG
    wo = A.alloc([8, D], BF16)
    load_w_cast(P, wo, dr["wo0" if layer == 0 else "wo1"], 8, D, "wo")
    if layer == 1:
        score = A.alloc([max(S, H * 256)], F32)
        tbf = score[:, 0:H * 256].rearrange("p (a b) -> p a b", a=H)
        tbn = "score0"
    else:
        tbf = A.alloc([H, 256], F32)
        tbn = "tbf"
    b31 = A.alloc([H], F32)
    cm0 = A.alloc([128], F32)
    TBp = A.alloc([H, 256], BF16)
    DMA(P, "sp", "tbld", tbf, dr["tb"], [], SALL if layer == 1 else [tbn])
    DMA(P, "sp", "b31ld", b31, dr["b31"], [], ["b31"])
    TS(P, "dve", cm0, C.io, 0.0, -BIG, ALU.is_lt, ALU.mult, ["io"], ["cm0"])
    for h in range(H):
        STT(P, TBp[:, h, 0:128], tbf[:, h, 0:128], b31[:, h:h + 1], cm0, ALU.subtract, ALU.add, (SALL if layer == 1 else [tbn]) + ["b31", "cm0"], ["TBp"])
        TS(P, "dve", TBp[:, h, 128:256], tbf[:, h, 128:256], b31[:, h:h + 1], None, ALU.subtract, None, (SALL if layer == 1 else [tbn]) + ["b31"], ["TBp"])
    qz = [[A.alloc([8, 512], BF16) for _ in range(2)] for _ in range(2)]
    for b in range(2):
        P.op("pool", (lambda e, b=b: e.memset(qz[b][0][64:128], 0.0)), writes=["qg%d" % b])
        P.op("pool", (lambda e, b=b: e.memset(qz[b][1][0:64], 0.0)), writes=["qg%d" % b])
    kbuf = [A.alloc([S], BF16) for _ in range(2)]
    vbuf = [A.alloc([NT, 128], BF16) for _ in range(2)]
    pbuf = [A.alloc([512], BF16) for _ in range(3)]
    oT = A.alloc([8, 512], BF16)
    rec = A.alloc([512], F32)
    xt = [A.alloc([D], F32) for _ in range(2)]
    if layer == 0:
        kbd = A.alloc([8, 32], BF16)
        nidx = A.alloc([256], F32)
        pneg = A.alloc([256], F32)
        ownm1 = A.alloc([256], F32)
        gm = A.alloc([256], F32)
        gm2 = A.alloc([256], F32)
        tmpg = A.alloc([256], F32)
        mx = A.alloc([16], F32)
        selv = A.alloc([256], BF16)
        selT = A.alloc([H, 512], BF16)
        Esel = A.alloc([16, 128], BF16)
        iot = A.alloc([16, 128], F32)
        P.op("pool", lambda e: e.memset(selT, 0.0), writes=["selT"])
        P.op("dve", lambda e: e.memset(kbd, 0.0), writes=["kbd"])
        COPY(P, "dve", kbd[0:64, :, 0:16], C.ksum[0:64, :, :], ["ksum", "kbd"], ["kbd"])
        COPY(P, "dve", kbd[64:128, :, 16:32], C.ksum[64:128, :, :], ["ksum", "kbd"], ["kbd"])
        P.op("pool", lambda e: e.iota(nidx.rearrange("p (a b) -> p a b", a=16), pattern=[[0, 16], [1, 16]], base=0,
                                      channel_multiplier=0, allow_small_or_imprecise_dtypes=True), writes=["nidx"])
        P.op("pool", lambda e: e.iota(iot, pattern=[[1, 16], [0, 128]], base=0, channel_multiplier=-1,
                                      allow_small_or_imprecise_dtypes=True), writes=["iot"])
        TS(P, "dve", Esel, iot, 0.0, BIG, ALU.is_equal, ALU.mult, ["iot"], ["Esel"])
    else:
        qig = A.alloc([4, 512], BF16)
        madd = A.alloc([S], BF16)
        junkb = madd
        maskT = A.alloc([NT, 512], BF16)
        cneg = A.alloc([128], F32)
        cpos = A.alloc([128], F32)
        dtmp = A.alloc([128], F32)
        bs = A.alloc([16], F32)
        bsi = A.alloc([4], I32)
        TS(P, "dve", cneg, C.io, 0.0, NEGF, ALU.is_gt, ALU.mult, ["io"], ["cneg"])
        TS(P, "dve", cpos, C.io, 0.0, -NEGF, ALU.is_gt, ALU.mult, ["io"], ["cpos"])
    xin_t = xin.rearrange("(t p) d -> t p d", p=128)
    xout_t = xout.rearrange("(t p) d -> t p d", p=128)
    lrot = [2, 3, 4]
    lc = [0]
    ucnt = [0]
    for g in range(NG):
        qb = g % 2
        qname = "qg%d" % qb
        QZ = qz[qb]
        DMAN(P, "sp", qname, [(QZ[0][0:64], dr["qT"][:, 0:64, g * 512:(g + 1) * 512].rearrange("h p t -> p h t")),
                              (QZ[1][64:128], dr["qT"][:, 64:128, g * 512:(g + 1) * 512].rearrange("h p t -> p h t"))], [], [qname])
        nch = 4 * (g + 1)
        if layer == 0:
            for j in range(4):
                t = g * 4 + j
                blk = t // 2
                if t % 2 == 0:
                    TS(P, "dve", pneg, nidx, float(blk), NEGF, ALU.is_ge, ALU.mult, ["nidx"], ["pneg"])
                    TS(P, "dve", ownm1, nidx, float(blk), -1.0, ALU.is_ge, ALU.add, ["nidx"], ["ownm1"])
                for hp in range(8):
                    for h2 in range(2):
                        MM(P, C.pb[0][:, hp * 32:(hp + 1) * 32], QZ[h2][:, hp, j * 128:(j + 1) * 128], kbd[:, hp, :], h2 == 0, h2 == 1,
                           [qname, "kbd"], ["pb0"])
                P.strict_default = True
                TT(P, "dve", gm, C.pb[0][:, 0:256], pneg, ALU.add, ["pb0", "pneg"], ["gm"])
                g3 = gm.rearrange("p (a b) -> p a b", a=16)
                g23 = gm2.rearrange("p (a b) -> p a b", a=16)
                t3 = tmpg.rearrange("p (a b) -> p a b", a=16)
                mb = bcast_last(mx, 16)
                RED(P, mx, g3, ALU.max, ["gm"], ["mx"])
                TT(P, "dve", t3, g3, mb, ALU.is_ge, ["gm", "mx"], ["tmpg"])
                STT(P, gm2, tmpg, NEGF, gm, ALU.mult, ALU.add, ["tmpg", "gm"], ["gm2"])
                RED(P, mx, g23, ALU.max, ["gm2"], ["mx"])
                TT(P, "dve", t3, g23, mb, ALU.is_ge, ["gm2", "mx"], ["tmpg"])
                STT(P, gm2, tmpg, NEGF, gm2, ALU.mult, ALU.add, ["tmpg", "gm2"], ["gm2"])
                RED(P, mx, g23, ALU.max, ["gm2"], ["mx"])
                TT(P, "dve", t3, g3, mb, ALU.is_ge, ["gm", "mx"], ["tmpg"])
                STT(P, selv, tmpg, -1.0, ownm1, ALU.add, ALU.max, ["tmpg", "ownm1"], ["selv"])
                P.strict_default = False
                pv1 = C.pb[1][:].bitcast(BF16)
                for rnd in range(2):
                    for hh in range(8):
                        h = rnd * 8 + hh
                        TR(P, pv1[0:16, hh * 128:(hh + 1) * 128], selv[:, h * 16:(h + 1) * 16], C.ident, ["selv", "ident"], ["pb1"])
                    COPY(P, "act", selT[0:16, rnd * 8:(rnd + 1) * 8, j * 128:(j + 1) * 128],
                         pv1[0:16, :].rearrange("p (a b) -> p a b", a=8), ["pb1"], ["selT"])
        else:
            DMA(P, "sp", "qig", qig, dr["qiT"][:, :, g * 512:(g + 1) * 512].rearrange("h p t -> p h t"), [], ["qig"])
            for j in range(4):
                t = g * 4 + j
                nk = 128 * (t + 1)
                qc = slice(j * 128, (j + 1) * 128)
                nsg = (nk + 511) // 512
                for hi in range(8):
                    ch = hi // 2
                    for sg in range(nsg):
                        ncol = min(512, nk - sg * 512)
                        sc = slice(sg * 512, sg * 512 + ncol)
                        bm = (0, 1, 2)[ucnt[0] % 3]
                        br = (3, 4, 6, 7)[ucnt[0] % 4]
                        ucnt[0] += 1
                        sn_ = "score%d" % sg
                        MM(P, C.pb[bm][:, 0:ncol], qig[:, ch, qc], C.kiTz[hi % 2][:, sc], True, True, ["qig", "kiT"], ["pb%d" % bm])
                        ACT(P, C.pb[br][:, 0:ncol], C.pb[bm][:, 0:ncol], AF.Relu, ["pb%d" % bm, "wabs"], ["pb%d" % br],
                            scale=C.wabs[:, t, hi:hi + 1])
                        if hi == 0:
                            TS(P, "dve", score[:, sc], C.pb[br][:, 0:ncol], C.wsgn[:, t, 0:1], None, ALU.mult, None,
                               ["pb%d" % br, "wsgn"], [sn_])
                        else:
                            STT(P, score[:, sc], C.pb[br][:, 0:ncol], C.wsgn[:, t, hi:hi + 1], score[:, sc], ALU.mult, ALU.add,
                                ["pb%d" % br, "wsgn", sn_], [sn_])
                dg = slice(128 * t, 128 * t + 128)
                lo, hi_, rg, mid, cnt, m1 = (bs[:, k:k + 1] for k in range(6))
                fl = bsi[:, 0:1]
                P.strict_default = True
                TT(P, "dve", dtmp, score[:, dg], cpos, ALU.add, SALL + ["cpos"], ["dtmp"])
                RED(P, lo, dtmp, ALU.min, ["dtmp"], ["bs"])
                if t > 0:
                    RED(P, m1, score[:, 0:128 * t], ALU.min, SALL, ["bs"])
                    TT(P, "dve", lo, lo, m1, ALU.min, ["bs"], ["bs"])
                TT(P, "dve", score[:, dg], score[:, dg], cneg, ALU.add, SALL + ["cneg"], SALL)
                if nk > 256:
                    RED(P, hi_, score[:, 0:nk], ALU.max, SALL, ["bs"])
                    TT(P, "dve", rg, hi_, lo, ALU.subtract, ["bs"], ["bs"])
                    for it in range(NBIS):
                        TS(P, "dve", mid, rg, 2.0 ** -(it + 1), lo, ALU.mult, ALU.add, ["bs"], ["bs"])
                        TS(P, "dve", junkb[:, 0:nk], score[:, 0:nk], mid, None, ALU.is_ge, ALU.add, SALL + ["bs"], ["junkb", "bs"], accum=cnt)
                        TS(P, "dve", fl, cnt, 256.0, None, ALU.is_ge, None, ["bs"], ["bsi"])
                        P.op("dve", (lambda e, lo=lo, fl=fl, mid=mid: e.copy_predicated(out=lo, mask=fl, data=mid)), reads=["bs", "bsi"], writes=["bs"])
                TS(P, "dve", madd[:, 0:nk], score[:, 0:nk], lo, 1.0, ALU.is_ge, ALU.subtract, SALL + ["bs"], ["madd"])
                P.strict_default = False
                pv7 = C.pb[5][:].bitcast(BF16)
                for c0 in range(0, t + 1, 8):
                    cn_ = min(8, t + 1 - c0)
                    for cc in range(cn_):
                        c = c0 + cc
                        TR(P, pv7[:, cc * 128:(cc + 1) * 128], madd[:, c * 128:(c + 1) * 128], C.ident, ["madd", "ident"], ["pb5"])
                    COPY(P, "act", maskT[:, c0:c0 + cn_, qc], pv7[:, 0:cn_ * 128].rearrange("p (a b) -> p a b", a=cn_), ["pb5"], ["maskT"])
        items = [(h, c) for h in range(H) for c in range(nch)]
        LOOK = 3
        lbanks = [2, 3, 4, 7] if layer == 0 else [0, 1, 2, 5]

        def hinfo(h):
            hp, h2 = h // 2, h % 2
            r0 = 64 * h2
            return hp, h2, slice(r0, r0 + 64), slice(64 - r0, 128 - r0), hp % 2, h % 2

        def issue_qk(idx):
            h, c = items[idx]
            hp, h2, rs_, so, kb, vb = hinfo(h)
            kname = "kbuf%d" % kb
            if c == 0:
                if h2 == 0:
                    DMA(P, "sp", kname, kbuf[kb][:, 0:nch * 128], dr["kT"][hp, :, 0:nch * 128], [], [kname])
                DMA(P, "sp", "vbuf%d" % vb, vbuf[vb][:, 0:nch, :], dr["va"][h, :, 0:nch, :], [], ["vbuf%d" % vb])
            dc = c - 4 * g
            col0 = max(0, dc * 128)
            cs = slice(col0, 512)
            lb = lbanks[idx % 4]
            lbn = "pb%d" % lb
            psl = C.pb[lb]
            n_blk = c // 2
            need_sel = (layer == 1) or (n_blk < 2 * g + 1)
            need_tb = dc >= -1
            MM(P, psl[:, cs], kbuf[kb][:, c * 128:(c + 1) * 128], QZ[h2][:, hp, cs], True, not (need_sel or need_tb), [kname, qname], [lbn])
            if need_sel:
                if layer == 0:
                    MM(P, psl[:, cs], Esel[:, n_blk, :], selT[:, h, cs], False, not need_tb, ["Esel", "selT"], [lbn])
                else:
                    MM(P, psl[:, cs], C.identB, maskT[:, c, cs], False, not need_tb, ["identB", "maskT"], [lbn])
            if need_tb:
                if dc == -1:
                    MM(P, psl[:, 0:128], C.ident, TBp[:, h, 128:256], False, True, ["ident", "TBp"], [lbn])
                elif dc == 3:
                    MM(P, psl[:, 384:512], C.ident, TBp[:, h, 0:128], False, True, ["ident", "TBp"], [lbn])
                else:
                    MM(P, psl[:, col0:col0 + 256], C.ident, TBp[:, h, 0:256], False, True, ["ident", "TBp"], [lbn])

        for idx in range(min(LOOK, len(items))):
            issue_qk(idx)
        for idx in range(len(items)):
            if idx + LOOK < len(items):
                issue_qk(idx + LOOK)
            h, c = items[idx]
            hp, h2, rs_, so, kb, vb = hinfo(h)
            ob = 5 + (h % 2) if layer == 0 else 3 + (h % 2)
            obn = "pb%d" % ob
            pso = C.pb[ob]
            dc = c - 4 * g
            col0 = max(0, dc * 128)
            cs = slice(col0, 512)
            lb = lbanks[idx % 4]
            lbn = "pb%d" % lb
            pi = idx % 3
            pn = "pbuf%d" % pi
            ACT(P, pbuf[pi][:, cs], C.pb[lb][:, cs], AF.Exp, [lbn], [pn])
            MM(P, pso[:, cs], vbuf[vb][:, c, :], pbuf[pi][:, cs], c == 0, c == nch - 1, ["vbuf%d" % vb, pn], [obn])
            if c == nch - 1:
                ACT(P, rec[so, :], pso[so, :], AF.Ln, [obn], ["rec"])
                ACT(P, rec[so, :], rec[so, :], AF.Exp, ["rec"], ["rec"], scale=-1.0)
                TT(P, "dve", oT[rs_, hp, :], pso[rs_, :], rec[so, :], ALU.mult, [obn, "rec"], ["oT"])
        for j in range(4):
            t = g * 4 + j
            xb = t % 2
            xn_ = "xta%d" % xb
            DMA(P, "sp", xn_, xt[xb], xin_t[t], [], [xn_])
            for half in range(2):
                b = 6 + half if layer == 1 else (0, 1)[half]
                bn = "pb%d" % b
                for hp in range(8):
                    MM(P, C.pb[b][:, :], oT[:, hp, j * 128:(j + 1) * 128], wo[:, hp, half * 512:(half + 1) * 512], hp == 0, hp == 7, ["oT", "wo"], [bn])
                hs = slice(half * 512, (half + 1) * 512)
                TT(P, "dve", xt[xb][:, hs], C.pb[b][:, :], xt[xb][:, hs], ALU.add, [bn, xn_], [xn_])
            DMA(P, "pool", "xst%d" % xb, xout_t[t], xt[xb], [xn_], [])


def mlp_phase(P, C, dr, xin, xout, layer, final):
    A = C.A
    S, NT = C.S, C.NT
    wup = A.alloc([8, DFF], BF16)
    wdn = A.alloc([32, D], BF16)
    load_w_cast(P, wup, dr["wup%d" % layer], 8, DFF, "wup")
    load_w_cast(P, wdn, dr["wdn%d" % layer], 32, D, "wdn")
    nm = Normer(P, C, dr["gb"][2 * layer + 1], nbuf=4)
    if final:
        fnb = A.alloc([D], F32)
        DMA(P, "sp", "fnld", fnb, dr["gb"][4], [], ["fnb"])
        junkf = A.alloc([D], BF16)
    hnT = [A.alloc([8, 256], BF16) for _ in range(2)]
    hTt = A.alloc([32, 256], BF16)
    rbuf = [A.alloc([512], BF16) for _ in range(3)]
    xin_t = xin.rearrange("(t p) d -> t p d", p=128)
    xout_t = xout.rearrange("(t p) d -> t p d", p=128)
    rcnt = [0]
    for g2 in range(NT // 2):
        hb = g2 % 2
        hname = "mhT%d" % hb
        xi = []
        for j in range(2):
            t = g2 * 2 + j
            i = nm.load(xin_t[t])
            xi.append(i)
            nm.norm_T(i, hnT[hb], j * 128, hname, evac_eng="dve")
        for f2 in range(16):
            b = 2 + (f2 % 2)
            bn = "pb%d" % b
            for q in range(2):
                ffc = f2 * 2 + q
                for kc in range(8):
                    MM(P, C.pb[b][:, q * 256:(q + 1) * 256], wup[:, kc, ffc * 128:(ffc + 1) * 128], hnT[hb][:, kc, :], kc == 0, kc == 7,
                       ["wup", hname], [bn])
            ri = rcnt[0] % 3
            rcnt[0] += 1
            rn = "rbuf%d" % ri
            ACT(P, rbuf[ri], C.pb[b][:, :], AF.Relu, [bn], [rn])
            TT(P, "dve", hTt[:, f2 * 2:f2 * 2 + 2, :], C.pb[b][:, :].rearrange("p (a b) -> p a b", a=2),
               rbuf[ri].rearrange("p (a b) -> p a b", a=2), ALU.mult, [bn, rn], ["hTt"])
        for j in range(2):
            t = g2 * 2 + j
            i = xi[j]
            xn_ = "xt%d" % i
            for half in range(2):
                b = 4 + j * 2 + half
                bn = "pb%d" % b
                for ffc in range(32):
                    MM(P, C.pb[b][:, :], hTt[:, ffc, j * 128:(j + 1) * 128], wdn[:, ffc, half * 512:(half + 1) * 512], ffc == 0, ffc == 31,
                       ["hTt", "wdn"], [bn])
                hs = slice(half * 512, (half + 1) * 512)
                TT(P, "dve", nm.xt[i][:, hs], C.pb[b][:, :], nm.xt[i][:, hs], ALU.add, [bn, xn_], [xn_])
            if final:
                ss = C.ssb[:, 48 + (t % 4) * 2:48 + (t % 4) * 2 + 1]
                rs = C.ssb[:, 48 + (t % 4) * 2 + 1:48 + (t % 4) * 2 + 2]
                sn = "fs%d" % (t % 4)
                ACT(P, junkf, nm.xt[i], AF.Square, [xn_], ["junkf", sn], accum=ss)
                with P.strict():
                    ACT(P, rs, ss, AF.Ln, [sn], [sn + "r"], scale=1.0 / D, bias=EPS)
                    ACT(P, rs, rs, AF.Exp, [sn + "r"], [sn + "r"], scale=-0.5)
                STT(P, nm.xt[i], nm.xt[i], rs, fnb, ALU.mult, ALU.mult, [xn_, sn + "r", "fnb"], [xn_])
            DMA(P, "pool", "xst%d" % i, xout_t[t], nm.xt[i], [xn_], [])


def _rel_bucket_np(dist):
    n = np.maximum(dist, 0)
    exact = 16
    nf = np.maximum(n, 1).astype(np.float32)
    large = exact + (np.log(nf / np.float32(exact)) / np.float32(math.log(128 / exact)) * np.float32(32 - exact)).astype(np.int32)
    large = np.minimum(large, 31)
    return np.where(n < exact, n, large)


def host_layout(inp):
    f = lambda a: np.ascontiguousarray(np.asarray(a, dtype=np.float32))
    rb = f(inp["rel_bias"])
    s_idx = np.arange(128)[:, None]
    q_idx = np.arange(128)[None, :]
    bk0 = _rel_bucket_np(q_idx - s_idx)
    bk1 = _rel_bucket_np(q_idx - s_idx + 128)
    tb = np.empty((128, H, 256), np.float32)
    tb[:, :, 0:128] = rb[bk0].transpose(0, 2, 1)
    tb[:, :, 128:256] = rb[bk1].transpose(0, 2, 1)
    gb = np.stack([inp["ln_attn"][0], inp["ln_mlp"][0], inp["ln_attn"][1], inp["ln_mlp"][1], inp["final_norm"]])
    gb = f(np.broadcast_to(gb[:, None, :], (5, 128, D)))
    gqkv = f(np.broadcast_to(np.concatenate([inp["dsa_g_q"][0], inp["dsa_g_kv"][0]])[None, :], (128, 512)))
    m = {
        "gb": gb, "gqkv": gqkv, "tb": f(tb), "b31": f(np.broadcast_to(rb[31][None, :], (128, H))),
        "wqkv": f(inp["moba_w_qkv"][0]), "wo0": f(inp["moba_w_o"][0]), "win": f(inp["dsa_w_in"][0]),
        "wuq": f(inp["dsa_w_uq"][0]), "wqi": f(inp["dsa_w_qi"][0]),
        "wukT": f(np.transpose(inp["dsa_w_uk"][0], (2, 0, 1)).reshape(256, D)),
        "wuv": f(np.transpose(inp["dsa_w_uv"][0], (1, 0, 2)).reshape(256, D)),
        "wo1": f(inp["dsa_w_o"][0]),
        "wup0": f(inp["mlp_w_up"][0]), "wup1": f(inp["mlp_w_up"][1]),
        "wdn0": f(inp["mlp_w_down"][0]), "wdn1": f(inp["mlp_w_down"][1]),
    }
    return m


_NC_CACHE = {}


def kernel(**inputs):
    x = np.asarray(inputs["x"], dtype=np.float32)
    B = x.shape[0]
    shared = host_layout(inputs)
    key = ("full", x.shape[1])
    if key not in _NC_CACHE:
        _NC_CACHE[key] = build(("a0", "m0", "a1", "m1"), S=x.shape[1])
    nc = _NC_CACHE[key]
    in_maps = []
    for b in range(B):
        m = dict(shared)
        m["x"] = np.ascontiguousarray(x[b])
        in_maps.append(m)
    res = run_bass_kernel_spmd(nc, in_maps, core_ids=list(range(B)))
    return np.stack([np.asarray(r["y"], dtype=np.float32) for r in res.results], axis=0)
```

```python
import math
import numpy as np
import concourse.bass as bass
import concourse.mybir as mybir
from concourse.bass_utils import run_bass_kernel_spmd
from contextlib import ExitStack

F32 = mybir.dt.float32
BF16 = mybir.dt.bfloat16
I32 = mybir.dt.int32
AF = mybir.ActivationFunctionType
ALU = mybir.AluOpType
AX = mybir.AxisListType

D = 1024
H = 16
DFF = 4096
BIG = 30000.0
NEGF = -1.0e30
EPS = 1e-6
NBIS = 16


class Op:
    __slots__ = ("eng", "fn", "deps", "needs_inc", "tok", "sem", "is_dma", "chan", "n")


class Prog:
    ENG = ("pe", "act", "dve", "pool", "sp")

    def __init__(self, nc):
        self.nc = nc
        self.ops = {e: [] for e in self.ENG}
        self.res = {}
        self.stack = ExitStack()
        self.chan_sems = {}
        self.chan_cnt = {}
        self.last = {e: None for e in self.ENG}
        self.last_dma = {}
        self.strict_default = False

    def sb(self, name, shape, dt):
        return self.stack.enter_context(self.nc.sbuf_tensor(name, list(shape), dt))

    def ps(self, name, shape, dt):
        return self.stack.enter_context(self.nc.psum_tensor(name, list(shape), dt))

    def _add(self, eng, fn, reads, writes, chan=None, n=1, strict=False):
        op = Op()
        op.eng, op.fn, op.needs_inc, op.is_dma, op.chan, op.n = eng, fn, False, chan is not None, chan, n
        deps = set()
        raw = set()
        for r in reads:
            st = self.res.get(r)
            if st is not None and st[0] is not None:
                deps.add(st[0])
                raw.add(st[0])
            if st is not None and r.startswith("pb"):
                deps.update(st[1])
        for w in writes:
            st = self.res.get(w)
            if st is not None:
                if st[0] is not None:
                    deps.add(st[0])
                deps.update(st[1])
        keep = []
        for d in deps:
            if d is op or d.fn is None:
                continue
            if (not d.is_dma) and d.eng == eng and eng == "pe":
                continue
            d.needs_inc = True
            keep.append(d)
        op.deps = keep
        for r in reads:
            lst = self.res.setdefault(r, [None, []])[1]
            if not op.is_dma:
                lst[:] = [o for o in lst if o.is_dma or o.eng != eng]
            lst.append(op)
        for w in writes:
            self.res[w] = [op, []]
        self.ops[eng].append(op)
        if op.is_dma:
            self.last_dma[chan] = op
        else:
            self.last[eng] = op
        return op

    def op(self, eng, fn, reads=(), writes=(), strict=None):
        if strict is None:
            strict = self.strict_default
        return self._add(eng, fn, reads, writes, strict=strict)

    def dma(self, eng, chan, fn, reads=(), writes=(), n=1):
        return self._add(eng, fn, reads, writes, chan=chan, n=n)

    def strict(self):
        P = self

        class _S:
            def __enter__(self_):
                self_.old = P.strict_default
                P.strict_default = True

            def __exit__(self_, *a):
                P.strict_default = self_.old
        return _S()

    def barrier(self):
        lasts = [o for o in self.last.values() if o is not None]
        dmas = list(self.last_dma.values())
        for e in self.ENG:
            op = Op()
            op.eng, op.fn, op.needs_inc, op.is_dma, op.chan, op.n = e, None, False, False, None, 0
            op.deps = []
            for d in lasts:
                if d.eng != e:
                    d.needs_inc = True
                    op.deps.append(d)
            for d in dmas:
                op.deps.append(d)
            self.ops[e].append(op)
        self.res = {}

    def emit(self):
        nc = self.nc
        st = self.stack
        esem = {e: st.enter_context(nc.semaphore("sem_" + e)) for e in self.ENG}
        for e in self.ENG:
            for op in self.ops[e]:
                if op.is_dma and op.chan not in self.chan_sems:
                    self.chan_sems[op.chan] = st.enter_context(nc.semaphore("ch_" + op.chan))
                    self.chan_cnt[op.chan] = 0
        for e in self.ENG:
            c = 0
            for op in self.ops[e]:
                if op.is_dma:
                    self.chan_cnt[op.chan] += 16 * op.n
                    op.sem = self.chan_sems[op.chan]
                    op.tok = self.chan_cnt[op.chan]
                else:
                    if op.needs_inc:
                        c += 1
                    op.sem = esem[e]
                    op.tok = c
        finals = [(s, self.chan_cnt[ch]) for ch, s in self.chan_sems.items()]
        block = st.enter_context(nc.Block())

        def body_for(e):
            def body(eng):
                known = {}
                for op in self.ops[e]:
                    need = {}
                    for d in op.deps:
                        k = id(d.sem)
                        if k not in need or need[k][1] < d.tok:
                            need[k] = (d.sem, d.tok)
                    for k, (s, v) in need.items():
                        if known.get(k, 0) >= v:
                            continue
                        eng.wait_ge(s, v)
                        known[k] = v
                    if op.fn is None:
                        continue
                    ins = op.fn(eng)
                    if op.is_dma:
                        if op.n == 1:
                            ins.then_inc(op.sem, 16)
                        else:
                            for i_ in ins:
                                i_.then_inc(op.sem, 16)
                    elif op.needs_inc:
                        ins.then_inc(op.sem, 1)
                if e == "sp":
                    for s, v in finals:
                        eng.wait_ge(s, v)
            return body

        block.tensor(body_for("pe"))
        block.scalar(body_for("act"))
        block.vector(body_for("dve"))
        block.gpsimd(body_for("pool"))
        block.sync(body_for("sp"))
        st.close()


class Arena:
    def __init__(self, P, kbytes):
        self.t = P.sb("arena", [128, kbytes * 256], F32)
        self.cap = kbytes * 1024
        self.off = 0

    def alloc(self, free_shape, dt):
        n = 1
        for s in free_shape:
            n *= s
        nbytes = n * mybir.dt.size(dt)
        nbytes = (nbytes + 63) // 64 * 64
        assert self.off + nbytes <= self.cap, ("arena overflow", self.off, nbytes, self.cap)
        a = self.t[:, self.off // 4:(self.off + nbytes) // 4]
        self.off += nbytes
        if dt != F32:
            a = a.bitcast(dt)
        a = a[:, 0:n]
        if len(free_shape) == 2:
            a = a.rearrange("p (a b) -> p a b", a=free_shape[0])
        elif len(free_shape) == 3:
            a = a.rearrange("p (a b c) -> p a b c", a=free_shape[0], b=free_shape[1])
        return a


def bcast_last(ap2d, n):
    return bass.AP(ap2d.tensor, ap2d.offset, [list(ap2d.ap[0]), list(ap2d.ap[1]), [0, n]])


def MM(P, out, lhsT, rhs, start, stop, r, w):
    P.op("pe", lambda e: e.matmul(out, lhsT=lhsT, rhs=rhs, start=start, stop=stop), reads=r, writes=w)


def TR(P, out, in_, ident, r, w):
    P.op("pe", lambda e: e.transpose(out, in_, ident), reads=r, writes=w)


def ACT(P, out, in_, func, r, w, scale=None, bias=None, accum=None):
    kw = {}
    if scale is not None:
        kw["scale"] = scale
    if bias is not None:
        kw["bias"] = bias
    if accum is not None:
        kw["accum_out"] = accum
    P.op("act", lambda e: e.activation(out=out, in_=in_, func=func, **kw), reads=r, writes=w)


def TS(P, eng, out, in0, s1, s2, op0, op1, r, w, accum=None):
    kw = {}
    if op1 is not None:
        kw["op1"] = op1
    if accum is not None:
        kw["accum_out"] = accum
    P.op(eng, lambda e: e.tensor_scalar(out=out, in0=in0, scalar1=s1, scalar2=s2, op0=op0, **kw), reads=r, writes=w)


def TT(P, eng, out, in0, in1, op, r, w):
    P.op(eng, lambda e: e.tensor_tensor(out=out, in0=in0, in1=in1, op=op), reads=r, writes=w)


def STT(P, out, in0, scalar, in1, op0, op1, r, w):
    P.op("dve", lambda e: e.scalar_tensor_tensor(out=out, in0=in0, scalar=scalar, in1=in1, op0=op0, op1=op1), reads=r, writes=w)


SKIP_RED = [False]


def RED(P, out, in_, op, r, w):
    if SKIP_RED[0]:
        return
    P.op("dve", lambda e: e.tensor_reduce(out=out, in_=in_, axis=AX.X, op=op), reads=r, writes=w)


def COPY(P, eng, out, in_, r, w):
    if eng == "act":
        P.op("act", lambda e: e.copy(out=out, in_=in_), reads=r, writes=w)
    else:
        P.op(eng, lambda e: e.tensor_copy(out=out, in_=in_), reads=r, writes=w)


SKIP_CH = set()


def DMA(P, eng, chan, out, in_, r, w):
    if chan in SKIP_CH:
        return
    P.dma(eng, chan, lambda e: e.dma_start(out=out, in_=in_), reads=r, writes=w)


def DMAN(P, eng, chan, pairs, r, w):
    pairs = list(pairs)
    P.dma(eng, chan, lambda e: [e.dma_start(out=o, in_=i) for (o, i) in pairs], reads=r, writes=w, n=len(pairs))


class Ctx:
    pass


def build(stages=("a0", "m0", "a1", "m1"), S=4096, debug=None):
    NT = S // 128
    NG = S // 512
    nc = bass.Bass("TRN2", target_bir_lowering=False)
    P = Prog(nc)
    C = Ctx()
    C.S, C.NT, C.NG = S, NT, NG

    def din(name, shape, dt=F32):
        return nc.dram_tensor(name, list(shape), dt, kind="ExternalInput").ap()

    def dscr(name, shape, dt):
        return nc.dram_tensor(name, list(shape), dt, kind="Internal").ap()

    dr = {}
    dr["x"] = din("x", [S, D])
    dr["y"] = nc.dram_tensor("y", [S, D], F32, kind="ExternalOutput").ap()
    dr["gb"] = din("gb", [5, 128, D])
    dr["gqkv"] = din("gqkv", [128, 512])
    dr["tb"] = din("tb", [128, H, 256])
    dr["b31"] = din("b31", [128, H])
    dr["wqkv"] = din("wqkv", [D, 3 * D])
    dr["wo0"] = din("wo0", [D, D])
    dr["win"] = din("win", [D, 584])
    dr["wuq"] = din("wuq", [256, D])
    dr["wqi"] = din("wqi", [256, 512])
    dr["wukT"] = din("wukT", [256, D])
    dr["wuv"] = din("wuv", [256, D])
    dr["wo1"] = din("wo1", [D, D])
    dr["wup0"] = din("wup0", [D, DFF])
    dr["wup1"] = din("wup1", [D, DFF])
    dr["wdn0"] = din("wdn0", [DFF, D])
    dr["wdn1"] = din("wdn1", [DFF, D])
    dr["qT"] = dscr("qT", [8, 128, S], BF16)
    dr["kT"] = dscr("kT", [8, 128, S], BF16)
    dr["va"] = dscr("va", [H, 128, NT, 128], BF16)
    dr["qiT"] = dscr("qiT", [4, 128, S], BF16)
    xs = [dr["x"]]
    for i, stg in enumerate(stages):
        if i == len(stages) - 1:
            xs.append(dr["y"])
        else:
            xs.append(dscr("xs%d" % i, [S, D], F32))

    C.pb = [P.ps("pb%d" % i, [128, 512], F32) for i in range(8)]
    A = Arena(P, 200)
    C.A = A
    C.ident = A.alloc([128], BF16)
    C.identB = A.alloc([128], BF16)
    C.io = A.alloc([128], F32)
    C.ssb = A.alloc([64], F32)
    C.ksum = A.alloc([8, 16], F32)
    P.op("pool", lambda e: e.iota(C.io, pattern=[[1, 128]], base=0, channel_multiplier=-1,
                                  allow_small_or_imprecise_dtypes=True), writes=["io"])
    TS(P, "dve", C.ident, C.io, 0.0, None, ALU.is_equal, None, ["io"], ["ident"])
    TS(P, "dve", C.identB, C.io, 0.0, BIG, ALU.is_equal, ALU.mult, ["io"], ["identB"])
    base_off = A.off
    P.barrier()

    for i, stg in enumerate(stages):
        A.off = base_off
        last = (i == len(stages) - 1)
        if stg == "a0":
            attn_phaseA(P, C, dr, xs[i], 0)
            P.barrier()
            A.off = base_off
            if debug is not None and debug.startswith("A"):
                dbg_copy(P, C, dr, xs[i], xs[i + 1])
            else:
                attn_phaseB(P, C, dr, xs[i], xs[i + 1], 0, debug)
        elif stg == "a1":
            C.kiTz = [A.alloc([S], BF16) for _ in range(2)]
            C.wabs = A.alloc([NT, 8], F32)
            C.wsgn = A.alloc([NT, 8], F32)
            base1 = A.off
            attn_phaseA(P, C, dr, xs[i], 1)
            P.barrier()
            A.off = base1
            attn_phaseB(P, C, dr, xs[i], xs[i + 1], 1)
        elif stg == "m0":
            mlp_phase(P, C, dr, xs[i], xs[i + 1], 0, False)
        elif stg == "m1":
            mlp_phase(P, C, dr, xs[i], xs[i + 1], 1, True)
        P.barrier()
    P.emit()
    return nc


def load_w_cast(P, dst3, src, nk, ncols, name, c0=0):
    pairs = []
    step = 2048
    for k in range(nk):
        for cc in range(0, ncols, step):
            w = min(step, ncols - cc)
            pairs.append((dst3[:, k, cc:cc + w], src[k * 128:(k + 1) * 128, c0 + cc:c0 + cc + w]))
    DMAN(P, "pool", "w_" + name, pairs, [], [name])


class Normer:
    def __init__(self, P, C, gb_dram_row, nbuf=3, tag="n"):
        A = C.A
        self.P, self.C = P, C
        self.gb = A.alloc([D], F32)
        DMA(P, "sp", "gbld", self.gb, gb_dram_row, [], ["gb"])
        self.nbuf = nbuf
        self.xt = [A.alloc([D], F32) for _ in range(nbuf)]
        self.xn = [A.alloc([D], BF16) for _ in range(2)]
        self.junk = A.alloc([D], BF16)
        self.cnt = 0

    def load(self, src_tile):
        i = self.cnt % self.nbuf
        DMA(self.P, "sp", "xt%d" % i, self.xt[i], src_tile, [], ["xt%d" % i])
        return i

    def norm_T(self, i, dstT, c0, dst_name, evac_eng="act"):
        P, C = self.P, self.C
        k = self.cnt
        self.cnt += 1
        xt = self.xt[i]
        xn = self.xn[k % 2]
        xnn = "xn%d" % (k % 2)
        ss = C.ssb[:, (k % 8) * 2:(k % 8) * 2 + 1]
        rs = C.ssb[:, (k % 8) * 2 + 1:(k % 8) * 2 + 2]
        sn = "ss%d" % (k % 8)
        ACT(P, self.junk, xt, AF.Square, ["xt%d" % i], ["junk", sn], accum=ss)
        with P.strict():
            ACT(P, rs, ss, AF.Ln, [sn], [sn + "r"], scale=1.0 / D, bias=EPS)
            ACT(P, rs, rs, AF.Exp, [sn + "r"], [sn + "r"], scale=-0.5)
        STT(P, xn, xt, rs, self.gb, ALU.mult, ALU.mult, ["xt%d" % i, sn + "r", "gb"], [xnn])
        pbi = k % 2
        pbv = C.pb[pbi][:].bitcast(BF16)
        for kc in range(8):
            TR(P, pbv[:, kc * 128:(kc + 1) * 128], xn[:, kc * 128:(kc + 1) * 128], C.ident, [xnn, "ident"], ["pb%d" % pbi])
        COPY(P, evac_eng, dstT[:, :, c0:c0 + 128], pbv.rearrange("p (a b) -> p a b", a=8), ["pb%d" % pbi], [dst_name])


def attn_phaseA(P, C, dr, xin, layer):
    A = C.A
    S, NT, NG = C.S, C.NT, C.NG
    nm = Normer(P, C, dr["gb"][2 * layer], nbuf=3)
    hnT = [A.alloc([8, 512], BF16) for _ in range(2)]
    qst = A.alloc([8, 512], BF16)
    kst = A.alloc([8, 512], BF16)
    vst = [A.alloc([8, 2, 128], BF16) for _ in range(2)]
    for b in range(2):
        P.op("pool", (lambda e, b=b: e.memset(vst[b], 1.0)), writes=["vst%d" % b])
    if layer == 0:
        wq = A.alloc([8, D], BF16)
        wk = A.alloc([8, D], BF16)
        wv = A.alloc([8, D], BF16)
        load_w_cast(P, wq, dr["wqkv"], 8, D, "wq", 0)
        load_w_cast(P, wk, dr["wqkv"], 8, D, "wk", D)
        load_w_cast(P, wv, dr["wqkv"], 8, D, "wv", 2 * D)
        P.op("dve", lambda e: e.memset(C.ksum, 0.0), writes=["ksum"])
    else:
        win = A.alloc([8, 584], BF16)
        wuq = A.alloc([2, D], BF16)
        wqi = A.alloc([2, 512], BF16)
        wuk = A.alloc([2, D], BF16)
        wuv = A.alloc([2, D], BF16)
        load_w_cast(P, win, dr["win"], 8, 584, "win")
        load_w_cast(P, wuq, dr["wuq"], 2, D, "wuq")
        load_w_cast(P, wqi, dr["wqi"], 2, 512, "wqi")
        load_w_cast(P, wuk, dr["wukT"], 2, D, "wuk")
        load_w_cast(P, wuv, dr["wuv"], 2, D, "wuv")
        gq = A.alloc([512], F32)
        DMA(P, "sp", "gqld", gq, dr["gqkv"], [], ["gq"])
        cn = [A.alloc([512], BF16) for _ in range(2)]
        ki2 = [A.alloc([2, 128], BF16) for _ in range(2)]
        for b in range(2):
            P.op("pool", (lambda e, b=b: e.memset(ki2[b], 0.0)), writes=["ki2%d" % b])
        cnT = [A.alloc([4, 512], BF16) for _ in range(2)]
        qist = A.alloc([4, 512], BF16)
        junk2 = A.alloc([256], BF16)
    rot = [2, 3, 4, 5, 6, 7]
    rc = [0]

    def nextbank():
        b = rot[rc[0] % len(rot)]
        rc[0] += 1
        return b

    xtiles = xin.rearrange("(t p) d -> t p d", p=128)
    for g in range(NG):
        hb = g % 2
        hT = hnT[hb]
        hname = "hnT%d" % hb
        for j in range(4):
            t = g * 4 + j
            i = nm.load(xtiles[t])
            nm.norm_T(i, hT, j * 128, hname, evac_eng="act" if layer == 0 else "dve")
        if layer == 0:
            for which, wmat, stg, sname in (("q", wq, qst, "qst"), ("k", wk, kst, "kst")):
                for hp in range(8):
                    b = nextbank()
                    for kc in range(8):
                        MM(P, C.pb[b][:, :], wmat[:, kc, hp * 128:(hp + 1) * 128], hT[:, kc, :], kc == 0, kc == 7,
                           ["w" + which, hname], ["pb%d" % b])
                    if which == "q":
                        ACT(P, stg[:, hp, :], C.pb[b][:, :], AF.Identity, ["pb%d" % b], [sname], scale=0.125)
                    else:
                        COPY(P, "act", stg[:, hp, :], C.pb[b][:, :], ["pb%d" % b], [sname])
                        RED(P, C.ksum[:, hp, 2 * g:2 * g + 2], C.pb[b][:, :].rearrange("p (a b) -> p a b", a=2), ALU.add,
                            ["pb%d" % b], ["ksum"])
                dst = dr["qT" if which == "q" else "kT"][:, :, g * 512:(g + 1) * 512].rearrange("h p t -> p h t")
                DMA(P, "sp", sname, dst, stg, [sname], [])
        else:
            for j in range(4):
                t = g * 4 + j
                tc = slice(j * 128, (j + 1) * 128)
                ba = nextbank()
                bb = nextbank()
                for kc in range(8):
                    MM(P, C.pb[ba][:, :], hT[:, kc, tc], win[:, kc, 0:512], kc == 0, kc == 7, [hname, "win"], ["pb%d" % ba])
                for kc in range(8):
                    MM(P, C.pb[bb][:, 0:72], hT[:, kc, tc], win[:, kc, 512:584], kc == 0, kc == 7, [hname, "win"], ["pb%d" % bb])
                cb = t % 2
                ssq = C.ssb[:, 32 + cb * 4:32 + cb * 4 + 1]
                ssk = C.ssb[:, 32 + cb * 4 + 1:32 + cb * 4 + 2]
                rsq = C.ssb[:, 32 + cb * 4 + 2:32 + cb * 4 + 3]
                rsk = C.ssb[:, 32 + cb * 4 + 3:32 + cb * 4 + 4]
                sn = "cs%d" % cb
                ACT(P, junk2, C.pb[ba][:, 0:256], AF.Square, ["pb%d" % ba], ["junk2", sn], accum=ssq)
                ACT(P, junk2, C.pb[ba][:, 256:512], AF.Square, ["pb%d" % ba], ["junk2", sn], accum=ssk)
                with P.strict():
                    ACT(P, rsq, ssq, AF.Ln, [sn], [sn + "q"], scale=1.0 / 256, bias=EPS)
                    ACT(P, rsq, rsq, AF.Exp, [sn + "q"], [sn + "q"], scale=-0.5)
                    ACT(P, rsk, ssk, AF.Ln, [sn], [sn + "k"], scale=1.0 / 256, bias=EPS)
                    ACT(P, rsk, rsk, AF.Exp, [sn + "k"], [sn + "k"], scale=-0.5)
                cname = "cn%d" % cb
                STT(P, cn[cb][:, 0:256], C.pb[ba][:, 0:256], rsq, gq[:, 0:256], ALU.mult, ALU.mult,
                    ["pb%d" % ba, sn + "q", "gq"], [cname])
                STT(P, cn[cb][:, 256:512], C.pb[ba][:, 256:512], rsk, gq[:, 256:512], ALU.mult, ALU.mult,
                    ["pb%d" % ba, sn + "k", "gq"], [cname])
                kname = "ki2%d" % cb
                COPY(P, "act", ki2[cb][:, 0, 0:64], C.pb[bb][:, 0:64], ["pb%d" % bb], [kname])
                COPY(P, "act", ki2[cb][:, 1, 64:128], C.pb[bb][:, 0:64], ["pb%d" % bb], [kname])
                TS(P, "dve", C.wabs[:, t, :], C.pb[bb][:, 64:72], (8.0 ** -0.5) * (64.0 ** -0.5), None, ALU.mult, None,
                   ["pb%d" % bb], ["wabs"])
                TS(P, "dve", C.wsgn[:, t, :], C.wabs[:, t, :], 0.0, 2.0, ALU.is_ge, ALU.mult, ["wabs"], ["wsgn"])
                TS(P, "dve", C.wsgn[:, t, :], C.wsgn[:, t, :], -1.0, None, ALU.add, None, ["wsgn"], ["wsgn"])
                STT(P, C.wabs[:, t, :], C.wabs[:, t, :], -1.0, C.wabs[:, t, :], ALU.mult, ALU.max, ["wabs"], ["wabs"])
                pbv = C.pb[cb][:].bitcast(BF16)
                for q4 in range(4):
                    TR(P, pbv[:, q4 * 128:(q4 + 1) * 128], cn[cb][:, q4 * 128:(q4 + 1) * 128], C.ident, [cname, "ident"], ["pb%d" % cb])
                TR(P, pbv[:, 512:640], ki2[cb][:, 0, :], C.ident, [kname, "ident"], ["pb%d" % cb])
                TR(P, pbv[:, 640:768], ki2[cb][:, 1, :], C.ident, [kname, "ident"], ["pb%d" % cb])
                COPY(P, "act", cnT[hb][:, :, tc], pbv[:, 0:512].rearrange("p (a b) -> p a b", a=4), ["pb%d" % cb], ["cnT%d" % hb])
                COPY(P, "act", C.kiTz[0][:, t * 128:(t + 1) * 128], pbv[:, 512:640], ["pb%d" % cb], ["kiT"])
                COPY(P, "act", C.kiTz[1][:, t * 128:(t + 1) * 128], pbv[:, 640:768], ["pb%d" % cb], ["kiT"])
            cT = cnT[hb]
            cTn = "cnT%d" % hb
            for hp in range(8):
                b = nextbank()
                for kc in range(2):
                    MM(P, C.pb[b][:, :], wuq[:, kc, hp * 128:(hp + 1) * 128], cT[:, kc, :], kc == 0, kc == 1, ["wuq", cTn], ["pb%d" % b])
                ACT(P, qst[:, hp, :], C.pb[b][:, :], AF.Identity, ["pb%d" % b], ["qst"], scale=0.125)
            DMA(P, "sp", "qst", dr["qT"][:, :, g * 512:(g + 1) * 512].rearrange("h p t -> p h t"), qst, ["qst"], [])
            for ch in range(4):
                b = nextbank()
                for kc in range(2):
                    MM(P, C.pb[b][:, :], wqi[:, kc, ch * 128:(ch + 1) * 128], cT[:, kc, :], kc == 0, kc == 1, ["wqi", cTn], ["pb%d" % b])
                COPY(P, "dve", qist[:, ch, :], C.pb[b][:, :], ["pb%d" % b], ["qist"])
            DMA(P, "sp", "qist", dr["qiT"][:, :, g * 512:(g + 1) * 512].rearrange("h p t -> p h t"), qist, ["qist"], [])
            for hp in range(8):
                b = nextbank()
                for kc in range(2):
                    MM(P, C.pb[b][:, :], wuk[:, kc, hp * 128:(hp + 1) * 128], cT[:, 2 + kc, :], kc == 0, kc == 1, ["wuk", cTn], ["pb%d" % b])
                COPY(P, "act", kst[:, hp, :], C.pb[b][:, :], ["pb%d" % b], ["kst"])
            DMA(P, "sp", "kst", dr["kT"][:, :, g * 512:(g + 1) * 512].rearrange("h p t -> p h t"), kst, ["kst"], [])
        for j in range(4):
            t = g * 4 + j
            tc = slice(j * 128, (j + 1) * 128)
            vb = t % 2
            vname = "vst%d" % vb
            for half in range(2):
                b = nextbank()
                if layer == 0:
                    for kc in range(8):
                        MM(P, C.pb[b][:, :], hT[:, kc, tc], wv[:, kc, half * 512:(half + 1) * 512], kc == 0, kc == 7, [hname, "wv"], ["pb%d" % b])
                else:
                    for kc in range(2):
                        MM(P, C.pb[b][:, :], cT[:, 2 + kc, tc], wuv[:, kc, half * 512:(half + 1) * 512], kc == 0, kc == 1, [cTn, "wuv"], ["pb%d" % b])
                psv = C.pb[b][:, :].rearrange("p (a b c) -> p a b c", a=4, b=2)
                COPY(P, "dve", vst[vb][:, half * 4:(half + 1) * 4, 0, 0:64], psv[:, :, 0, :], ["pb%d" % b], [vname])
                COPY(P, "dve", vst[vb][:, half * 4:(half + 1) * 4, 1, 64:128], psv[:, :, 1, :], ["pb%d" % b], [vname])
            dst = dr["va"][:, :, t, :].rearrange("(a b) p d -> p a b d", b=2)
            DMA(P, "sp", vname, dst, vst[vb], [vname], [])


def dbg_copy(P, C, dr, xin, xout):
    A = C.A
    xt = A.alloc([D], F32)
    xin_t = xin.rearrange("(t p) d -> t p d", p=128)
    xout_t = xout.rearrange("(t p) d -> t p d", p=128)
    for t in range(C.NT):
        DMA(P, "sp", "dbgl", xt, xin_t[t], [], ["dbgx"])
        DMA(P, "pool", "dbgs", xout_t[t], xt, ["dbgx"], [])


SALL = ["score%d" % i_ for i_ in range(8)]


def attn_phaseB(P, C, dr, xin, xout, layer, debug=None):
    A = C.A
    S, NT, NG = C.S, C.NT, C.NG
    wo = A.alloc([8, D], BF16)
    load_w_cast(P, wo, dr["wo0" if layer == 0 else "wo1"], 8, D, "wo")
    if layer == 1:
        score = A.alloc([max(S, H * 256)], F32)
        tbf = score[:, 0:H * 256].rearrange("p (a b) -> p a b", a=H)
        tbn = "score0"
    else:
        tbf = A.alloc([H, 256], F32)
        tbn = "tbf"
    b31 = A.alloc([H], F32)
    cm0 = A.alloc([128], F32)
    TBp = A.alloc([H, 256], BF16)
    DMA(P, "sp", "tbld", tbf, dr["tb"], [], SALL if layer == 1 else [tbn])
    DMA(P, "sp", "b31ld", b31, dr["b31"], [], ["b31"])
    TS(P, "dve", cm0, C.io, 0.0, -BIG, ALU.is_lt, ALU.mult, ["io"], ["cm0"])
    for h in range(H):
        STT(P, TBp[:, h, 0:128], tbf[:, h, 0:128], b31[:, h:h + 1], cm0, ALU.subtract, ALU.add, (SALL if layer == 1 else [tbn]) + ["b31", "cm0"], ["TBp"])
        TS(P, "dve", TBp[:, h, 128:256], tbf[:, h, 128:256], b31[:, h:h + 1], None, ALU.subtract, None, (SALL if layer == 1 else [tbn]) + ["b31"], ["TBp"])
    nqb = 2
    qz = [[A.alloc([8, 512], BF16) for _ in range(2)] for _ in range(nqb)]
    for b in range(2):
        P.op("pool", (lambda e, b=b: e.memset(qz[b][0][64:128], 0.0)), writes=["qg%d" % b])
        P.op("pool", (lambda e, b=b: e.memset(qz[b][1][0:64], 0.0)), writes=["qg%d" % b])
    kbuf = [A.alloc([S], BF16) for _ in range(2)]
    vbuf = [A.alloc([NT, 128], BF16) for _ in range(2)]
    pbuf = [A.alloc([512], BF16) for _ in range(3)]
    oT = A.alloc([8, 512], BF16)
    rec = A.alloc([512], F32)
    xt = [A.alloc([D], F32) for _ in range(2)]
    if layer == 0:
        kbd = A.alloc([8, 32], BF16)
        nidx = A.alloc([256], F32)
        pneg = A.alloc([256], F32)
        ownm1 = A.alloc([256], F32)
        gm = A.alloc([256], F32)
        gm2 = A.alloc([256], F32)
        tmpg = A.alloc([256], F32)
        mx = A.alloc([16], F32)
        selv = A.alloc([256], BF16)
        selT2 = [A.alloc([H, 512], BF16) for _ in range(2)]
        Esel = A.alloc([16, 128], BF16)
        iot = A.alloc([16, 128], F32)
        for b_ in range(2):
            P.op("pool", (lambda e, b_=b_: e.memset(selT2[b_], 0.0)), writes=["selT%d" % b_])
        P.op("dve", lambda e: e.memset(kbd, 0.0), writes=["kbd"])
        COPY(P, "dve", kbd[0:64, :, 0:16], C.ksum[0:64, :, :], ["ksum", "kbd"], ["kbd"])
        COPY(P, "dve", kbd[64:128, :, 16:32], C.ksum[64:128, :, :], ["ksum", "kbd"], ["kbd"])
        P.op("pool", lambda e: e.iota(nidx.rearrange("p (a b) -> p a b", a=16), pattern=[[0, 16], [1, 16]], base=0,
                                      channel_multiplier=0, allow_small_or_imprecise_dtypes=True), writes=["nidx"])
        P.op("pool", lambda e: e.iota(iot, pattern=[[1, 16], [0, 128]], base=0, channel_multiplier=-1,
                                      allow_small_or_imprecise_dtypes=True), writes=["iot"])
        TS(P, "dve", Esel, iot, 0.0, BIG, ALU.is_equal, ALU.mult, ["iot"], ["Esel"])
    else:
        qig = A.alloc([4, 512], BF16)
        madd = A.alloc([S], BF16)
        junkb = madd
        maskT = A.alloc([NT, 512], BF16)
        cneg = A.alloc([128], F32)
        cpos = A.alloc([128], F32)
        dtmp = A.alloc([128], F32)
        bs = A.alloc([16], F32)
        bsi = A.alloc([4], I32)
        pw = A.alloc([NBIS + 1], F32)
        sk = A.alloc([NBIS + 1], F32)
        for k_ in range(NBIS + 1):
            P.op("dve", (lambda e, k_=k_: e.memset(pw[:, k_:k_ + 1], 2.0 ** -(k_ + 1))), writes=["pw"])
        TS(P, "dve", cneg, C.io, 0.0, NEGF, ALU.is_gt, ALU.mult, ["io"], ["cneg"])
        TS(P, "dve", cpos, C.io, 0.0, -NEGF, ALU.is_gt, ALU.mult, ["io"], ["cpos"])
    xin_t = xin.rearrange("(t p) d -> t p d", p=128)
    xout_t = xout.rearrange("(t p) d -> t p d", p=128)
    lrot = [2, 3, 4]
    lc = [0]
    ucnt = [0]
    def load_q(gq):
        qb_ = gq % nqb
        DMAN(P, "sp", "qg%d" % qb_, [(qz[qb_][0][0:64], dr["qT"][:, 0:64, gq * 512:(gq + 1) * 512].rearrange("h p t -> p h t")),
                                    (qz[qb_][1][64:128], dr["qT"][:, 64:128, gq * 512:(gq + 1) * 512].rearrange("h p t -> p h t"))],
             [], ["qg%d" % qb_])

    def gate_a(gq, j):
        t = gq * 4 + j
        blk = t // 2
        QG = qz[gq % nqb]
        qn_ = "qg%d" % (gq % nqb)
        if t % 2 == 0:
            TS(P, "dve", pneg, nidx, float(blk), NEGF, ALU.is_ge, ALU.mult, ["nidx"], ["pneg"])
            TS(P, "dve", ownm1, nidx, float(blk), -1.0, ALU.is_ge, ALU.add, ["nidx"], ["ownm1"])
        for hp in range(8):
            for h2 in range(2):
                MM(P, C.pb[0][:, hp * 32:(hp + 1) * 32], QG[h2][:, hp, j * 128:(j + 1) * 128], kbd[:, hp, :], h2 == 0, h2 == 1,
                   [qn_, "kbd"], ["pb0"])
        TT(P, "dve", gm, C.pb[0][:, 0:256], pneg, ALU.add, ["pb0", "pneg"], ["gm"])
        g3 = gm.rearrange("p (a b) -> p a b", a=16)
        g23 = gm2.rearrange("p (a b) -> p a b", a=16)
        t3 = tmpg.rearrange("p (a b) -> p a b", a=16)
        mb = bcast_last(mx, 16)
        RED(P, mx, g3, ALU.max, ["gm"], ["mx"])
        TT(P, "dve", t3, g3, mb, ALU.is_ge, ["gm", "mx"], ["tmpg"])
        STT(P, gm2, tmpg, NEGF, gm, ALU.mult, ALU.add, ["tmpg", "gm"], ["gm2"])
        RED(P, mx, g23, ALU.max, ["gm2"], ["mx"])
        TT(P, "dve", t3, g23, mb, ALU.is_ge, ["gm2", "mx"], ["tmpg"])
        STT(P, gm2, tmpg, NEGF, gm2, ALU.mult, ALU.add, ["tmpg", "gm2"], ["gm2"])
        RED(P, mx, g23, ALU.max, ["gm2"], ["mx"])
        TT(P, "dve", t3, g3, mb, ALU.is_ge, ["gm", "mx"], ["tmpg"])
        STT(P, selv, tmpg, -1.0, ownm1, ALU.add, ALU.max, ["tmpg", "ownm1"], ["selv"])

    def gate_b(gq, j):
        sT = selT2[gq % 2]
        pv1 = C.pb[1][:].bitcast(BF16)
        for rnd in range(2):
            for hh in range(8):
                h = rnd * 8 + hh
                TR(P, pv1[0:16, hh * 128:(hh + 1) * 128], selv[:, h * 16:(h + 1) * 16], C.ident, ["selv", "ident"], ["pb1"])
            COPY(P, "act", sT[0:16, rnd * 8:(rnd + 1) * 8, j * 128:(j + 1) * 128],
                 pv1[0:16, :].rearrange("p (a b) -> p a b", a=8), ["pb1"], ["selT%d" % (gq % 2)])

    load_q(0)
    if layer == 0:
        for j in range(4):
            gate_a(0, j)
            gate_b(0, j)
    for g in range(NG):
        qb = g % nqb
        qname = "qg%d" % qb
        QZ = qz[qb]
        nch = 4 * (g + 1)
        if g + 1 < NG:
            load_q(g + 1)
        if layer == 0:
            selT = selT2[g % 2]
            sTn = "selT%d" % (g % 2)
        else:
            DMA(P, "sp", "qig", qig, dr["qiT"][:, :, g * 512:(g + 1) * 512].rearrange("h p t -> p h t"), [], ["qig"])
            for j in range(4):
                t = g * 4 + j
                nk = 128 * (t + 1)
                qc = slice(j * 128, (j + 1) * 128)
                nsg = (nk + 511) // 512
                for hi in range(8):
                    ch = hi // 2
                    for sg in range(nsg):
                        ncol = min(512, nk - sg * 512)
                        sc = slice(sg * 512, sg * 512 + ncol)
                        bm = (0, 1, 2)[ucnt[0] % 3]
                        br = (3, 4, 6, 7)[ucnt[0] % 4]
                        ucnt[0] += 1
                        sn_ = "score%d" % sg
                        MM(P, C.pb[bm][:, 0:ncol], qig[:, ch, qc], C.kiTz[hi % 2][:, sc], True, True, ["qig", "kiT"], ["pb%d" % bm])
                        ACT(P, C.pb[br][:, 0:ncol], C.pb[bm][:, 0:ncol], AF.Relu, ["pb%d" % bm, "wabs"], ["pb%d" % br],
                            scale=C.wabs[:, t, hi:hi + 1])
                        if hi == 0:
                            TS(P, "dve", score[:, sc], C.pb[br][:, 0:ncol], C.wsgn[:, t, 0:1], None, ALU.mult, None,
                               ["pb%d" % br, "wsgn"], [sn_])
                        else:
                            STT(P, score[:, sc], C.pb[br][:, 0:ncol], C.wsgn[:, t, hi:hi + 1], score[:, sc], ALU.mult, ALU.add,
                                ["pb%d" % br, "wsgn", sn_], [sn_])
                dg = slice(128 * t, 128 * t + 128)
                lo, hi_, rg, mid, cnt, m1 = (bs[:, k:k + 1] for k in range(6))
                fl = bsi[:, 0:1]
                P.strict_default = True
                TT(P, "dve", dtmp, score[:, dg], cpos, ALU.add, SALL + ["cpos"], ["dtmp"])
                RED(P, lo, dtmp, ALU.min, ["dtmp"], ["bs"])
                if t > 0:
                    RED(P, m1, score[:, 0:128 * t], ALU.min, SALL, ["bs"])
                    TT(P, "dve", lo, lo, m1, ALU.min, ["bs"], ["bs"])
                TT(P, "dve", score[:, dg], score[:, dg], cneg, ALU.add, SALL + ["cneg"], SALL)
                if nk > 256:
                    negS, tq = bs[:, 6:7], bs[:, 7:8]
                    RED(P, hi_, score[:, 0:nk], ALU.max, SALL, ["bs"])
                    TT(P, "dve", rg, hi_, lo, ALU.subtract, ["bs"], ["bs"])
                    TS(P, "dve", sk, pw, rg, None, ALU.mult, None, ["pw", "bs"], ["sk"])
                    TT(P, "dve", mid, lo, sk[:, 0:1], ALU.add, ["bs", "sk"], ["mid"])
                    split = nk >= 1536
                    n1 = (int(nk * 0.42) // 128) * 128 if split else nk
                    thr_c = 256.0 - (nk - n1) / 2.0 - (0.25 if split else 0.0)
                    for it in range(NBIS):
                        if split:
                            ACT(P, madd[:, n1:nk], score[:, n1:nk], AF.Sign, SALL + ["mid"], ["maddA", "negS"],
                                scale=-1.0, bias=mid, accum=negS)
                        TS(P, "dve", madd[:, 0:n1], score[:, 0:n1], mid, None, ALU.is_ge, ALU.add, SALL + ["mid"], ["maddD", "cnt"], accum=cnt)
                        if split:
                            TS(P, "dve", cnt, negS, -0.5, cnt, ALU.mult, ALU.add, ["negS", "cnt"], ["cnt"])
                        STT(P, tq, cnt, thr_c, sk[:, it:it + 1], ALU.is_ge, ALU.mult, ["cnt", "sk"], ["tq"])
                        STT(P, mid, tq, sk[:, it + 1:it + 2], mid, ALU.subtract, ALU.add, ["tq", "sk", "mid"], ["mid"])
                    TT(P, "dve", lo, mid, sk[:, NBIS:NBIS + 1], ALU.subtract, ["mid", "sk"], ["bs"])
                TS(P, "dve", madd[:, 0:nk], score[:, 0:nk], lo, 1.0, ALU.is_ge, ALU.subtract, SALL + ["bs"], ["madd", "maddA", "maddD"])
                P.strict_default = False
                pv7 = C.pb[5][:].bitcast(BF16)
                for c0 in range(0, t + 1, 8):
                    cn_ = min(8, t + 1 - c0)
                    for cc in range(cn_):
                        c = c0 + cc
                        TR(P, pv7[:, cc * 128:(cc + 1) * 128], madd[:, c * 128:(c + 1) * 128], C.ident, ["madd", "maddA", "maddD", "ident"], ["pb5"])
                    COPY(P, "act", maskT[:, c0:c0 + cn_, qc], pv7[:, 0:cn_ * 128].rearrange("p (a b) -> p a b", a=cn_), ["pb5"], ["maskT"])
        items = [(h, c) for h in range(H) for c in range(nch)]
        LOOK = 3
        lbanks = [2, 3, 4, 7] if layer == 0 else [0, 1, 2, 5]

        def hinfo(h):
            hp, h2 = h // 2, h % 2
            r0 = 64 * h2
            return hp, h2, slice(r0, r0 + 64), slice(64 - r0, 128 - r0), hp % 2, h % 2

        def issue_qk(idx):
            h, c = items[idx]
            hp, h2, rs_, so, kb, vb = hinfo(h)
            kname = "kbuf%d" % kb
            if c == 0:
                if h2 == 0:
                    DMA(P, "sp", kname, kbuf[kb][:, 0:nch * 128], dr["kT"][hp, :, 0:nch * 128], [], [kname])
                DMA(P, "sp", "vbuf%d" % vb, vbuf[vb][:, 0:nch, :], dr["va"][h, :, 0:nch, :], [], ["vbuf%d" % vb])
            dc = c - 4 * g
            col0 = max(0, dc * 128)
            cs = slice(col0, 512)
            lb = lbanks[idx % 4]
            lbn = "pb%d" % lb
            psl = C.pb[lb]
            n_blk = c // 2
            need_sel = (layer == 1) or (n_blk < 2 * g + 1)
            need_tb = dc >= -1
            MM(P, psl[:, cs], kbuf[kb][:, c * 128:(c + 1) * 128], QZ[h2][:, hp, cs], True, not (need_sel or need_tb), [kname, qname], [lbn])
            if need_sel:
                if layer == 0:
                    MM(P, psl[:, cs], Esel[:, n_blk, :], selT[:, h, cs], False, not need_tb, ["Esel", sTn], [lbn])
                else:
                    MM(P, psl[:, cs], C.identB, maskT[:, c, cs], False, not need_tb, ["identB", "maskT"], [lbn])
            if need_tb:
                if dc == -1:
                    MM(P, psl[:, 0:128], C.ident, TBp[:, h, 128:256], False, True, ["ident", "TBp"], [lbn])
                elif dc == 3:
                    MM(P, psl[:, 384:512], C.ident, TBp[:, h, 0:128], False, True, ["ident", "TBp"], [lbn])
                else:
                    MM(P, psl[:, col0:col0 + 256], C.ident, TBp[:, h, 0:256], False, True, ["ident", "TBp"], [lbn])

        for idx in range(min(LOOK, len(items))):
            issue_qk(idx)
        for idx in range(len(items)):
            if idx + LOOK < len(items):
                issue_qk(idx + LOOK)
            h, c = items[idx]
            if layer == 0 and g + 1 < NG and c == 0:
                if h % 4 == 0:
                    gate_a(g + 1, h // 4)
                elif h % 4 == 2:
                    gate_b(g + 1, h // 4)
            hp, h2, rs_, so, kb, vb = hinfo(h)
            ob = 5 + (h % 2) if layer == 0 else 3 + (h % 2)
            obn = "pb%d" % ob
            pso = C.pb[ob]
            dc = c - 4 * g
            col0 = max(0, dc * 128)
            cs = slice(col0, 512)
            lb = lbanks[idx % 4]
            lbn = "pb%d" % lb
            pi = idx % 3
            pn = "pbuf%d" % pi
            ACT(P, pbuf[pi][:, cs], C.pb[lb][:, cs], AF.Exp, [lbn], [pn])
            MM(P, pso[:, cs], vbuf[vb][:, c, :], pbuf[pi][:, cs], c == 0, c == nch - 1, ["vbuf%d" % vb, pn], [obn])
            if c == nch - 1:
                ACT(P, rec[so, :], pso[so, :], AF.Ln, [obn], ["rec"])
                ACT(P, rec[so, :], rec[so, :], AF.Exp, ["rec"], ["rec"], scale=-1.0)
                TT(P, "dve", oT[rs_, hp, :], pso[rs_, :], rec[so, :], ALU.mult, [obn, "rec"], ["oT"])
        for j in range(4):
            t = g * 4 + j
            xb = t % 2
            xn_ = "xta%d" % xb
            DMA(P, "sp", xn_, xt[xb], xin_t[t], [], [xn_])
            for half in range(2):
                b = 6 + half if layer == 1 else (0, 1)[half]
                bn = "pb%d" % b
                for hp in range(8):
                    MM(P, C.pb[b][:, :], oT[:, hp, j * 128:(j + 1) * 128], wo[:, hp, half * 512:(half + 1) * 512], hp == 0, hp == 7, ["oT", "wo"], [bn])
                hs = slice(half * 512, (half + 1) * 512)
                TT(P, "dve", xt[xb][:, hs], C.pb[b][:, :], xt[xb][:, hs], ALU.add, [bn, xn_], [xn_])
            DMA(P, "pool", "xst%d" % xb, xout_t[t], xt[xb], [xn_], [])


def mlp_phase(P, C, dr, xin, xout, layer, final):
    A = C.A
    S, NT = C.S, C.NT
    wup = A.alloc([8, DFF], BF16)
    wdn = A.alloc([32, D], BF16)
    load_w_cast(P, wup, dr["wup%d" % layer], 8, DFF, "wup")
    load_w_cast(P, wdn, dr["wdn%d" % layer], 32, D, "wdn")
    nm = Normer(P, C, dr["gb"][2 * layer + 1], nbuf=4)
    if final:
        fnb = A.alloc([D], F32)
        DMA(P, "sp", "fnld", fnb, dr["gb"][4], [], ["fnb"])
        junkf = A.alloc([D], BF16)
    hnT = [A.alloc([8, 256], BF16) for _ in range(2)]
    hTt = A.alloc([32, 256], BF16)
    rbuf = [A.alloc([512], BF16) for _ in range(3)]
    xin_t = xin.rearrange("(t p) d -> t p d", p=128)
    xout_t = xout.rearrange("(t p) d -> t p d", p=128)
    rcnt = [0]
    for g2 in range(NT // 2):
        hb = g2 % 2
        hname = "mhT%d" % hb
        xi = []
        for j in range(2):
            t = g2 * 2 + j
            i = nm.load(xin_t[t])
            xi.append(i)
            nm.norm_T(i, hnT[hb], j * 128, hname, evac_eng="dve")
        for f2 in range(16):
            b = 2 + (f2 % 2)
            bn = "pb%d" % b
            for q in range(2):
                ffc = f2 * 2 + q
                for kc in range(8):
                    MM(P, C.pb[b][:, q * 256:(q + 1) * 256], wup[:, kc, ffc * 128:(ffc + 1) * 128], hnT[hb][:, kc, :], kc == 0, kc == 7,
                       ["wup", hname], [bn])
            ri = rcnt[0] % 3
            rcnt[0] += 1
            rn = "rbuf%d" % ri
            ACT(P, rbuf[ri], C.pb[b][:, :], AF.Relu, [bn], [rn])
            TT(P, "dve", hTt[:, f2 * 2:f2 * 2 + 2, :], C.pb[b][:, :].rearrange("p (a b) -> p a b", a=2),
               rbuf[ri].rearrange("p (a b) -> p a b", a=2), ALU.mult, [bn, rn], ["hTt"])
        for j in range(2):
            t = g2 * 2 + j
            i = xi[j]
            xn_ = "xt%d" % i
            for half in range(2):
                b = 4 + j * 2 + half
                bn = "pb%d" % b
                for ffc in range(32):
                    MM(P, C.pb[b][:, :], hTt[:, ffc, j * 128:(j + 1) * 128], wdn[:, ffc, half * 512:(half + 1) * 512], ffc == 0, ffc == 31,
                       ["hTt", "wdn"], [bn])
                hs = slice(half * 512, (half + 1) * 512)
                TT(P, "dve", nm.xt[i][:, hs], C.pb[b][:, :], nm.xt[i][:, hs], ALU.add, [bn, xn_], [xn_])
            if final:
                ss = C.ssb[:, 48 + (t % 4) * 2:48 + (t % 4) * 2 + 1]
                rs = C.ssb[:, 48 + (t % 4) * 2 + 1:48 + (t % 4) * 2 + 2]
                sn = "fs%d" % (t % 4)
                ACT(P, junkf, nm.xt[i], AF.Square, [xn_], ["junkf", sn], accum=ss)
                with P.strict():
                    ACT(P, rs, ss, AF.Ln, [sn], [sn + "r"], scale=1.0 / D, bias=EPS)
                    ACT(P, rs, rs, AF.Exp, [sn + "r"], [sn + "r"], scale=-0.5)
                STT(P, nm.xt[i], nm.xt[i], rs, fnb, ALU.mult, ALU.mult, [xn_, sn + "r", "fnb"], [xn_])
            DMA(P, "pool", "xst%d" % i, xout_t[t], nm.xt[i], [xn_], [])


def _rel_bucket_np(dist):
    n = np.maximum(dist, 0)
    exact = 16
    nf = np.maximum(n, 1).astype(np.float32)
    large = exact + (np.log(nf / np.float32(exact)) / np.float32(math.log(128 / exact)) * np.float32(32 - exact)).astype(np.int32)
    large = np.minimum(large, 31)
    return np.where(n < exact, n, large)


def host_layout(inp):
    f = lambda a: np.ascontiguousarray(np.asarray(a, dtype=np.float32))
    rb = f(inp["rel_bias"])
    s_idx = np.arange(128)[:, None]
    q_idx = np.arange(128)[None, :]
    bk0 = _rel_bucket_np(q_idx - s_idx)
    bk1 = _rel_bucket_np(q_idx - s_idx + 128)
    tb = np.empty((128, H, 256), np.float32)
    tb[:, :, 0:128] = rb[bk0].transpose(0, 2, 1)
    tb[:, :, 128:256] = rb[bk1].transpose(0, 2, 1)
    gb = np.stack([inp["ln_attn"][0], inp["ln_mlp"][0], inp["ln_attn"][1], inp["ln_mlp"][1], inp["final_norm"]])
    gb = f(np.broadcast_to(gb[:, None, :], (5, 128, D)))
    gqkv = f(np.broadcast_to(np.concatenate([inp["dsa_g_q"][0], inp["dsa_g_kv"][0]])[None, :], (128, 512)))
    m = {
        "gb": gb, "gqkv": gqkv, "tb": f(tb), "b31": f(np.broadcast_to(rb[31][None, :], (128, H))),
        "wqkv": f(inp["moba_w_qkv"][0]), "wo0": f(inp["moba_w_o"][0]), "win": f(inp["dsa_w_in"][0]),
        "wuq": f(inp["dsa_w_uq"][0]), "wqi": f(inp["dsa_w_qi"][0]),
        "wukT": f(np.transpose(inp["dsa_w_uk"][0], (2, 0, 1)).reshape(256, D)),
        "wuv": f(np.transpose(inp["dsa_w_uv"][0], (1, 0, 2)).reshape(256, D)),
        "wo1": f(inp["dsa_w_o"][0]),
        "wup0": f(inp["mlp_w_up"][0]), "wup1": f(inp["mlp_w_up"][1]),
        "wdn0": f(inp["mlp_w_down"][0]), "wdn1": f(inp["mlp_w_down"][1]),
    }
    return m


_NC_CACHE = {}


def kernel(**inputs):
    x = np.asarray(inputs["x"], dtype=np.float32)
    B = x.shape[0]
    shared = host_layout(inputs)
    key = ("full", x.shape[1])
    if key not in _NC_CACHE:
        _NC_CACHE[key] = build(("a0", "m0", "a1", "m1"), S=x.shape[1])
    nc = _NC_CACHE[key]
    in_maps = []
    for b in range(B):
        m = dict(shared)
        m["x"] = np.ascontiguousarray(x[b])
        in_maps.append(m)
    res = run_bass_kernel_spmd(nc, in_maps, core_ids=list(range(B)))
    return np.stack([np.asarray(r["y"], dtype=np.float32) for r in res.results], axis=0)
```

```python
import math
import numpy as np
import concourse.bass as bass
import concourse.mybir as mybir
from concourse.bass_utils import run_bass_kernel_spmd
from contextlib import ExitStack

F32 = mybir.dt.float32
BF16 = mybir.dt.bfloat16
I32 = mybir.dt.int32
AF = mybir.ActivationFunctionType
ALU = mybir.AluOpType
AX = mybir.AxisListType

D = 1024
H = 16
DFF = 4096
BIG = 30000.0
NEGF = -1.0e30
EPS = 1e-6
NBIS = 16


class Op:
    __slots__ = ("eng", "fn", "deps", "needs_inc", "tok", "sem", "is_dma", "chan", "n")


class Prog:
    ENG = ("pe", "act", "dve", "pool", "sp")

    def __init__(self, nc):
        self.nc = nc
        self.ops = {e: [] for e in self.ENG}
        self.res = {}
        self.stack = ExitStack()
        self.chan_sems = {}
        self.chan_cnt = {}
        self.last = {e: None for e in self.ENG}
        self.last_dma = {}
        self.strict_default = False

    def sb(self, name, shape, dt):
        return self.stack.enter_context(self.nc.sbuf_tensor(name, list(shape), dt))

    def ps(self, name, shape, dt):
        return self.stack.enter_context(self.nc.psum_tensor(name, list(shape), dt))

    def _add(self, eng, fn, reads, writes, chan=None, n=1, strict=False):
        op = Op()
        op.eng, op.fn, op.needs_inc, op.is_dma, op.chan, op.n = eng, fn, False, chan is not None, chan, n
        deps = set()
        raw = set()
        for r in reads:
            st = self.res.get(r)
            if st is not None and st[0] is not None:
                deps.add(st[0])
                raw.add(st[0])
            if st is not None and r.startswith("pb"):
                deps.update(st[1])
        for w in writes:
            st = self.res.get(w)
            if st is not None:
                if st[0] is not None:
                    deps.add(st[0])
                deps.update(st[1])
        keep = []
        for d in deps:
            if d is op or d.fn is None:
                continue
            if (not d.is_dma) and d.eng == eng and eng == "pe":
                continue
            d.needs_inc = True
            keep.append(d)
        op.deps = keep
        for r in reads:
            lst = self.res.setdefault(r, [None, []])[1]
            if not op.is_dma:
                lst[:] = [o for o in lst if o.is_dma or o.eng != eng]
            lst.append(op)
        for w in writes:
            self.res[w] = [op, []]
        self.ops[eng].append(op)
        if op.is_dma:
            self.last_dma[chan] = op
        else:
            self.last[eng] = op
        return op

    def op(self, eng, fn, reads=(), writes=(), strict=None):
        if strict is None:
            strict = self.strict_default
        return self._add(eng, fn, reads, writes, strict=strict)

    def dma(self, eng, chan, fn, reads=(), writes=(), n=1):
        return self._add(eng, fn, reads, writes, chan=chan, n=n)

    def strict(self):
        P = self

        class _S:
            def __enter__(self_):
                self_.old = P.strict_default
                P.strict_default = True

            def __exit__(self_, *a):
                P.strict_default = self_.old
        return _S()

    def barrier(self):
        lasts = [o for o in self.last.values() if o is not None]
        dmas = list(self.last_dma.values())
        for e in self.ENG:
            op = Op()
            op.eng, op.fn, op.needs_inc, op.is_dma, op.chan, op.n = e, None, False, False, None, 0
            op.deps = []
            for d in lasts:
                if d.eng != e:
                    d.needs_inc = True
                    op.deps.append(d)
            for d in dmas:
                op.deps.append(d)
            self.ops[e].append(op)
        self.res = {}

    def emit(self):
        nc = self.nc
        st = self.stack
        esem = {e: st.enter_context(nc.semaphore("sem_" + e)) for e in self.ENG}
        for e in self.ENG:
            for op in self.ops[e]:
                if op.is_dma and op.chan not in self.chan_sems:
                    self.chan_sems[op.chan] = st.enter_context(nc.semaphore("ch_" + op.chan))
                    self.chan_cnt[op.chan] = 0
        for e in self.ENG:
            c = 0
            for op in self.ops[e]:
                if op.is_dma:
                    self.chan_cnt[op.chan] += 16 * op.n
                    op.sem = self.chan_sems[op.chan]
                    op.tok = self.chan_cnt[op.chan]
                else:
                    if op.needs_inc:
                        c += 1
                    op.sem = esem[e]
                    op.tok = c
        finals = [(s, self.chan_cnt[ch]) for ch, s in self.chan_sems.items()]
        block = st.enter_context(nc.Block())

        def body_for(e):
            def body(eng):
                known = {}
                for op in self.ops[e]:
                    need = {}
                    for d in op.deps:
                        k = id(d.sem)
                        if k not in need or need[k][1] < d.tok:
                            need[k] = (d.sem, d.tok)
                    for k, (s, v) in need.items():
                        if known.get(k, 0) >= v:
                            continue
                        eng.wait_ge(s, v)
                        known[k] = v
                    if op.fn is None:
                        continue
                    ins = op.fn(eng)
                    if op.is_dma:
                        if op.n == 1:
                            ins.then_inc(op.sem, 16)
                        else:
                            for i_ in ins:
                                i_.then_inc(op.sem, 16)
                    elif op.needs_inc:
                        ins.then_inc(op.sem, 1)
                if e == "sp":
                    for s, v in finals:
                        eng.wait_ge(s, v)
            return body

        block.tensor(body_for("pe"))
        block.scalar(body_for("act"))
        block.vector(body_for("dve"))
        block.gpsimd(body_for("pool"))
        block.sync(body_for("sp"))
        st.close()


class Arena:
    def __init__(self, P, kbytes):
        self.t = P.sb("arena", [128, kbytes * 256], F32)
        self.cap = kbytes * 1024
        self.off = 0

    def alloc(self, free_shape, dt):
        n = 1
        for s in free_shape:
            n *= s
        nbytes = n * mybir.dt.size(dt)
        nbytes = (nbytes + 63) // 64 * 64
        assert self.off + nbytes <= self.cap, ("arena overflow", self.off, nbytes, self.cap)
        a = self.t[:, self.off // 4:(self.off + nbytes) // 4]
        self.off += nbytes
        if dt != F32:
            a = a.bitcast(dt)
        a = a[:, 0:n]
        if len(free_shape) == 2:
            a = a.rearrange("p (a b) -> p a b", a=free_shape[0])
        elif len(free_shape) == 3:
            a = a.rearrange("p (a b c) -> p a b c", a=free_shape[0], b=free_shape[1])
        return a


def bcast_last(ap2d, n):
    return bass.AP(ap2d.tensor, ap2d.offset, [list(ap2d.ap[0]), list(ap2d.ap[1]), [0, n]])


def MM(P, out, lhsT, rhs, start, stop, r, w):
    P.op("pe", lambda e: e.matmul(out, lhsT=lhsT, rhs=rhs, start=start, stop=stop), reads=r, writes=w)


def TR(P, out, in_, ident, r, w):
    P.op("pe", lambda e: e.transpose(out, in_, ident), reads=r, writes=w)


def ACT(P, out, in_, func, r, w, scale=None, bias=None, accum=None):
    kw = {}
    if scale is not None:
        kw["scale"] = scale
    if bias is not None:
        kw["bias"] = bias
    if accum is not None:
        kw["accum_out"] = accum
    P.op("act", lambda e: e.activation(out=out, in_=in_, func=func, **kw), reads=r, writes=w)


def TS(P, eng, out, in0, s1, s2, op0, op1, r, w, accum=None):
    kw = {}
    if op1 is not None:
        kw["op1"] = op1
    if accum is not None:
        kw["accum_out"] = accum
    P.op(eng, lambda e: e.tensor_scalar(out=out, in0=in0, scalar1=s1, scalar2=s2, op0=op0, **kw), reads=r, writes=w)


def TT(P, eng, out, in0, in1, op, r, w):
    P.op(eng, lambda e: e.tensor_tensor(out=out, in0=in0, in1=in1, op=op), reads=r, writes=w)


def STT(P, out, in0, scalar, in1, op0, op1, r, w):
    P.op("dve", lambda e: e.scalar_tensor_tensor(out=out, in0=in0, scalar=scalar, in1=in1, op0=op0, op1=op1), reads=r, writes=w)


SKIP_RED = [False]


def RED(P, out, in_, op, r, w):
    if SKIP_RED[0]:
        return
    P.op("dve", lambda e: e.tensor_reduce(out=out, in_=in_, axis=AX.X, op=op), reads=r, writes=w)


def COPY(P, eng, out, in_, r, w):
    if eng == "act":
        P.op("act", lambda e: e.copy(out=out, in_=in_), reads=r, writes=w)
    else:
        P.op(eng, lambda e: e.tensor_copy(out=out, in_=in_), reads=r, writes=w)


SKIP_CH = set()


def DMA(P, eng, chan, out, in_, r, w):
    if chan in SKIP_CH:
        return
    P.dma(eng, chan, lambda e: e.dma_start(out=out, in_=in_), reads=r, writes=w)


def DMAN(P, eng, chan, pairs, r, w):
    pairs = list(pairs)
    P.dma(eng, chan, lambda e: [e.dma_start(out=o, in_=i) for (o, i) in pairs], reads=r, writes=w, n=len(pairs))


class Ctx:
    pass


def build(stages=("a0", "m0", "a1", "m1"), S=4096, debug=None):
    NT = S // 128
    NG = S // 512
    nc = bass.Bass("TRN2", target_bir_lowering=False)
    P = Prog(nc)
    C = Ctx()
    C.S, C.NT, C.NG = S, NT, NG

    def din(name, shape, dt=F32):
        return nc.dram_tensor(name, list(shape), dt, kind="ExternalInput").ap()

    def dscr(name, shape, dt):
        return nc.dram_tensor(name, list(shape), dt, kind="Internal").ap()

    dr = {}
    dr["x"] = din("x", [S, D])
    dr["y"] = nc.dram_tensor("y", [S, D], F32, kind="ExternalOutput").ap()
    dr["gb"] = din("gb", [5, 128, D])
    dr["gqkv"] = din("gqkv", [128, 512])
    dr["tb"] = din("tb", [128, H, 256])
    dr["b31"] = din("b31", [128, H])
    dr["wqkv"] = din("wqkv", [D, 3 * D])
    dr["wo0"] = din("wo0", [D, D])
    dr["win"] = din("win", [D, 584])
    dr["wuq"] = din("wuq", [256, D])
    dr["wqi"] = din("wqi", [256, 512])
    dr["wukT"] = din("wukT", [256, D])
    dr["wuv"] = din("wuv", [256, D])
    dr["wo1"] = din("wo1", [D, D])
    dr["wup0"] = din("wup0", [D, DFF])
    dr["wup1"] = din("wup1", [D, DFF])
    dr["wdn0"] = din("wdn0", [DFF, D])
    dr["wdn1"] = din("wdn1", [DFF, D])
    dr["qT"] = dscr("qT", [8, 128, S], BF16)
    dr["kT"] = dscr("kT", [8, 128, S], BF16)
    dr["va"] = dscr("va", [H, 128, NT, 128], BF16)
    dr["qiT"] = dscr("qiT", [4, 128, S], BF16)
    xs = [dr["x"]]
    for i, stg in enumerate(stages):
        if i == len(stages) - 1:
            xs.append(dr["y"])
        else:
            xs.append(dscr("xs%d" % i, [S, D], F32))

    C.pb = [P.ps("pb%d" % i, [128, 512], F32) for i in range(8)]
    A = Arena(P, 200)
    C.A = A
    C.ident = A.alloc([128], BF16)
    C.identB = A.alloc([128], BF16)
    C.io = A.alloc([128], F32)
    C.ssb = A.alloc([64], F32)
    C.ksum = A.alloc([8, 16], F32)
    P.op("pool", lambda e: e.iota(C.io, pattern=[[1, 128]], base=0, channel_multiplier=-1,
                                  allow_small_or_imprecise_dtypes=True), writes=["io"])
    TS(P, "dve", C.ident, C.io, 0.0, None, ALU.is_equal, None, ["io"], ["ident"])
    TS(P, "dve", C.identB, C.io, 0.0, BIG, ALU.is_equal, ALU.mult, ["io"], ["identB"])
    base_off = A.off
    P.barrier()

    for i, stg in enumerate(stages):
        A.off = base_off
        last = (i == len(stages) - 1)
        if stg == "a0":
            attn_phaseA(P, C, dr, xs[i], 0)
            P.barrier()
            A.off = base_off
            if debug is not None and debug.startswith("A"):
                dbg_copy(P, C, dr, xs[i], xs[i + 1])
            else:
                attn_phaseB(P, C, dr, xs[i], xs[i + 1], 0, debug)
        elif stg == "a1":
            C.kiTz = [A.alloc([S], BF16) for _ in range(2)]
            C.wabs = A.alloc([NT, 8], F32)
            C.wsgn = A.alloc([NT, 8], F32)
            base1 = A.off
            attn_phaseA(P, C, dr, xs[i], 1)
            P.barrier()
            A.off = base1
            attn_phaseB(P, C, dr, xs[i], xs[i + 1], 1)
        elif stg == "m0":
            mlp_phase(P, C, dr, xs[i], xs[i + 1], 0, False)
        elif stg == "m1":
            mlp_phase(P, C, dr, xs[i], xs[i + 1], 1, True)
        P.barrier()
    P.emit()
    return nc


def load_w_cast(P, dst3, src, nk, ncols, name, c0=0):
    pairs = []
    step = 2048
    for k in range(nk):
        for cc in range(0, ncols, step):
            w = min(step, ncols - cc)
            pairs.append((dst3[:, k, cc:cc + w], src[k * 128:(k + 1) * 128, c0 + cc:c0 + cc + w]))
    DMAN(P, "pool", "w_" + name, pairs, [], [name])


class Normer:
    def __init__(self, P, C, gb_dram_row, nbuf=3, tag="n"):
        A = C.A
        self.P, self.C = P, C
        self.gb = A.alloc([D], F32)
        DMA(P, "sp", "gbld", self.gb, gb_dram_row, [], ["gb"])
        self.nbuf = nbuf
        self.xt = [A.alloc([D], F32) for _ in range(nbuf)]
        self.xn = [A.alloc([D], BF16) for _ in range(2)]
        self.junk = A.alloc([D], BF16)
        self.cnt = 0

    def load(self, src_tile):
        i = self.cnt % self.nbuf
        DMA(self.P, "sp", "xt%d" % i, self.xt[i], src_tile, [], ["xt%d" % i])
        return i

    def norm_T(self, i, dstT, c0, dst_name, evac_eng="act"):
        P, C = self.P, self.C
        k = self.cnt
        self.cnt += 1
        xt = self.xt[i]
        xn = self.xn[k % 2]
        xnn = "xn%d" % (k % 2)
        ss = C.ssb[:, (k % 8) * 2:(k % 8) * 2 + 1]
        rs = C.ssb[:, (k % 8) * 2 + 1:(k % 8) * 2 + 2]
        sn = "ss%d" % (k % 8)
        ACT(P, self.junk, xt, AF.Square, ["xt%d" % i], ["junk", sn], accum=ss)
        with P.strict():
            ACT(P, rs, ss, AF.Ln, [sn], [sn + "r"], scale=1.0 / D, bias=EPS)
            ACT(P, rs, rs, AF.Exp, [sn + "r"], [sn + "r"], scale=-0.5)
        STT(P, xn, xt, rs, self.gb, ALU.mult, ALU.mult, ["xt%d" % i, sn + "r", "gb"], [xnn])
        pbi = k % 2
        pbv = C.pb[pbi][:].bitcast(BF16)
        for kc in range(8):
            TR(P, pbv[:, kc * 128:(kc + 1) * 128], xn[:, kc * 128:(kc + 1) * 128], C.ident, [xnn, "ident"], ["pb%d" % pbi])
        COPY(P, evac_eng, dstT[:, :, c0:c0 + 128], pbv.rearrange("p (a b) -> p a b", a=8), ["pb%d" % pbi], [dst_name])


def attn_phaseA(P, C, dr, xin, layer):
    A = C.A
    S, NT, NG = C.S, C.NT, C.NG
    nm = Normer(P, C, dr["gb"][2 * layer], nbuf=3)
    hnT = [A.alloc([8, 512], BF16) for _ in range(2)]
    qst = A.alloc([8, 512], BF16)
    kst = A.alloc([8, 512], BF16)
    vst = [A.alloc([8, 2, 128], BF16) for _ in range(2)]
    for b in range(2):
        P.op("pool", (lambda e, b=b: e.memset(vst[b], 1.0)), writes=["vst%d" % b])
    if layer == 0:
        wq = A.alloc([8, D], BF16)
        wk = A.alloc([8, D], BF16)
        wv = A.alloc([8, D], BF16)
        load_w_cast(P, wq, dr["wqkv"], 8, D, "wq", 0)
        load_w_cast(P, wk, dr["wqkv"], 8, D, "wk", D)
        load_w_cast(P, wv, dr["wqkv"], 8, D, "wv", 2 * D)
        P.op("dve", lambda e: e.memset(C.ksum, 0.0), writes=["ksum"])
    else:
        win = A.alloc([8, 584], BF16)
        wuq = A.alloc([2, D], BF16)
        wqi = A.alloc([2, 512], BF16)
        wuk = A.alloc([2, D], BF16)
        wuv = A.alloc([2, D], BF16)
        load_w_cast(P, win, dr["win"], 8, 584, "win")
        load_w_cast(P, wuq, dr["wuq"], 2, D, "wuq")
        load_w_cast(P, wqi, dr["wqi"], 2, 512, "wqi")
        load_w_cast(P, wuk, dr["wukT"], 2, D, "wuk")
        load_w_cast(P, wuv, dr["wuv"], 2, D, "wuv")
        gq = A.alloc([512], F32)
        DMA(P, "sp", "gqld", gq, dr["gqkv"], [], ["gq"])
        cn = [A.alloc([512], BF16) for _ in range(2)]
        ki2 = [A.alloc([2, 128], BF16) for _ in range(2)]
        for b in range(2):
            P.op("pool", (lambda e, b=b: e.memset(ki2[b], 0.0)), writes=["ki2%d" % b])
        cnT = [A.alloc([4, 512], BF16) for _ in range(2)]
        qist = A.alloc([4, 512], BF16)
        junk2 = A.alloc([256], BF16)
    rot = [2, 3, 4, 5, 6, 7]
    rc = [0]

    def nextbank():
        b = rot[rc[0] % len(rot)]
        rc[0] += 1
        return b

    xtiles = xin.rearrange("(t p) d -> t p d", p=128)
    for g in range(NG):
        hb = g % 2
        hT = hnT[hb]
        hname = "hnT%d" % hb
        for j in range(4):
            t = g * 4 + j
            i = nm.load(xtiles[t])
            nm.norm_T(i, hT, j * 128, hname, evac_eng="act" if layer == 0 else "dve")
        if layer == 0:
            for which, wmat, stg, sname in (("q", wq, qst, "qst"), ("k", wk, kst, "kst")):
                for hp in range(8):
                    b = nextbank()
                    for kc in range(8):
                        MM(P, C.pb[b][:, :], wmat[:, kc, hp * 128:(hp + 1) * 128], hT[:, kc, :], kc == 0, kc == 7,
                           ["w" + which, hname], ["pb%d" % b])
                    if which == "q":
                        ACT(P, stg[:, hp, :], C.pb[b][:, :], AF.Identity, ["pb%d" % b], [sname], scale=0.125)
                    else:
                        COPY(P, "act", stg[:, hp, :], C.pb[b][:, :], ["pb%d" % b], [sname])
                        RED(P, C.ksum[:, hp, 2 * g:2 * g + 2], C.pb[b][:, :].rearrange("p (a b) -> p a b", a=2), ALU.add,
                            ["pb%d" % b], ["ksum"])
                dst = dr["qT" if which == "q" else "kT"][:, :, g * 512:(g + 1) * 512].rearrange("h p t -> p h t")
                DMA(P, "sp", sname, dst, stg, [sname], [])
        else:
            for j in range(4):
                t = g * 4 + j
                tc = slice(j * 128, (j + 1) * 128)
                ba = nextbank()
                bb = nextbank()
                for kc in range(8):
                    MM(P, C.pb[ba][:, :], hT[:, kc, tc], win[:, kc, 0:512], kc == 0, kc == 7, [hname, "win"], ["pb%d" % ba])
                for kc in range(8):
                    MM(P, C.pb[bb][:, 0:72], hT[:, kc, tc], win[:, kc, 512:584], kc == 0, kc == 7, [hname, "win"], ["pb%d" % bb])
                cb = t % 2
                ssq = C.ssb[:, 32 + cb * 4:32 + cb * 4 + 1]
                ssk = C.ssb[:, 32 + cb * 4 + 1:32 + cb * 4 + 2]
                rsq = C.ssb[:, 32 + cb * 4 + 2:32 + cb * 4 + 3]
                rsk = C.ssb[:, 32 + cb * 4 + 3:32 + cb * 4 + 4]
                sn = "cs%d" % cb
                ACT(P, junk2, C.pb[ba][:, 0:256], AF.Square, ["pb%d" % ba], ["junk2", sn], accum=ssq)
                ACT(P, junk2, C.pb[ba][:, 256:512], AF.Square, ["pb%d" % ba], ["junk2", sn], accum=ssk)
                with P.strict():
                    ACT(P, rsq, ssq, AF.Ln, [sn], [sn + "q"], scale=1.0 / 256, bias=EPS)
                    ACT(P, rsq, rsq, AF.Exp, [sn + "q"], [sn + "q"], scale=-0.5)
                    ACT(P, rsk, ssk, AF.Ln, [sn], [sn + "k"], scale=1.0 / 256, bias=EPS)
                    ACT(P, rsk, rsk, AF.Exp, [sn + "k"], [sn + "k"], scale=-0.5)
                cname = "cn%d" % cb
                STT(P, cn[cb][:, 0:256], C.pb[ba][:, 0:256], rsq, gq[:, 0:256], ALU.mult, ALU.mult,
                    ["pb%d" % ba, sn + "q", "gq"], [cname])
                STT(P, cn[cb][:, 256:512], C.pb[ba][:, 256:512], rsk, gq[:, 256:512], ALU.mult, ALU.mult,
                    ["pb%d" % ba, sn + "k", "gq"], [cname])
                kname = "ki2%d" % cb
                COPY(P, "act", ki2[cb][:, 0, 0:64], C.pb[bb][:, 0:64], ["pb%d" % bb], [kname])
                COPY(P, "act", ki2[cb][:, 1, 64:128], C.pb[bb][:, 0:64], ["pb%d" % bb], [kname])
                TS(P, "dve", C.wabs[:, t, :], C.pb[bb][:, 64:72], (8.0 ** -0.5) * (64.0 ** -0.5), None, ALU.mult, None,
                   ["pb%d" % bb], ["wabs"])
                pbv = C.pb[cb][:].bitcast(BF16)
                for q4 in range(4):
                    TR(P, pbv[:, q4 * 128:(q4 + 1) * 128], cn[cb][:, q4 * 128:(q4 + 1) * 128], C.ident, [cname, "ident"], ["pb%d" % cb])
                TR(P, pbv[:, 512:640], ki2[cb][:, 0, :], C.ident, [kname, "ident"], ["pb%d" % cb])
                TR(P, pbv[:, 640:768], ki2[cb][:, 1, :], C.ident, [kname, "ident"], ["pb%d" % cb])
                COPY(P, "act", cnT[hb][:, :, tc], pbv[:, 0:512].rearrange("p (a b) -> p a b", a=4), ["pb%d" % cb], ["cnT%d" % hb])
                COPY(P, "act", C.kiTz[0][:, t * 128:(t + 1) * 128], pbv[:, 512:640], ["pb%d" % cb], ["kiT"])
                COPY(P, "act", C.kiTz[1][:, t * 128:(t + 1) * 128], pbv[:, 640:768], ["pb%d" % cb], ["kiT"])
            cT = cnT[hb]
            cTn = "cnT%d" % hb
            for hp in range(8):
                b = nextbank()
                for kc in range(2):
                    MM(P, C.pb[b][:, :], wuq[:, kc, hp * 128:(hp + 1) * 128], cT[:, kc, :], kc == 0, kc == 1, ["wuq", cTn], ["pb%d" % b])
                ACT(P, qst[:, hp, :], C.pb[b][:, :], AF.Identity, ["pb%d" % b], ["qst"], scale=0.125)
            DMA(P, "sp", "qst", dr["qT"][:, :, g * 512:(g + 1) * 512].rearrange("h p t -> p h t"), qst, ["qst"], [])
            for ch in range(4):
                b = nextbank()
                for kc in range(2):
                    MM(P, C.pb[b][:, :], wqi[:, kc, ch * 128:(ch + 1) * 128], cT[:, kc, :], kc == 0, kc == 1, ["wqi", cTn], ["pb%d" % b])
                COPY(P, "dve", qist[:, ch, :], C.pb[b][:, :], ["pb%d" % b], ["qist"])
            DMA(P, "sp", "qist", dr["qiT"][:, :, g * 512:(g + 1) * 512].rearrange("h p t -> p h t"), qist, ["qist"], [])
            for hp in range(8):
                b = nextbank()
                for kc in range(2):
                    MM(P, C.pb[b][:, :], wuk[:, kc, hp * 128:(hp + 1) * 128], cT[:, 2 + kc, :], kc == 0, kc == 1, ["wuk", cTn], ["pb%d" % b])
                COPY(P, "act", kst[:, hp, :], C.pb[b][:, :], ["pb%d" % b], ["kst"])
            DMA(P, "sp", "kst", dr["kT"][:, :, g * 512:(g + 1) * 512].rearrange("h p t -> p h t"), kst, ["kst"], [])
        for j in range(4):
            t = g * 4 + j
            tc = slice(j * 128, (j + 1) * 128)
            vb = t % 2
            vname = "vst%d" % vb
            for half in range(2):
                b = nextbank()
                if layer == 0:
                    for kc in range(8):
                        MM(P, C.pb[b][:, :], hT[:, kc, tc], wv[:, kc, half * 512:(half + 1) * 512], kc == 0, kc == 7, [hname, "wv"], ["pb%d" % b])
                else:
                    for kc in range(2):
                        MM(P, C.pb[b][:, :], cT[:, 2 + kc, tc], wuv[:, kc, half * 512:(half + 1) * 512], kc == 0, kc == 1, [cTn, "wuv"], ["pb%d" % b])
                psv = C.pb[b][:, :].rearrange("p (a b c) -> p a b c", a=4, b=2)
                COPY(P, "dve", vst[vb][:, half * 4:(half + 1) * 4, 0, 0:64], psv[:, :, 0, :], ["pb%d" % b], [vname])
                COPY(P, "dve", vst[vb][:, half * 4:(half + 1) * 4, 1, 64:128], psv[:, :, 1, :], ["pb%d" % b], [vname])
            dst = dr["va"][:, :, t, :].rearrange("(a b) p d -> p a b d", b=2)
            DMA(P, "sp", vname, dst, vst[vb], [vname], [])


def dbg_copy(P, C, dr, xin, xout):
    A = C.A
    xt = A.alloc([D], F32)
    xin_t = xin.rearrange("(t p) d -> t p d", p=128)
    xout_t = xout.rearrange("(t p) d -> t p d", p=128)
    for t in range(C.NT):
        DMA(P, "sp", "dbgl", xt, xin_t[t], [], ["dbgx"])
        DMA(P, "pool", "dbgs", xout_t[t], xt, ["dbgx"], [])


SALL = ["score%d" % i_ for i_ in range(8)]


def attn_phaseB(P, C, dr, xin, xout, layer, debug=None):
    A = C.A
    S, NT, NG = C.S, C.NT, C.NG
    wo = A.alloc([8, D], BF16)
    load_w_cast(P, wo, dr["wo0" if layer == 0 else "wo1"], 8, D, "wo")
    if layer == 1:
        score = A.alloc([max(S, H * 256)], F32)
        tbf = score[:, 0:H * 256].rearrange("p (a b) -> p a b", a=H)
        tbn = "score0"
    else:
        tbf = A.alloc([H, 256], F32)
        tbn = "tbf"
    b31 = A.alloc([H], F32)
    cm0 = A.alloc([128], F32)
    TBp = A.alloc([H, 256], BF16)
    DMA(P, "sp", "tbld", tbf, dr["tb"], [], SALL if layer == 1 else [tbn])
    DMA(P, "sp", "b31ld", b31, dr["b31"], [], ["b31"])
    TS(P, "dve", cm0, C.io, 0.0, -BIG, ALU.is_lt, ALU.mult, ["io"], ["cm0"])
    for h in range(H):
        STT(P, TBp[:, h, 0:128], tbf[:, h, 0:128], b31[:, h:h + 1], cm0, ALU.subtract, ALU.add, (SALL if layer == 1 else [tbn]) + ["b31", "cm0"], ["TBp"])
        TS(P, "dve", TBp[:, h, 128:256], tbf[:, h, 128:256], b31[:, h:h + 1], None, ALU.subtract, None, (SALL if layer == 1 else [tbn]) + ["b31"], ["TBp"])
    nqb = 2
    qz = [[A.alloc([8, 512], BF16) for _ in range(2)] for _ in range(nqb)]
    for b in range(2):
        P.op("pool", (lambda e, b=b: e.memset(qz[b][0][64:128], 0.0)), writes=["qg%d" % b])
        P.op("pool", (lambda e, b=b: e.memset(qz[b][1][0:64], 0.0)), writes=["qg%d" % b])
    kbuf = [A.alloc([S], BF16) for _ in range(2)]
    vbuf = [A.alloc([NT, 128], BF16) for _ in range(2)]
    pbuf = [A.alloc([512], BF16) for _ in range(3)]
    oT = A.alloc([8, 512], BF16)
    rec = A.alloc([512], F32)
    xt = [A.alloc([D], F32) for _ in range(2)]
    if layer == 0:
        kbd = A.alloc([8, 32], BF16)
        nidx = A.alloc([256], F32)
        pneg = A.alloc([256], F32)
        ownm1 = A.alloc([256], F32)
        gm = A.alloc([256], F32)
        gm2 = A.alloc([256], F32)
        tmpg = A.alloc([256], F32)
        mx = A.alloc([16], F32)
        selv = A.alloc([256], BF16)
        selT2 = [A.alloc([H, 512], BF16) for _ in range(2)]
        Esel = A.alloc([16, 128], BF16)
        iot = A.alloc([16, 128], F32)
        for b_ in range(2):
            P.op("pool", (lambda e, b_=b_: e.memset(selT2[b_], 0.0)), writes=["selT%d" % b_])
        P.op("dve", lambda e: e.memset(kbd, 0.0), writes=["kbd"])
        COPY(P, "dve", kbd[0:64, :, 0:16], C.ksum[0:64, :, :], ["ksum", "kbd"], ["kbd"])
        COPY(P, "dve", kbd[64:128, :, 16:32], C.ksum[64:128, :, :], ["ksum", "kbd"], ["kbd"])
        P.op("pool", lambda e: e.iota(nidx.rearrange("p (a b) -> p a b", a=16), pattern=[[0, 16], [1, 16]], base=0,
                                      channel_multiplier=0, allow_small_or_imprecise_dtypes=True), writes=["nidx"])
        P.op("pool", lambda e: e.iota(iot, pattern=[[1, 16], [0, 128]], base=0, channel_multiplier=-1,
                                      allow_small_or_imprecise_dtypes=True), writes=["iot"])
        TS(P, "dve", Esel, iot, 0.0, BIG, ALU.is_equal, ALU.mult, ["iot"], ["Esel"])
    else:
        qig = A.alloc([4, 512], BF16)
        madd = A.alloc([S], BF16)
        junkb = madd
        maskT = A.alloc([NT, 512], BF16)
        cneg = A.alloc([128], F32)
        cpos = A.alloc([128], F32)
        dtmp = A.alloc([128], F32)
        bs = A.alloc([16], F32)
        bsi = A.alloc([4], I32)
        pw = A.alloc([NBIS + 1], F32)
        sk = A.alloc([NBIS + 1], F32)
        for k_ in range(NBIS + 1):
            P.op("dve", (lambda e, k_=k_: e.memset(pw[:, k_:k_ + 1], 2.0 ** -(k_ + 1))), writes=["pw"])
        TS(P, "dve", cneg, C.io, 0.0, NEGF, ALU.is_gt, ALU.mult, ["io"], ["cneg"])
        TS(P, "dve", cpos, C.io, 0.0, -NEGF, ALU.is_gt, ALU.mult, ["io"], ["cpos"])
    xin_t = xin.rearrange("(t p) d -> t p d", p=128)
    xout_t = xout.rearrange("(t p) d -> t p d", p=128)
    lrot = [2, 3, 4]
    lc = [0]
    ucnt = [0]
    def load_q(gq):
        qb_ = gq % nqb
        DMAN(P, "sp", "qg%d" % qb_, [(qz[qb_][0][0:64], dr["qT"][:, 0:64, gq * 512:(gq + 1) * 512].rearrange("h p t -> p h t")),
                                    (qz[qb_][1][64:128], dr["qT"][:, 64:128, gq * 512:(gq + 1) * 512].rearrange("h p t -> p h t"))],
             [], ["qg%d" % qb_])

    def gate_a(gq, j):
        t = gq * 4 + j
        blk = t // 2
        QG = qz[gq % nqb]
        qn_ = "qg%d" % (gq % nqb)
        if t % 2 == 0:
            TS(P, "dve", pneg, nidx, float(blk), NEGF, ALU.is_ge, ALU.mult, ["nidx"], ["pneg"])
            TS(P, "dve", ownm1, nidx, float(blk), -1.0, ALU.is_ge, ALU.add, ["nidx"], ["ownm1"])
        for hp in range(8):
            for h2 in range(2):
                MM(P, C.pb[0][:, hp * 32:(hp + 1) * 32], QG[h2][:, hp, j * 128:(j + 1) * 128], kbd[:, hp, :], h2 == 0, h2 == 1,
                   [qn_, "kbd"], ["pb0"])
        TT(P, "dve", gm, C.pb[0][:, 0:256], pneg, ALU.add, ["pb0", "pneg"], ["gm"])
        g3 = gm.rearrange("p (a b) -> p a b", a=16)
        g23 = gm2.rearrange("p (a b) -> p a b", a=16)
        t3 = tmpg.rearrange("p (a b) -> p a b", a=16)
        mb = bcast_last(mx, 16)
        RED(P, mx, g3, ALU.max, ["gm"], ["mx"])
        TT(P, "dve", t3, g3, mb, ALU.is_ge, ["gm", "mx"], ["tmpg"])
        STT(P, gm2, tmpg, NEGF, gm, ALU.mult, ALU.add, ["tmpg", "gm"], ["gm2"])
        RED(P, mx, g23, ALU.max, ["gm2"], ["mx"])
        TT(P, "dve", t3, g23, mb, ALU.is_ge, ["gm2", "mx"], ["tmpg"])
        STT(P, gm2, tmpg, NEGF, gm2, ALU.mult, ALU.add, ["tmpg", "gm2"], ["gm2"])
        RED(P, mx, g23, ALU.max, ["gm2"], ["mx"])
        TT(P, "dve", t3, g3, mb, ALU.is_ge, ["gm", "mx"], ["tmpg"])
        STT(P, selv, tmpg, -1.0, ownm1, ALU.add, ALU.max, ["tmpg", "ownm1"], ["selv"])

    def gate_b(gq, j):
        sT = selT2[gq % 2]
        pv1 = C.pb[1][:].bitcast(BF16)
        for rnd in range(2):
            for hh in range(8):
                h = rnd * 8 + hh
                TR(P, pv1[0:16, hh * 128:(hh + 1) * 128], selv[:, h * 16:(h + 1) * 16], C.ident, ["selv", "ident"], ["pb1"])
            COPY(P, "act", sT[0:16, rnd * 8:(rnd + 1) * 8, j * 128:(j + 1) * 128],
                 pv1[0:16, :].rearrange("p (a b) -> p a b", a=8), ["pb1"], ["selT%d" % (gq % 2)])

    load_q(0)
    if layer == 0:
        for j in range(4):
            gate_a(0, j)
            gate_b(0, j)
    for g in range(NG):
        qb = g % nqb
        qname = "qg%d" % qb
        QZ = qz[qb]
        nch = 4 * (g + 1)
        if g + 1 < NG:
            load_q(g + 1)
        if layer == 0:
            selT = selT2[g % 2]
            sTn = "selT%d" % (g % 2)
        else:
            DMA(P, "sp", "qig", qig, dr["qiT"][:, :, g * 512:(g + 1) * 512].rearrange("h p t -> p h t"), [], ["qig"])
            for j in range(4):
                t = g * 4 + j
                nk = 128 * (t + 1)
                qc = slice(j * 128, (j + 1) * 128)
                nsg = (nk + 511) // 512
                for hi in range(8):
                    ch = hi // 2
                    for sg in range(nsg):
                        ncol = min(512, nk - sg * 512)
                        sc = slice(sg * 512, sg * 512 + ncol)
                        bm = (0, 1, 2)[ucnt[0] % 3]
                        br = (3, 4, 6, 7)[ucnt[0] % 4]
                        ucnt[0] += 1
                        sn_ = "score%d" % sg
                        MM(P, C.pb[bm][:, 0:ncol], qig[:, ch, qc], C.kiTz[hi % 2][:, sc], True, True, ["qig", "kiT"], ["pb%d" % bm])
                        ACT(P, C.pb[br][:, 0:ncol], C.pb[bm][:, 0:ncol], AF.Relu, ["pb%d" % bm], ["pb%d" % br])
                        if hi == 0:
                            TS(P, "dve", score[:, sc], C.pb[br][:, 0:ncol], C.wabs[:, t, 0:1], None, ALU.mult, None,
                               ["pb%d" % br, "wabs"], [sn_])
                        else:
                            STT(P, score[:, sc], C.pb[br][:, 0:ncol], C.wabs[:, t, hi:hi + 1], score[:, sc], ALU.mult, ALU.add,
                                ["pb%d" % br, "wabs", sn_], [sn_])
                dg = slice(128 * t, 128 * t + 128)
                lo, hi_, rg, mid, cnt, m1 = (bs[:, k:k + 1] for k in range(6))
                fl = bsi[:, 0:1]
                P.strict_default = True
                TT(P, "dve", dtmp, score[:, dg], cpos, ALU.add, SALL + ["cpos"], ["dtmp"])
                RED(P, lo, dtmp, ALU.min, ["dtmp"], ["bs"])
                if t > 0:
                    RED(P, m1, score[:, 0:128 * t], ALU.min, SALL, ["bs"])
                    TT(P, "dve", lo, lo, m1, ALU.min, ["bs"], ["bs"])
                TT(P, "dve", score[:, dg], score[:, dg], cneg, ALU.add, SALL + ["cneg"], SALL)
                if nk > 256:
                    negS, tq = bs[:, 6:7], bs[:, 7:8]
                    RED(P, hi_, score[:, 0:nk], ALU.max, SALL, ["bs"])
                    TT(P, "dve", rg, hi_, lo, ALU.subtract, ["bs"], ["bs"])
                    TS(P, "dve", sk, pw, rg, None, ALU.mult, None, ["pw", "bs"], ["sk"])
                    TT(P, "dve", mid, lo, sk[:, 0:1], ALU.add, ["bs", "sk"], ["mid"])
                    split = nk >= 1536
                    n1 = (int(nk * 0.42) // 128) * 128 if split else nk
                    thr_c = 256.0 - (nk - n1) / 2.0 - (0.25 if split else 0.0)
                    for it in range(NBIS):
                        if split:
                            ACT(P, madd[:, n1:nk], score[:, n1:nk], AF.Sign, SALL + ["mid"], ["maddA", "negS"],
                                scale=-1.0, bias=mid, accum=negS)
                        TS(P, "dve", madd[:, 0:n1], score[:, 0:n1], mid, None, ALU.is_ge, ALU.add, SALL + ["mid"], ["maddD", "cnt"], accum=cnt)
                        if split:
                            TS(P, "dve", cnt, negS, -0.5, cnt, ALU.mult, ALU.add, ["negS", "cnt"], ["cnt"])
                        STT(P, tq, cnt, thr_c, sk[:, it:it + 1], ALU.is_ge, ALU.mult, ["cnt", "sk"], ["tq"])
                        STT(P, mid, tq, sk[:, it + 1:it + 2], mid, ALU.subtract, ALU.add, ["tq", "sk", "mid"], ["mid"])
                    TT(P, "dve", lo, mid, sk[:, NBIS:NBIS + 1], ALU.subtract, ["mid", "sk"], ["bs"])
                TS(P, "dve", madd[:, 0:nk], score[:, 0:nk], lo, 1.0, ALU.is_ge, ALU.subtract, SALL + ["bs"], ["madd", "maddA", "maddD"])
                P.strict_default = False
                pv7 = C.pb[5][:].bitcast(BF16)
                for c0 in range(0, t + 1, 8):
                    cn_ = min(8, t + 1 - c0)
                    for cc in range(cn_):
                        c = c0 + cc
                        TR(P, pv7[:, cc * 128:(cc + 1) * 128], madd[:, c * 128:(c + 1) * 128], C.ident, ["madd", "maddA", "maddD", "ident"], ["pb5"])
                    COPY(P, "act", maskT[:, c0:c0 + cn_, qc], pv7[:, 0:cn_ * 128].rearrange("p (a b) -> p a b", a=cn_), ["pb5"], ["maskT"])
        items = [(h, c) for h in range(H) for c in range(nch)]
        LOOK = 3
        lbanks = [2, 3, 4, 7] if layer == 0 else [0, 1, 2, 5]

        def hinfo(h):
            hp, h2 = h // 2, h % 2
            r0 = 64 * h2
            return hp, h2, slice(r0, r0 + 64), slice(64 - r0, 128 - r0), hp % 2, h % 2

        def issue_qk(idx):
            h, c = items[idx]
            hp, h2, rs_, so, kb, vb = hinfo(h)
            kname = "kbuf%d" % kb
            if c == 0:
                if h2 == 0:
                    DMA(P, "sp", kname, kbuf[kb][:, 0:nch * 128], dr["kT"][hp, :, 0:nch * 128], [], [kname])
                DMA(P, "sp", "vbuf%d" % vb, vbuf[vb][:, 0:nch, :], dr["va"][h, :, 0:nch, :], [], ["vbuf%d" % vb])
            dc = c - 4 * g
            col0 = max(0, dc * 128)
            cs = slice(col0, 512)
            lb = lbanks[idx % 4]
            lbn = "pb%d" % lb
            psl = C.pb[lb]
            n_blk = c // 2
            need_sel = (layer == 1) or (n_blk < 2 * g + 1)
            need_tb = dc >= -1
            MM(P, psl[:, cs], kbuf[kb][:, c * 128:(c + 1) * 128], QZ[h2][:, hp, cs], True, not (need_sel or need_tb), [kname, qname], [lbn])
            if need_sel:
                if layer == 0:
                    MM(P, psl[:, cs], Esel[:, n_blk, :], selT[:, h, cs], False, not need_tb, ["Esel", sTn], [lbn])
                else:
                    MM(P, psl[:, cs], C.identB, maskT[:, c, cs], False, not need_tb, ["identB", "maskT"], [lbn])
            if need_tb:
                if dc == -1:
                    MM(P, psl[:, 0:128], C.ident, TBp[:, h, 128:256], False, True, ["ident", "TBp"], [lbn])
                elif dc == 3:
                    MM(P, psl[:, 384:512], C.ident, TBp[:, h, 0:128], False, True, ["ident", "TBp"], [lbn])
                else:
                    MM(P, psl[:, col0:col0 + 256], C.ident, TBp[:, h, 0:256], False, True, ["ident", "TBp"], [lbn])

        for idx in range(min(LOOK, len(items))):
            issue_qk(idx)
        for idx in range(len(items)):
            if idx + LOOK < len(items):
                issue_qk(idx + LOOK)
            h, c = items[idx]
            if layer == 0 and g + 1 < NG and c == 0:
                if h % 4 == 0:
                    gate_a(g + 1, h // 4)
                elif h % 4 == 2:
                    gate_b(g + 1, h // 4)
            hp, h2, rs_, so, kb, vb = hinfo(h)
            ob = 5 + (h % 2) if layer == 0 else 3 + (h % 2)
            obn = "pb%d" % ob
            pso = C.pb[ob]
            dc = c - 4 * g
            col0 = max(0, dc * 128)
            cs = slice(col0, 512)
            lb = lbanks[idx % 4]
            lbn = "pb%d" % lb
            pi = idx % 3
            pn = "pbuf%d" % pi
            ACT(P, pbuf[pi][:, cs], C.pb[lb][:, cs], AF.Exp, [lbn], [pn])
            MM(P, pso[:, cs], vbuf[vb][:, c, :], pbuf[pi][:, cs], c == 0, c == nch - 1, ["vbuf%d" % vb, pn], [obn])
            if c == nch - 1:
                P.op("dve", (lambda e, o_=rec[so, :], i_=pso[so, :]: e.reciprocal(out=o_, in_=i_)), reads=[obn], writes=["rec"])
                TT(P, "dve", oT[rs_, hp, :], pso[rs_, :], rec[so, :], ALU.mult, [obn, "rec"], ["oT"])
        for j in range(4):
            t = g * 4 + j
            xb = t % 2
            xn_ = "xta%d" % xb
            DMA(P, "sp", xn_, xt[xb], xin_t[t], [], [xn_])
            for half in range(2):
                b = 6 + half if layer == 1 else (0, 1)[half]
                bn = "pb%d" % b
                for hp in range(8):
                    MM(P, C.pb[b][:, :], oT[:, hp, j * 128:(j + 1) * 128], wo[:, hp, half * 512:(half + 1) * 512], hp == 0, hp == 7, ["oT", "wo"], [bn])
                hs = slice(half * 512, (half + 1) * 512)
                TT(P, "dve", xt[xb][:, hs], C.pb[b][:, :], xt[xb][:, hs], ALU.add, [bn, xn_], [xn_])
            DMA(P, "pool", "xst%d" % xb, xout_t[t], xt[xb], [xn_], [])


def mlp_phase(P, C, dr, xin, xout, layer, final):
    A = C.A
    S, NT = C.S, C.NT
    wup = A.alloc([8, DFF], BF16)
    wdn = A.alloc([32, D], BF16)
    load_w_cast(P, wup, dr["wup%d" % layer], 8, DFF, "wup")
    load_w_cast(P, wdn, dr["wdn%d" % layer], 32, D, "wdn")
    nm = Normer(P, C, dr["gb"][2 * layer + 1], nbuf=4)
    if final:
        fnb = A.alloc([D], F32)
        DMA(P, "sp", "fnld", fnb, dr["gb"][4], [], ["fnb"])
        junkf = A.alloc([D], BF16)
    hnT = [A.alloc([8, 256], BF16) for _ in range(2)]
    hTt = A.alloc([32, 256], BF16)
    rbuf = [A.alloc([512], BF16) for _ in range(3)]
    xin_t = xin.rearrange("(t p) d -> t p d", p=128)
    xout_t = xout.rearrange("(t p) d -> t p d", p=128)
    rcnt = [0]
    for g2 in range(NT // 2):
        hb = g2 % 2
        hname = "mhT%d" % hb
        xi = []
        for j in range(2):
            t = g2 * 2 + j
            i = nm.load(xin_t[t])
            xi.append(i)
            nm.norm_T(i, hnT[hb], j * 128, hname, evac_eng="dve")
        for f2 in range(16):
            b = 2 + (f2 % 2)
            bn = "pb%d" % b
            for q in range(2):
                ffc = f2 * 2 + q
                for kc in range(8):
                    MM(P, C.pb[b][:, q * 256:(q + 1) * 256], wup[:, kc, ffc * 128:(ffc + 1) * 128], hnT[hb][:, kc, :], kc == 0, kc == 7,
                       ["wup", hname], [bn])
            ri = rcnt[0] % 3
            rcnt[0] += 1
            rn = "rbuf%d" % ri
            ACT(P, rbuf[ri], C.pb[b][:, :], AF.Relu, [bn], [rn])
            TT(P, "dve", hTt[:, f2 * 2:f2 * 2 + 2, :], C.pb[b][:, :].rearrange("p (a b) -> p a b", a=2),
               rbuf[ri].rearrange("p (a b) -> p a b", a=2), ALU.mult, [bn, rn], ["hTt"])
        for j in range(2):
            t = g2 * 2 + j
            i = xi[j]
            xn_ = "xt%d" % i
            for half in range(2):
                b = 4 + j * 2 + half
                bn = "pb%d" % b
                for ffc in range(32):
                    MM(P, C.pb[b][:, :], hTt[:, ffc, j * 128:(j + 1) * 128], wdn[:, ffc, half * 512:(half + 1) * 512], ffc == 0, ffc == 31,
                       ["hTt", "wdn"], [bn])
                hs = slice(half * 512, (half + 1) * 512)
                TT(P, "dve", nm.xt[i][:, hs], C.pb[b][:, :], nm.xt[i][:, hs], ALU.add, [bn, xn_], [xn_])
            if final:
                ss = C.ssb[:, 48 + (t % 4) * 2:48 + (t % 4) * 2 + 1]
                rs = C.ssb[:, 48 + (t % 4) * 2 + 1:48 + (t % 4) * 2 + 2]
                sn = "fs%d" % (t % 4)
                ACT(P, junkf, nm.xt[i], AF.Square, [xn_], ["junkf", sn], accum=ss)
                with P.strict():
                    ACT(P, rs, ss, AF.Ln, [sn], [sn + "r"], scale=1.0 / D, bias=EPS)
                    ACT(P, rs, rs, AF.Exp, [sn + "r"], [sn + "r"], scale=-0.5)
                STT(P, nm.xt[i], nm.xt[i], rs, fnb, ALU.mult, ALU.mult, [xn_, sn + "r", "fnb"], [xn_])
            DMA(P, "pool", "xst%d" % i, xout_t[t], nm.xt[i], [xn_], [])


def _rel_bucket_np(dist):
    n = np.maximum(dist, 0)
    exact = 16
    nf = np.maximum(n, 1).astype(np.float32)
    large = exact + (np.log(nf / np.float32(exact)) / np.float32(math.log(128 / exact)) * np.float32(32 - exact)).astype(np.int32)
    large = np.minimum(large, 31)
    return np.where(n < exact, n, large)


def host_layout(inp):
    f = lambda a: np.ascontiguousarray(np.asarray(a, dtype=np.float32))
    rb = f(inp["rel_bias"])
    s_idx = np.arange(128)[:, None]
    q_idx = np.arange(128)[None, :]
    bk0 = _rel_bucket_np(q_idx - s_idx)
    bk1 = _rel_bucket_np(q_idx - s_idx + 128)
    tb = np.empty((128, H, 256), np.float32)
    tb[:, :, 0:128] = rb[bk0].transpose(0, 2, 1)
    tb[:, :, 128:256] = rb[bk1].transpose(0, 2, 1)
    gb = np.stack([inp["ln_attn"][0], inp["ln_mlp"][0], inp["ln_attn"][1], inp["ln_mlp"][1], inp["final_norm"]])
    gb = f(np.broadcast_to(gb[:, None, :], (5, 128, D)))
    gqkv = f(np.broadcast_to(np.concatenate([inp["dsa_g_q"][0], inp["dsa_g_kv"][0]])[None, :], (128, 512)))
    m = {
        "gb": gb, "gqkv": gqkv, "tb": f(tb), "b31": f(np.broadcast_to(rb[31][None, :], (128, H))),
        "wqkv": f(inp["moba_w_qkv"][0]), "wo0": f(inp["moba_w_o"][0]), "win": f(inp["dsa_w_in"][0]),
        "wuq": f(inp["dsa_w_uq"][0]), "wqi": f(inp["dsa_w_qi"][0]),
        "wukT": f(np.transpose(inp["dsa_w_uk"][0], (2, 0, 1)).reshape(256, D)),
        "wuv": f(np.transpose(inp["dsa_w_uv"][0], (1, 0, 2)).reshape(256, D)),
        "wo1": f(inp["dsa_w_o"][0]),
        "wup0": f(inp["mlp_w_up"][0]), "wup1": f(inp["mlp_w_up"][1]),
        "wdn0": f(inp["mlp_w_down"][0]), "wdn1": f(inp["mlp_w_down"][1]),
    }
    return m


_NC_CACHE = {}


def kernel(**inputs):
    x = np.asarray(inputs["x"], dtype=np.float32)
    B = x.shape[0]
    shared = host_layout(inputs)
    key = ("full", x.shape[1])
    if key not in _NC_CACHE:
        _NC_CACHE[key] = build(("a0", "m0", "a1", "m1"), S=x.shape[1])
    nc = _NC_CACHE[key]
    in_maps = []
    for b in range(B):
        m = dict(shared)
        m["x"] = np.ascontiguousarray(x[b])
        in_maps.append(m)
    res = run_bass_kernel_spmd(nc, in_maps, core_ids=list(range(B)))
    return np.stack([np.asarray(r["y"], dtype=np.float32) for r in res.results], axis=0)
```

```python
import math
import numpy as np
import concourse.bass as bass
import concourse.mybir as mybir
from concourse.bass_utils import run_bass_kernel_spmd
from contextlib import ExitStack

F32 = mybir.dt.float32
BF16 = mybir.dt.bfloat16
I32 = mybir.dt.int32
AF = mybir.ActivationFunctionType
ALU = mybir.AluOpType
AX = mybir.AxisListType

D = 1024
H = 16
DFF = 4096
BIG = 30000.0
NEGF = -1.0e30
EPS = 1e-6
NBIS = 16


class Op:
    __slots__ = ("eng", "fn", "deps", "needs_inc", "tok", "sem", "is_dma", "chan", "n")


class Prog:
    ENG = ("pe", "act", "dve", "pool", "sp")

    def __init__(self, nc):
        self.nc = nc
        self.ops = {e: [] for e in self.ENG}
        self.res = {}
        self.stack = ExitStack()
        self.chan_sems = {}
        self.chan_cnt = {}
        self.last = {e: None for e in self.ENG}
        self.last_dma = {}
        self.strict_default = False

    def sb(self, name, shape, dt):
        return self.stack.enter_context(self.nc.sbuf_tensor(name, list(shape), dt))

    def ps(self, name, shape, dt):
        return self.stack.enter_context(self.nc.psum_tensor(name, list(shape), dt))

    def _add(self, eng, fn, reads, writes, chan=None, n=1, strict=False):
        op = Op()
        op.eng, op.fn, op.needs_inc, op.is_dma, op.chan, op.n = eng, fn, False, chan is not None, chan, n
        deps = set()
        raw = set()
        for r in reads:
            st = self.res.get(r)
            if st is not None and st[0] is not None:
                deps.add(st[0])
                raw.add(st[0])
            if st is not None and r.startswith("pb"):
                deps.update(st[1])
        for w in writes:
            st = self.res.get(w)
            if st is not None:
                if st[0] is not None:
                    deps.add(st[0])
                deps.update(st[1])
        keep = []
        for d in deps:
            if d is op or d.fn is None:
                continue
            if (not d.is_dma) and d.eng == eng and eng == "pe":
                continue
            d.needs_inc = True
            keep.append(d)
        op.deps = keep
        for r in reads:
            lst = self.res.setdefault(r, [None, []])[1]
            if not op.is_dma:
                lst[:] = [o for o in lst if o.is_dma or o.eng != eng]
            lst.append(op)
        for w in writes:
            self.res[w] = [op, []]
        self.ops[eng].append(op)
        if op.is_dma:
            self.last_dma[chan] = op
        else:
            self.last[eng] = op
        return op

    def op(self, eng, fn, reads=(), writes=(), strict=None):
        if strict is None:
            strict = self.strict_default
        return self._add(eng, fn, reads, writes, strict=strict)

    def dma(self, eng, chan, fn, reads=(), writes=(), n=1):
        return self._add(eng, fn, reads, writes, chan=chan, n=n)

    def strict(self):
        P = self

        class _S:
            def __enter__(self_):
                self_.old = P.strict_default
                P.strict_default = True

            def __exit__(self_, *a):
                P.strict_default = self_.old
        return _S()

    def barrier(self):
        lasts = [o for o in self.last.values() if o is not None]
        dmas = list(self.last_dma.values())
        for e in self.ENG:
            op = Op()
            op.eng, op.fn, op.needs_inc, op.is_dma, op.chan, op.n = e, None, False, False, None, 0
            op.deps = []
            for d in lasts:
                if d.eng != e:
                    d.needs_inc = True
                    op.deps.append(d)
            for d in dmas:
                op.deps.append(d)
            self.ops[e].append(op)
        self.res = {}

    def emit(self):
        nc = self.nc
        st = self.stack
        esem = {e: st.enter_context(nc.semaphore("sem_" + e)) for e in self.ENG}
        for e in self.ENG:
            for op in self.ops[e]:
                if op.is_dma and op.chan not in self.chan_sems:
                    self.chan_sems[op.chan] = st.enter_context(nc.semaphore("ch_" + op.chan))
                    self.chan_cnt[op.chan] = 0
        for e in self.ENG:
            c = 0
            for op in self.ops[e]:
                if op.is_dma:
                    self.chan_cnt[op.chan] += 16 * op.n
                    op.sem = self.chan_sems[op.chan]
                    op.tok = self.chan_cnt[op.chan]
                else:
                    if op.needs_inc:
                        c += 1
                    op.sem = esem[e]
                    op.tok = c
        finals = [(s, self.chan_cnt[ch]) for ch, s in self.chan_sems.items()]
        block = st.enter_context(nc.Block())

        def body_for(e):
            def body(eng):
                known = {}
                for op in self.ops[e]:
                    need = {}
                    for d in op.deps:
                        k = id(d.sem)
                        if k not in need or need[k][1] < d.tok:
                            need[k] = (d.sem, d.tok)
                    for k, (s, v) in need.items():
                        if known.get(k, 0) >= v:
                            continue
                        eng.wait_ge(s, v)
                        known[k] = v
                    if op.fn is None:
                        continue
                    ins = op.fn(eng)
                    if op.is_dma:
                        if op.n == 1:
                            ins.then_inc(op.sem, 16)
                        else:
                            for i_ in ins:
                                i_.then_inc(op.sem, 16)
                    elif op.needs_inc:
                        ins.then_inc(op.sem, 1)
                if e == "sp":
                    for s, v in finals:
                        eng.wait_ge(s, v)
            return body

        block.tensor(body_for("pe"))
        block.scalar(body_for("act"))
        block.vector(body_for("dve"))
        block.gpsimd(body_for("pool"))
        block.sync(body_for("sp"))
        st.close()


class Arena:
    def __init__(self, P, kbytes):
        self.t = P.sb("arena", [128, kbytes * 256], F32)
        self.cap = kbytes * 1024
        self.off = 0

    def alloc(self, free_shape, dt):
        n = 1
        for s in free_shape:
            n *= s
        nbytes = n * mybir.dt.size(dt)
        nbytes = (nbytes + 63) // 64 * 64
        assert self.off + nbytes <= self.cap, ("arena overflow", self.off, nbytes, self.cap)
        a = self.t[:, self.off // 4:(self.off + nbytes) // 4]
        self.off += nbytes
        if dt != F32:
            a = a.bitcast(dt)
        a = a[:, 0:n]
        if len(free_shape) == 2:
            a = a.rearrange("p (a b) -> p a b", a=free_shape[0])
        elif len(free_shape) == 3:
            a = a.rearrange("p (a b c) -> p a b c", a=free_shape[0], b=free_shape[1])
        return a


def bcast_last(ap2d, n):
    return bass.AP(ap2d.tensor, ap2d.offset, [list(ap2d.ap[0]), list(ap2d.ap[1]), [0, n]])


def MM(P, out, lhsT, rhs, start, stop, r, w):
    P.op("pe", lambda e: e.matmul(out, lhsT=lhsT, rhs=rhs, start=start, stop=stop), reads=r, writes=w)


def TR(P, out, in_, ident, r, w):
    P.op("pe", lambda e: e.transpose(out, in_, ident), reads=r, writes=w)


def ACT(P, out, in_, func, r, w, scale=None, bias=None, accum=None):
    kw = {}
    if scale is not None:
        kw["scale"] = scale
    if bias is not None:
        kw["bias"] = bias
    if accum is not None:
        kw["accum_out"] = accum
    P.op("act", lambda e: e.activation(out=out, in_=in_, func=func, **kw), reads=r, writes=w)


def TS(P, eng, out, in0, s1, s2, op0, op1, r, w, accum=None):
    kw = {}
    if op1 is not None:
        kw["op1"] = op1
    if accum is not None:
        kw["accum_out"] = accum
    P.op(eng, lambda e: e.tensor_scalar(out=out, in0=in0, scalar1=s1, scalar2=s2, op0=op0, **kw), reads=r, writes=w)


def TT(P, eng, out, in0, in1, op, r, w):
    P.op(eng, lambda e: e.tensor_tensor(out=out, in0=in0, in1=in1, op=op), reads=r, writes=w)


def STT(P, out, in0, scalar, in1, op0, op1, r, w):
    P.op("dve", lambda e: e.scalar_tensor_tensor(out=out, in0=in0, scalar=scalar, in1=in1, op0=op0, op1=op1), reads=r, writes=w)


SKIP_RED = [False]


def RED(P, out, in_, op, r, w):
    if SKIP_RED[0]:
        return
    P.op("dve", lambda e: e.tensor_reduce(out=out, in_=in_, axis=AX.X, op=op), reads=r, writes=w)


def COPY(P, eng, out, in_, r, w):
    if eng == "act":
        P.op("act", lambda e: e.copy(out=out, in_=in_), reads=r, writes=w)
    else:
        P.op(eng, lambda e: e.tensor_copy(out=out, in_=in_), reads=r, writes=w)


SKIP_CH = set()


def DMA(P, eng, chan, out, in_, r, w):
    if chan in SKIP_CH:
        return
    P.dma(eng, chan, lambda e: e.dma_start(out=out, in_=in_), reads=r, writes=w)


def DMAN(P, eng, chan, pairs, r, w):
    pairs = list(pairs)
    P.dma(eng, chan, lambda e: [e.dma_start(out=o, in_=i) for (o, i) in pairs], reads=r, writes=w, n=len(pairs))


class Ctx:
    pass


def build(stages=("a0", "m0", "a1", "m1"), S=4096, debug=None):
    NT = S // 128
    NG = S // 512
    nc = bass.Bass("TRN2", target_bir_lowering=False)
    P = Prog(nc)
    C = Ctx()
    C.S, C.NT, C.NG = S, NT, NG

    def din(name, shape, dt=F32):
        return nc.dram_tensor(name, list(shape), dt, kind="ExternalInput").ap()

    def dscr(name, shape, dt):
        return nc.dram_tensor(name, list(shape), dt, kind="Internal").ap()

    dr = {}
    dr["x"] = din("x", [S, D])
    dr["y"] = nc.dram_tensor("y", [S, D], F32, kind="ExternalOutput").ap()
    dr["gb"] = din("gb", [5, 128, D])
    dr["gqkv"] = din("gqkv", [128, 512])
    dr["tb"] = din("tb", [128, H, 256])
    dr["b31"] = din("b31", [128, H])
    dr["wqkv"] = din("wqkv", [D, 3 * D])
    dr["wo0"] = din("wo0", [D, D])
    dr["win"] = din("win", [D, 584])
    dr["wuq"] = din("wuq", [256, D])
    dr["wqi"] = din("wqi", [256, 512])
    dr["wukT"] = din("wukT", [256, D])
    dr["wuv"] = din("wuv", [256, D])
    dr["wo1"] = din("wo1", [D, D])
    dr["wup0"] = din("wup0", [D, DFF])
    dr["wup1"] = din("wup1", [D, DFF])
    dr["wdn0"] = din("wdn0", [DFF, D])
    dr["wdn1"] = din("wdn1", [DFF, D])
    dr["qT"] = dscr("qT", [8, 128, S], BF16)
    dr["kT"] = dscr("kT", [8, 128, S], BF16)
    dr["va"] = dscr("va", [H, 128, NT, 128], BF16)
    dr["qiT"] = dscr("qiT", [4, 128, S], BF16)
    xs = [dr["x"]]
    for i, stg in enumerate(stages):
        if i == len(stages) - 1:
            xs.append(dr["y"])
        else:
            xs.append(dscr("xs%d" % i, [S, D], F32))

    C.pb = [P.ps("pb%d" % i, [128, 512], F32) for i in range(8)]
    A = Arena(P, 200)
    C.A = A
    C.ident = A.alloc([128], BF16)
    C.identB = A.alloc([128], BF16)
    C.io = A.alloc([128], F32)
    C.ssb = A.alloc([64], F32)
    C.ksum = A.alloc([8, 16], F32)
    P.op("pool", lambda e: e.iota(C.io, pattern=[[1, 128]], base=0, channel_multiplier=-1,
                                  allow_small_or_imprecise_dtypes=True), writes=["io"])
    TS(P, "dve", C.ident, C.io, 0.0, None, ALU.is_equal, None, ["io"], ["ident"])
    TS(P, "dve", C.identB, C.io, 0.0, BIG, ALU.is_equal, ALU.mult, ["io"], ["identB"])
    base_off = A.off
    P.barrier()

    for i, stg in enumerate(stages):
        A.off = base_off
        last = (i == len(stages) - 1)
        if stg == "a0":
            attn_phaseA(P, C, dr, xs[i], 0)
            P.barrier()
            A.off = base_off
            if debug is not None and debug.startswith("A"):
                dbg_copy(P, C, dr, xs[i], xs[i + 1])
            else:
                attn_phaseB(P, C, dr, xs[i], xs[i + 1], 0, debug)
        elif stg == "a1":
            C.kiTz = [A.alloc([S], BF16) for _ in range(2)]
            C.wabs = A.alloc([NT, 8], F32)
            C.wsgn = A.alloc([NT, 8], F32)
            base1 = A.off
            attn_phaseA(P, C, dr, xs[i], 1)
            P.barrier()
            A.off = base1
            attn_phaseB(P, C, dr, xs[i], xs[i + 1], 1)
        elif stg == "m0":
            mlp_phase(P, C, dr, xs[i], xs[i + 1], 0, False)
        elif stg == "m1":
            mlp_phase(P, C, dr, xs[i], xs[i + 1], 1, True)
        P.barrier()
    P.emit()
    return nc


def load_w_cast(P, dst3, src, nk, ncols, name, c0=0):
    pairs = []
    step = 2048
    for k in range(nk):
        for cc in range(0, ncols, step):
            w = min(step, ncols - cc)
            pairs.append((dst3[:, k, cc:cc + w], src[k * 128:(k + 1) * 128, c0 + cc:c0 + cc + w]))
    DMAN(P, "pool", "w_" + name, pairs, [], [name])


class Normer:
    def __init__(self, P, C, gb_dram_row, nbuf=3, tag="n"):
        A = C.A
        self.P, self.C = P, C
        self.gb = A.alloc([D], F32)
        DMA(P, "sp", "gbld", self.gb, gb_dram_row, [], ["gb"])
        self.nbuf = nbuf
        self.xt = [A.alloc([D], F32) for _ in range(nbuf)]
        self.xn = [A.alloc([D], BF16) for _ in range(2)]
        self.junk = A.alloc([D], BF16)
        self.cnt = 0

    def load(self, src_tile):
        i = self.cnt % self.nbuf
        DMA(self.P, "sp", "xt%d" % i, self.xt[i], src_tile, [], ["xt%d" % i])
        return i

    def norm_T(self, i, dstT, c0, dst_name, evac_eng="act"):
        P, C = self.P, self.C
        k = self.cnt
        self.cnt += 1
        xt = self.xt[i]
        xn = self.xn[k % 2]
        xnn = "xn%d" % (k % 2)
        ss = C.ssb[:, (k % 8) * 2:(k % 8) * 2 + 1]
        rs = C.ssb[:, (k % 8) * 2 + 1:(k % 8) * 2 + 2]
        sn = "ss%d" % (k % 8)
        ACT(P, self.junk, xt, AF.Square, ["xt%d" % i], ["junk", sn], accum=ss)
        with P.strict():
            ACT(P, rs, ss, AF.Ln, [sn], [sn + "r"], scale=1.0 / D, bias=EPS)
            ACT(P, rs, rs, AF.Exp, [sn + "r"], [sn + "r"], scale=-0.5)
        STT(P, xn, xt, rs, self.gb, ALU.mult, ALU.mult, ["xt%d" % i, sn + "r", "gb"], [xnn])
        pbi = k % 2
        pbv = C.pb[pbi][:].bitcast(BF16)
        for kc in range(8):
            TR(P, pbv[:, kc * 128:(kc + 1) * 128], xn[:, kc * 128:(kc + 1) * 128], C.ident, [xnn, "ident"], ["pb%d" % pbi])
        COPY(P, evac_eng, dstT[:, :, c0:c0 + 128], pbv.rearrange("p (a b) -> p a b", a=8), ["pb%d" % pbi], [dst_name])


def attn_phaseA(P, C, dr, xin, layer):
    A = C.A
    S, NT, NG = C.S, C.NT, C.NG
    nm = Normer(P, C, dr["gb"][2 * layer], nbuf=3)
    hnT = [A.alloc([8, 512], BF16) for _ in range(2)]
    qst = A.alloc([8, 512], BF16)
    kst = A.alloc([8, 512], BF16)
    vst = [A.alloc([8, 2, 128], BF16) for _ in range(2)]
    for b in range(2):
        P.op("pool", (lambda e, b=b: e.memset(vst[b], 1.0)), writes=["vst%d" % b])
    if layer == 0:
        wq = A.alloc([8, D], BF16)
        wk = A.alloc([8, D], BF16)
        wv = A.alloc([8, D], BF16)
        load_w_cast(P, wq, dr["wqkv"], 8, D, "wq", 0)
        load_w_cast(P, wk, dr["wqkv"], 8, D, "wk", D)
        load_w_cast(P, wv, dr["wqkv"], 8, D, "wv", 2 * D)
        P.op("dve", lambda e: e.memset(C.ksum, 0.0), writes=["ksum"])
    else:
        win = A.alloc([8, 584], BF16)
        wuq = A.alloc([2, D], BF16)
        wqi = A.alloc([2, 512], BF16)
        wuk = A.alloc([2, D], BF16)
        wuv = A.alloc([2, D], BF16)
        load_w_cast(P, win, dr["win"], 8, 584, "win")
        load_w_cast(P, wuq, dr["wuq"], 2, D, "wuq")
        load_w_cast(P, wqi, dr["wqi"], 2, 512, "wqi")
        load_w_cast(P, wuk, dr["wukT"], 2, D, "wuk")
        load_w_cast(P, wuv, dr["wuv"], 2, D, "wuv")
        gq = A.alloc([512], F32)
        DMA(P, "sp", "gqld", gq, dr["gqkv"], [], ["gq"])
        cn = [A.alloc([512], BF16) for _ in range(2)]
        ki2 = [A.alloc([2, 128], BF16) for _ in range(2)]
        for b in range(2):
            P.op("pool", (lambda e, b=b: e.memset(ki2[b], 0.0)), writes=["ki2%d" % b])
        cnT = [A.alloc([4, 512], BF16) for _ in range(2)]
        qist = A.alloc([4, 512], BF16)
        junk2 = A.alloc([256], BF16)
    rot = [2, 3, 4, 5, 6, 7]
    rc = [0]

    def nextbank():
        b = rot[rc[0] % len(rot)]
        rc[0] += 1
        return b

    xtiles = xin.rearrange("(t p) d -> t p d", p=128)
    for g in range(NG):
        hb = g % 2
        hT = hnT[hb]
        hname = "hnT%d" % hb
        for j in range(4):
            t = g * 4 + j
            i = nm.load(xtiles[t])
            nm.norm_T(i, hT, j * 128, hname, evac_eng="act" if layer == 0 else "dve")
        if layer == 0:
            for which, wmat, stg, sname in (("q", wq, qst, "qst"), ("k", wk, kst, "kst")):
                for hp in range(8):
                    b = nextbank()
                    for kc in range(8):
                        MM(P, C.pb[b][:, :], wmat[:, kc, hp * 128:(hp + 1) * 128], hT[:, kc, :], kc == 0, kc == 7,
                           ["w" + which, hname], ["pb%d" % b])
                    if which == "q":
                        ACT(P, stg[:, hp, :], C.pb[b][:, :], AF.Identity, ["pb%d" % b], [sname], scale=0.125)
                    else:
                        COPY(P, "act", stg[:, hp, :], C.pb[b][:, :], ["pb%d" % b], [sname])
                        RED(P, C.ksum[:, hp, 2 * g:2 * g + 2], C.pb[b][:, :].rearrange("p (a b) -> p a b", a=2), ALU.add,
                            ["pb%d" % b], ["ksum"])
                dst = dr["qT" if which == "q" else "kT"][:, :, g * 512:(g + 1) * 512].rearrange("h p t -> p h t")
                DMA(P, "sp", sname, dst, stg, [sname], [])
        else:
            for j in range(4):
                t = g * 4 + j
                tc = slice(j * 128, (j + 1) * 128)
                ba = nextbank()
                bb = nextbank()
                for kc in range(8):
                    MM(P, C.pb[ba][:, :], hT[:, kc, tc], win[:, kc, 0:512], kc == 0, kc == 7, [hname, "win"], ["pb%d" % ba])
                for kc in range(8):
                    MM(P, C.pb[bb][:, 0:72], hT[:, kc, tc], win[:, kc, 512:584], kc == 0, kc == 7, [hname, "win"], ["pb%d" % bb])
                cb = t % 2
                ssq = C.ssb[:, 32 + cb * 4:32 + cb * 4 + 1]
                ssk = C.ssb[:, 32 + cb * 4 + 1:32 + cb * 4 + 2]
                rsq = C.ssb[:, 32 + cb * 4 + 2:32 + cb * 4 + 3]
                rsk = C.ssb[:, 32 + cb * 4 + 3:32 + cb * 4 + 4]
                sn = "cs%d" % cb
                ACT(P, junk2, C.pb[ba][:, 0:256], AF.Square, ["pb%d" % ba], ["junk2", sn], accum=ssq)
                ACT(P, junk2, C.pb[ba][:, 256:512], AF.Square, ["pb%d" % ba], ["junk2", sn], accum=ssk)
                with P.strict():
                    ACT(P, rsq, ssq, AF.Ln, [sn], [sn + "q"], scale=1.0 / 256, bias=EPS)
                    ACT(P, rsq, rsq, AF.Exp, [sn + "q"], [sn + "q"], scale=-0.5)
                    ACT(P, rsk, ssk, AF.Ln, [sn], [sn + "k"], scale=1.0 / 256, bias=EPS)
                    ACT(P, rsk, rsk, AF.Exp, [sn + "k"], [sn + "k"], scale=-0.5)
                cname = "cn%d" % cb
                STT(P, cn[cb][:, 0:256], C.pb[ba][:, 0:256], rsq, gq[:, 0:256], ALU.mult, ALU.mult,
                    ["pb%d" % ba, sn + "q", "gq"], [cname])
                STT(P, cn[cb][:, 256:512], C.pb[ba][:, 256:512], rsk, gq[:, 256:512], ALU.mult, ALU.mult,
                    ["pb%d" % ba, sn + "k", "gq"], [cname])
                kname = "ki2%d" % cb
                COPY(P, "act", ki2[cb][:, 0, 0:64], C.pb[bb][:, 0:64], ["pb%d" % bb], [kname])
                COPY(P, "act", ki2[cb][:, 1, 64:128], C.pb[bb][:, 0:64], ["pb%d" % bb], [kname])
                TS(P, "dve", C.wabs[:, t, :], C.pb[bb][:, 64:72], (8.0 ** -0.5) * (64.0 ** -0.5), None, ALU.mult, None,
                   ["pb%d" % bb], ["wabs"])
                pbv = C.pb[cb][:].bitcast(BF16)
                for q4 in range(4):
                    TR(P, pbv[:, q4 * 128:(q4 + 1) * 128], cn[cb][:, q4 * 128:(q4 + 1) * 128], C.ident, [cname, "ident"], ["pb%d" % cb])
                TR(P, pbv[:, 512:640], ki2[cb][:, 0, :], C.ident, [kname, "ident"], ["pb%d" % cb])
                TR(P, pbv[:, 640:768], ki2[cb][:, 1, :], C.ident, [kname, "ident"], ["pb%d" % cb])
                COPY(P, "act", cnT[hb][:, :, tc], pbv[:, 0:512].rearrange("p (a b) -> p a b", a=4), ["pb%d" % cb], ["cnT%d" % hb])
                COPY(P, "act", C.kiTz[0][:, t * 128:(t + 1) * 128], pbv[:, 512:640], ["pb%d" % cb], ["kiT"])
                COPY(P, "act", C.kiTz[1][:, t * 128:(t + 1) * 128], pbv[:, 640:768], ["pb%d" % cb], ["kiT"])
            cT = cnT[hb]
            cTn = "cnT%d" % hb
            for hp in range(8):
                b = nextbank()
                for kc in range(2):
                    MM(P, C.pb[b][:, :], wuq[:, kc, hp * 128:(hp + 1) * 128], cT[:, kc, :], kc == 0, kc == 1, ["wuq", cTn], ["pb%d" % b])
                ACT(P, qst[:, hp, :], C.pb[b][:, :], AF.Identity, ["pb%d" % b], ["qst"], scale=0.125)
            DMA(P, "sp", "qst", dr["qT"][:, :, g * 512:(g + 1) * 512].rearrange("h p t -> p h t"), qst, ["qst"], [])
            for ch in range(4):
                b = nextbank()
                for kc in range(2):
                    MM(P, C.pb[b][:, :], wqi[:, kc, ch * 128:(ch + 1) * 128], cT[:, kc, :], kc == 0, kc == 1, ["wqi", cTn], ["pb%d" % b])
                COPY(P, "dve", qist[:, ch, :], C.pb[b][:, :], ["pb%d" % b], ["qist"])
            DMA(P, "sp", "qist", dr["qiT"][:, :, g * 512:(g + 1) * 512].rearrange("h p t -> p h t"), qist, ["qist"], [])
            for hp in range(8):
                b = nextbank()
                for kc in range(2):
                    MM(P, C.pb[b][:, :], wuk[:, kc, hp * 128:(hp + 1) * 128], cT[:, 2 + kc, :], kc == 0, kc == 1, ["wuk", cTn], ["pb%d" % b])
                COPY(P, "act", kst[:, hp, :], C.pb[b][:, :], ["pb%d" % b], ["kst"])
            DMA(P, "sp", "kst", dr["kT"][:, :, g * 512:(g + 1) * 512].rearrange("h p t -> p h t"), kst, ["kst"], [])
        for j in range(4):
            t = g * 4 + j
            tc = slice(j * 128, (j + 1) * 128)
            vb = t % 2
            vname = "vst%d" % vb
            for half in range(2):
                b = nextbank()
                if layer == 0:
                    for kc in range(8):
                        MM(P, C.pb[b][:, :], hT[:, kc, tc], wv[:, kc, half * 512:(half + 1) * 512], kc == 0, kc == 7, [hname, "wv"], ["pb%d" % b])
                else:
                    for kc in range(2):
                        MM(P, C.pb[b][:, :], cT[:, 2 + kc, tc], wuv[:, kc, half * 512:(half + 1) * 512], kc == 0, kc == 1, [cTn, "wuv"], ["pb%d" % b])
                psv = C.pb[b][:, :].rearrange("p (a b c) -> p a b c", a=4, b=2)
                COPY(P, "dve", vst[vb][:, half * 4:(half + 1) * 4, 0, 0:64], psv[:, :, 0, :], ["pb%d" % b], [vname])
                COPY(P, "dve", vst[vb][:, half * 4:(half + 1) * 4, 1, 64:128], psv[:, :, 1, :], ["pb%d" % b], [vname])
            dst = dr["va"][:, :, t, :].rearrange("(a b) p d -> p a b d", b=2)
            DMA(P, "sp", vname, dst, vst[vb], [vname], [])


def dbg_copy(P, C, dr, xin, xout):
    A = C.A
    xt = A.alloc([D], F32)
    xin_t = xin.rearrange("(t p) d -> t p d", p=128)
    xout_t = xout.rearrange("(t p) d -> t p d", p=128)
    for t in range(C.NT):
        DMA(P, "sp", "dbgl", xt, xin_t[t], [], ["dbgx"])
        DMA(P, "pool", "dbgs", xout_t[t], xt, ["dbgx"], [])


SALL = ["score%d" % i_ for i_ in range(8)]


def attn_phaseB(P, C, dr, xin, xout, layer, debug=None):
    A = C.A
    S, NT, NG = C.S, C.NT, C.NG
    wo = A.alloc([8, D], BF16)
    load_w_cast(P, wo, dr["wo0" if layer == 0 else "wo1"], 8, D, "wo")
    if layer == 1:
        score = A.alloc([max(S, H * 256)], F32)
        tbf = score[:, 0:H * 256].rearrange("p (a b) -> p a b", a=H)
        tbn = "score0"
    else:
        tbf = A.alloc([H, 256], F32)
        tbn = "tbf"
    b31 = A.alloc([H], F32)
    cm0 = A.alloc([128], F32)
    TBp = A.alloc([H, 256], BF16)
    DMA(P, "sp", "tbld", tbf, dr["tb"], [], SALL if layer == 1 else [tbn])
    DMA(P, "sp", "b31ld", b31, dr["b31"], [], ["b31"])
    TS(P, "dve", cm0, C.io, 0.0, -BIG, ALU.is_lt, ALU.mult, ["io"], ["cm0"])
    for h in range(H):
        STT(P, TBp[:, h, 0:128], tbf[:, h, 0:128], b31[:, h:h + 1], cm0, ALU.subtract, ALU.add, (SALL if layer == 1 else [tbn]) + ["b31", "cm0"], ["TBp"])
        TS(P, "dve", TBp[:, h, 128:256], tbf[:, h, 128:256], b31[:, h:h + 1], None, ALU.subtract, None, (SALL if layer == 1 else [tbn]) + ["b31"], ["TBp"])
    nqb = 2
    qz = [[A.alloc([8, 512], BF16) for _ in range(2)] for _ in range(nqb)]
    for b in range(2):
        P.op("pool", (lambda e, b=b: e.memset(qz[b][0][64:128], 0.0)), writes=["qg%d" % b])
        P.op("pool", (lambda e, b=b: e.memset(qz[b][1][0:64], 0.0)), writes=["qg%d" % b])
    kbuf = [A.alloc([S], BF16) for _ in range(2)]
    vbuf = [A.alloc([NT, 128], BF16) for _ in range(2)]
    pbuf = [A.alloc([512], BF16) for _ in range(3)]
    oT = A.alloc([8, 512], BF16)
    rec = A.alloc([512], F32)
    xt = [A.alloc([D], F32) for _ in range(2)]
    if layer == 0:
        kbd = A.alloc([8, 32], BF16)
        nidx = A.alloc([256], F32)
        pneg = A.alloc([256], F32)
        ownm1 = A.alloc([256], F32)
        gm = A.alloc([256], F32)
        gm2 = A.alloc([256], F32)
        tmpg = A.alloc([256], F32)
        mx = A.alloc([16], F32)
        selv = A.alloc([256], BF16)
        selT2 = [A.alloc([H, 512], BF16) for _ in range(2)]
        Esel = A.alloc([16, 128], BF16)
        iot = A.alloc([16, 128], F32)
        for b_ in range(2):
            P.op("pool", (lambda e, b_=b_: e.memset(selT2[b_], 0.0)), writes=["selT%d" % b_])
        P.op("dve", lambda e: e.memset(kbd, 0.0), writes=["kbd"])
        COPY(P, "dve", kbd[0:64, :, 0:16], C.ksum[0:64, :, :], ["ksum", "kbd"], ["kbd"])
        COPY(P, "dve", kbd[64:128, :, 16:32], C.ksum[64:128, :, :], ["ksum", "kbd"], ["kbd"])
        P.op("pool", lambda e: e.iota(nidx.rearrange("p (a b) -> p a b", a=16), pattern=[[0, 16], [1, 16]], base=0,
                                      channel_multiplier=0, allow_small_or_imprecise_dtypes=True), writes=["nidx"])
        P.op("pool", lambda e: e.iota(iot, pattern=[[1, 16], [0, 128]], base=0, channel_multiplier=-1,
                                      allow_small_or_imprecise_dtypes=True), writes=["iot"])
        TS(P, "dve", Esel, iot, 0.0, BIG, ALU.is_equal, ALU.mult, ["iot"], ["Esel"])
    else:
        qig = A.alloc([4, 512], BF16)
        madd = A.alloc([S], BF16)
        junkb = madd
        maskT = A.alloc([NT, 512], BF16)
        cneg = A.alloc([128], F32)
        cpos = A.alloc([128], F32)
        dtmp = A.alloc([128], F32)
        bs = A.alloc([16], F32)
        bsi = A.alloc([4], I32)
        pw = A.alloc([NBIS + 1], F32)
        sk = A.alloc([NBIS + 1], F32)
        for k_ in range(NBIS + 1):
            P.op("dve", (lambda e, k_=k_: e.memset(pw[:, k_:k_ + 1], 2.0 ** -(k_ + 1))), writes=["pw"])
        TS(P, "dve", cneg, C.io, 0.0, NEGF, ALU.is_gt, ALU.mult, ["io"], ["cneg"])
        TS(P, "dve", cpos, C.io, 0.0, -NEGF, ALU.is_gt, ALU.mult, ["io"], ["cpos"])
    xin_t = xin.rearrange("(t p) d -> t p d", p=128)
    xout_t = xout.rearrange("(t p) d -> t p d", p=128)
    lrot = [2, 3, 4]
    lc = [0]
    ucnt = [0]
    def load_q(gq):
        qb_ = gq % nqb
        DMAN(P, "sp", "qg%d" % qb_, [(qz[qb_][0][0:64], dr["qT"][:, 0:64, gq * 512:(gq + 1) * 512].rearrange("h p t -> p h t")),
                                    (qz[qb_][1][64:128], dr["qT"][:, 64:128, gq * 512:(gq + 1) * 512].rearrange("h p t -> p h t"))],
             [], ["qg%d" % qb_])

    def gate_a(gq, j):
        t = gq * 4 + j
        blk = t // 2
        QG = qz[gq % nqb]
        qn_ = "qg%d" % (gq % nqb)
        if t % 2 == 0:
            TS(P, "dve", pneg, nidx, float(blk), NEGF, ALU.is_ge, ALU.mult, ["nidx"], ["pneg"])
            TS(P, "dve", ownm1, nidx, float(blk), -1.0, ALU.is_ge, ALU.add, ["nidx"], ["ownm1"])
        for hp in range(8):
            for h2 in range(2):
                MM(P, C.pb[0][:, hp * 32:(hp + 1) * 32], QG[h2][:, hp, j * 128:(j + 1) * 128], kbd[:, hp, :], h2 == 0, h2 == 1,
                   [qn_, "kbd"], ["pb0"])
        TT(P, "dve", gm, C.pb[0][:, 0:256], pneg, ALU.add, ["pb0", "pneg"], ["gm"])
        g3 = gm.rearrange("p (a b) -> p a b", a=16)
        g23 = gm2.rearrange("p (a b) -> p a b", a=16)
        t3 = tmpg.rearrange("p (a b) -> p a b", a=16)
        mb = bcast_last(mx, 16)
        RED(P, mx, g3, ALU.max, ["gm"], ["mx"])
        TT(P, "dve", t3, g3, mb, ALU.is_ge, ["gm", "mx"], ["tmpg"])
        STT(P, gm2, tmpg, NEGF, gm, ALU.mult, ALU.add, ["tmpg", "gm"], ["gm2"])
        RED(P, mx, g23, ALU.max, ["gm2"], ["mx"])
        TT(P, "dve", t3, g23, mb, ALU.is_ge, ["gm2", "mx"], ["tmpg"])
        STT(P, gm2, tmpg, NEGF, gm2, ALU.mult, ALU.add, ["tmpg", "gm2"], ["gm2"])
        RED(P, mx, g23, ALU.max, ["gm2"], ["mx"])
        TT(P, "dve", t3, g3, mb, ALU.is_ge, ["gm", "mx"], ["tmpg"])
        STT(P, selv, tmpg, -1.0, ownm1, ALU.add, ALU.max, ["tmpg", "ownm1"], ["selv"])

    def gate_b(gq, j):
        sT = selT2[gq % 2]
        pv1 = C.pb[1][:].bitcast(BF16)
        for rnd in range(2):
            for hh in range(8):
                h = rnd * 8 + hh
                TR(P, pv1[0:16, hh * 128:(hh + 1) * 128], selv[:, h * 16:(h + 1) * 16], C.ident, ["selv", "ident"], ["pb1"])
            COPY(P, "act", sT[0:16, rnd * 8:(rnd + 1) * 8, j * 128:(j + 1) * 128],
                 pv1[0:16, :].rearrange("p (a b) -> p a b", a=8), ["pb1"], ["selT%d" % (gq % 2)])

    load_q(0)
    if layer == 0:
        for j in range(4):
            gate_a(0, j)
            gate_b(0, j)
    for g in range(NG):
        qb = g % nqb
        qname = "qg%d" % qb
        QZ = qz[qb]
        nch = 4 * (g + 1)
        if g + 1 < NG:
            load_q(g + 1)
        if layer == 0:
            selT = selT2[g % 2]
            sTn = "selT%d" % (g % 2)
        else:
            DMA(P, "sp", "qig", qig, dr["qiT"][:, :, g * 512:(g + 1) * 512].rearrange("h p t -> p h t"), [], ["qig"])
            for j in range(4):
                t = g * 4 + j
                nk = 128 * (t + 1)
                qc = slice(j * 128, (j + 1) * 128)
                nsg = (nk + 511) // 512
                for hi in range(8):
                    ch = hi // 2
                    for sg in range(nsg):
                        ncol = min(512, nk - sg * 512)
                        sc = slice(sg * 512, sg * 512 + ncol)
                        bm = (0, 1, 2)[ucnt[0] % 3]
                        br = (3, 4, 6, 7)[ucnt[0] % 4]
                        ucnt[0] += 1
                        sn_ = "score%d" % sg
                        MM(P, C.pb[bm][:, 0:ncol], qig[:, ch, qc], C.kiTz[hi % 2][:, sc], True, True, ["qig", "kiT"], ["pb%d" % bm])
                        ACT(P, C.pb[br][:, 0:ncol], C.pb[bm][:, 0:ncol], AF.Relu, ["pb%d" % bm], ["pb%d" % br])
                        if hi == 0:
                            TS(P, "dve", score[:, sc], C.pb[br][:, 0:ncol], C.wabs[:, t, 0:1], None, ALU.mult, None,
                               ["pb%d" % br, "wabs"], [sn_])
                        else:
                            STT(P, score[:, sc], C.pb[br][:, 0:ncol], C.wabs[:, t, hi:hi + 1], score[:, sc], ALU.mult, ALU.add,
                                ["pb%d" % br, "wabs", sn_], [sn_])
                dg = slice(128 * t, 128 * t + 128)
                lo, hi_, rg, mid, cnt, m1 = (bs[:, k:k + 1] for k in range(6))
                fl = bsi[:, 0:1]
                P.strict_default = True
                TT(P, "dve", dtmp, score[:, dg], cpos, ALU.add, SALL + ["cpos"], ["dtmp"])
                RED(P, lo, dtmp, ALU.min, ["dtmp"], ["bs"])
                if t > 0:
                    RED(P, m1, score[:, 0:128 * t], ALU.min, SALL, ["bs"])
                    TT(P, "dve", lo, lo, m1, ALU.min, ["bs"], ["bs"])
                TT(P, "dve", score[:, dg], score[:, dg], cneg, ALU.add, SALL + ["cneg"], SALL)
                if nk > 256:
                    negS, tq = bs[:, 6:7], bs[:, 7:8]
                    RED(P, hi_, score[:, 0:nk], ALU.max, SALL, ["bs"])
                    TT(P, "dve", rg, hi_, lo, ALU.subtract, ["bs"], ["bs"])
                    TS(P, "dve", sk, pw, rg, None, ALU.mult, None, ["pw", "bs"], ["sk"])
                    TT(P, "dve", mid, lo, sk[:, 0:1], ALU.add, ["bs", "sk"], ["mid"])
                    split = nk >= 1536
                    n1 = (int(nk * 0.49) // 128) * 128 if split else nk
                    thr_c = 256.0 - (nk - n1) / 2.0 - (0.25 if split else 0.0)
                    for it in range(NBIS):
                        if split:
                            ACT(P, madd[:, n1:nk], score[:, n1:nk], AF.Sign, SALL + ["mid"], ["maddA", "negS"],
                                scale=-1.0, bias=mid, accum=negS)
                        TS(P, "dve", madd[:, 0:n1], score[:, 0:n1], mid, None, ALU.is_ge, ALU.add, SALL + ["mid"], ["maddD", "cnt"], accum=cnt)
                        if split:
                            TS(P, "dve", cnt, negS, -0.5, cnt, ALU.mult, ALU.add, ["negS", "cnt"], ["cnt"])
                        STT(P, tq, cnt, thr_c, sk[:, it:it + 1], ALU.is_ge, ALU.mult, ["cnt", "sk"], ["tq"])
                        STT(P, mid, tq, sk[:, it + 1:it + 2], mid, ALU.subtract, ALU.add, ["tq", "sk", "mid"], ["mid"])
                    TT(P, "dve", lo, mid, sk[:, NBIS:NBIS + 1], ALU.subtract, ["mid", "sk"], ["bs"])
                TS(P, "dve", madd[:, 0:nk], score[:, 0:nk], lo, 1.0, ALU.is_ge, ALU.subtract, SALL + ["bs"], ["madd", "maddA", "maddD"])
                P.strict_default = False
                for c0 in range(0, t + 1, 8):
                    cn_ = min(8, t + 1 - c0)
                    bk_ = 5 + ((c0 // 8) % 2)
                    pv7 = C.pb[bk_][:].bitcast(BF16)
                    for cc in range(cn_):
                        c = c0 + cc
                        TR(P, pv7[:, cc * 128:(cc + 1) * 128], madd[:, c * 128:(c + 1) * 128], C.ident, ["madd", "maddA", "maddD", "ident"], ["pb%d" % bk_])
                    COPY(P, "act", maskT[:, c0:c0 + cn_, qc], pv7[:, 0:cn_ * 128].rearrange("p (a b) -> p a b", a=cn_), ["pb%d" % bk_], ["maskT"])
        items = [(h, c) for h in range(H) for c in range(nch)]
        LOOK = 3
        lbanks = [2, 3, 4, 7] if layer == 0 else [0, 1, 2, 5]

        def hinfo(h):
            hp, h2 = h // 2, h % 2
            r0 = 64 * h2
            return hp, h2, slice(r0, r0 + 64), slice(64 - r0, 128 - r0), hp % 2, h % 2

        def issue_qk(idx):
            h, c = items[idx]
            hp, h2, rs_, so, kb, vb = hinfo(h)
            kname = "kbuf%d" % kb
            if c == 0:
                if h2 == 0:
                    DMA(P, "sp", kname, kbuf[kb][:, 0:nch * 128], dr["kT"][hp, :, 0:nch * 128], [], [kname])
                DMA(P, "sp", "vbuf%d" % vb, vbuf[vb][:, 0:nch, :], dr["va"][h, :, 0:nch, :], [], ["vbuf%d" % vb])
            dc = c - 4 * g
            col0 = max(0, dc * 128)
            cs = slice(col0, 512)
            lb = lbanks[idx % 4]
            lbn = "pb%d" % lb
            psl = C.pb[lb]
            n_blk = c // 2
            need_sel = (layer == 1) or (n_blk < 2 * g + 1)
            need_tb = dc >= -1
            MM(P, psl[:, cs], kbuf[kb][:, c * 128:(c + 1) * 128], QZ[h2][:, hp, cs], True, not (need_sel or need_tb), [kname, qname], [lbn])
            if need_sel:
                if layer == 0:
                    MM(P, psl[:, cs], Esel[:, n_blk, :], selT[:, h, cs], False, not need_tb, ["Esel", sTn], [lbn])
                else:
                    MM(P, psl[:, cs], C.identB, maskT[:, c, cs], False, not need_tb, ["identB", "maskT"], [lbn])
            if need_tb:
                if dc == -1:
                    MM(P, psl[:, 0:128], C.ident, TBp[:, h, 128:256], False, True, ["ident", "TBp"], [lbn])
                elif dc == 3:
                    MM(P, psl[:, 384:512], C.ident, TBp[:, h, 0:128], False, True, ["ident", "TBp"], [lbn])
                else:
                    MM(P, psl[:, col0:col0 + 256], C.ident, TBp[:, h, 0:256], False, True, ["ident", "TBp"], [lbn])

        for idx in range(min(LOOK, len(items))):
            issue_qk(idx)
        for idx in range(len(items)):
            if idx + LOOK < len(items):
                issue_qk(idx + LOOK)
            h, c = items[idx]
            if layer == 0 and g + 1 < NG and c == 0:
                if h % 4 == 0:
                    gate_a(g + 1, h // 4)
                elif h % 4 == 2:
                    gate_b(g + 1, h // 4)
            hp, h2, rs_, so, kb, vb = hinfo(h)
            ob = 5 + (h % 2) if layer == 0 else 3 + (h % 2)
            obn = "pb%d" % ob
            pso = C.pb[ob]
            dc = c - 4 * g
            col0 = max(0, dc * 128)
            cs = slice(col0, 512)
            lb = lbanks[idx % 4]
            lbn = "pb%d" % lb
            pi = idx % 3
            pn = "pbuf%d" % pi
            ACT(P, pbuf[pi][:, cs], C.pb[lb][:, cs], AF.Exp, [lbn], [pn])
            MM(P, pso[:, cs], vbuf[vb][:, c, :], pbuf[pi][:, cs], c == 0, c == nch - 1, ["vbuf%d" % vb, pn], [obn])
            if c == nch - 1:
                P.op("dve", (lambda e, o_=rec[so, :], i_=pso[so, :]: e.reciprocal(out=o_, in_=i_)), reads=[obn], writes=["rec"])
                TT(P, "dve", oT[rs_, hp, :], pso[rs_, :], rec[so, :], ALU.mult, [obn, "rec"], ["oT"])
        for j in range(4):
            t = g * 4 + j
            xb = t % 2
            xn_ = "xta%d" % xb
            DMA(P, "sp", xn_, xt[xb], xin_t[t], [], [xn_])
            for half in range(2):
                b = 6 + half if layer == 1 else (0, 1)[half]
                bn = "pb%d" % b
                for hp in range(8):
                    MM(P, C.pb[b][:, :], oT[:, hp, j * 128:(j + 1) * 128], wo[:, hp, half * 512:(half + 1) * 512], hp == 0, hp == 7, ["oT", "wo"], [bn])
                hs = slice(half * 512, (half + 1) * 512)
                TT(P, "dve", xt[xb][:, hs], C.pb[b][:, :], xt[xb][:, hs], ALU.add, [bn, xn_], [xn_])
            DMA(P, "pool", "xst%d" % xb, xout_t[t], xt[xb], [xn_], [])


def mlp_phase(P, C, dr, xin, xout, layer, final):
    A = C.A
    S, NT = C.S, C.NT
    wup = A.alloc([8, DFF], BF16)
    wdn = A.alloc([32, D], BF16)
    load_w_cast(P, wup, dr["wup%d" % layer], 8, DFF, "wup")
    load_w_cast(P, wdn, dr["wdn%d" % layer], 32, D, "wdn")
    nm = Normer(P, C, dr["gb"][2 * layer + 1], nbuf=4)
    if final:
        fnb = A.alloc([D], F32)
        DMA(P, "sp", "fnld", fnb, dr["gb"][4], [], ["fnb"])
        junkf = A.alloc([D], BF16)
    hnT = [A.alloc([8, 256], BF16) for _ in range(2)]
    hTt = A.alloc([32, 256], BF16)
    rbuf = [A.alloc([512], BF16) for _ in range(3)]
    xin_t = xin.rearrange("(t p) d -> t p d", p=128)
    xout_t = xout.rearrange("(t p) d -> t p d", p=128)
    rcnt = [0]
    for g2 in range(NT // 2):
        hb = g2 % 2
        hname = "mhT%d" % hb
        xi = []
        for j in range(2):
            t = g2 * 2 + j
            i = nm.load(xin_t[t])
            xi.append(i)
            nm.norm_T(i, hnT[hb], j * 128, hname, evac_eng="dve")
        for f2 in range(16):
            b = 2 + (f2 % 2)
            bn = "pb%d" % b
            for q in range(2):
                ffc = f2 * 2 + q
                for kc in range(8):
                    MM(P, C.pb[b][:, q * 256:(q + 1) * 256], wup[:, kc, ffc * 128:(ffc + 1) * 128], hnT[hb][:, kc, :], kc == 0, kc == 7,
                       ["wup", hname], [bn])
            ri = rcnt[0] % 3
            rcnt[0] += 1
            rn = "rbuf%d" % ri
            ACT(P, rbuf[ri], C.pb[b][:, :], AF.Relu, [bn], [rn])
            TT(P, "dve", hTt[:, f2 * 2:f2 * 2 + 2, :], C.pb[b][:, :].rearrange("p (a b) -> p a b", a=2),
               rbuf[ri].rearrange("p (a b) -> p a b", a=2), ALU.mult, [bn, rn], ["hTt"])
        for j in range(2):
            t = g2 * 2 + j
            i = xi[j]
            xn_ = "xt%d" % i
            for half in range(2):
                b = 4 + j * 2 + half
                bn = "pb%d" % b
                for ffc in range(32):
                    MM(P, C.pb[b][:, :], hTt[:, ffc, j * 128:(j + 1) * 128], wdn[:, ffc, half * 512:(half + 1) * 512], ffc == 0, ffc == 31,
                       ["hTt", "wdn"], [bn])
                hs = slice(half * 512, (half + 1) * 512)
                TT(P, "dve", nm.xt[i][:, hs], C.pb[b][:, :], nm.xt[i][:, hs], ALU.add, [bn, xn_], [xn_])
            if final:
                ss = C.ssb[:, 48 + (t % 4) * 2:48 + (t % 4) * 2 + 1]
                rs = C.ssb[:, 48 + (t % 4) * 2 + 1:48 + (t % 4) * 2 + 2]
                sn = "fs%d" % (t % 4)
                ACT(P, junkf, nm.xt[i], AF.Square, [xn_], ["junkf", sn], accum=ss)
                with P.strict():
                    ACT(P, rs, ss, AF.Ln, [sn], [sn + "r"], scale=1.0 / D, bias=EPS)
                    ACT(P, rs, rs, AF.Exp, [sn + "r"], [sn + "r"], scale=-0.5)
                STT(P, nm.xt[i], nm.xt[i], rs, fnb, ALU.mult, ALU.mult, [xn_, sn + "r", "fnb"], [xn_])
            DMA(P, "pool", "xst%d" % i, xout_t[t], nm.xt[i], [xn_], [])


def _rel_bucket_np(dist):
    n = np.maximum(dist, 0)
    exact = 16
    nf = np.maximum(n, 1).astype(np.float32)
    large = exact + (np.log(nf / np.float32(exact)) / np.float32(math.log(128 / exact)) * np.float32(32 - exact)).astype(np.int32)
    large = np.minimum(large, 31)
    return np.where(n < exact, n, large)


def host_layout(inp):
    f = lambda a: np.ascontiguousarray(np.asarray(a, dtype=np.float32))
    rb = f(inp["rel_bias"])
    s_idx = np.arange(128)[:, None]
    q_idx = np.arange(128)[None, :]
    bk0 = _rel_bucket_np(q_idx - s_idx)
    bk1 = _rel_bucket_np(q_idx - s_idx + 128)
    tb = np.empty((128, H, 256), np.float32)
    tb[:, :, 0:128] = rb[bk0].transpose(0, 2, 1)
    tb[:, :, 128:256] = rb[bk1].transpose(0, 2, 1)
    gb = np.stack([inp["ln_attn"][0], inp["ln_mlp"][0], inp["ln_attn"][1], inp["ln_mlp"][1], inp["final_norm"]])
    gb = f(np.broadcast_to(gb[:, None, :], (5, 128, D)))
    gqkv = f(np.broadcast_to(np.concatenate([inp["dsa_g_q"][0], inp["dsa_g_kv"][0]])[None, :], (128, 512)))
    m = {
        "gb": gb, "gqkv": gqkv, "tb": f(tb), "b31": f(np.broadcast_to(rb[31][None, :], (128, H))),
        "wqkv": f(inp["moba_w_qkv"][0]), "wo0": f(inp["moba_w_o"][0]), "win": f(inp["dsa_w_in"][0]),
        "wuq": f(inp["dsa_w_uq"][0]), "wqi": f(inp["dsa_w_qi"][0]),
        "wukT": f(np.transpose(inp["dsa_w_uk"][0], (2, 0, 1)).reshape(256, D)),
        "wuv": f(np.transpose(inp["dsa_w_uv"][0], (1, 0, 2)).reshape(256, D)),
        "wo1": f(inp["dsa_w_o"][0]),
        "wup0": f(inp["mlp_w_up"][0]), "wup1": f(inp["mlp_w_up"][1]),
        "wdn0": f(inp["mlp_w_down"][0]), "wdn1": f(inp["mlp_w_down"][1]),
    }
    return m


_NC_CACHE = {}


def kernel(**inputs):
    x = np.asarray(inputs["x"], dtype=np.float32)
    B = x.shape[0]
    shared = host_layout(inputs)
    key = ("full", x.shape[1])
    if key not in _NC_CACHE:
        _NC_CACHE[key] = build(("a0", "m0", "a1", "m1"), S=x.shape[1])
    nc = _NC_CACHE[key]
    in_maps = []
    for b in range(B):
        m = dict(shared)
        m["x"] = np.ascontiguousarray(x[b])
        in_maps.append(m)
    res = run_bass_kernel_spmd(nc, in_maps, core_ids=list(range(B)))
    return np.stack([np.asarray(r["y"], dtype=np.float32) for r in res.results], axis=0)
```

```python
import math
import numpy as np
import concourse.bass as bass
import concourse.mybir as mybir
from concourse.bass_utils import run_bass_kernel_spmd
from contextlib import ExitStack

F32 = mybir.dt.float32
BF16 = mybir.dt.bfloat16
I32 = mybir.dt.int32
AF = mybir.ActivationFunctionType
ALU = mybir.AluOpType
AX = mybir.AxisListType

D = 1024
H = 16
DFF = 4096
BIG = 30000.0
NEGF = -1.0e30
EPS = 1e-6
NBIS = 16


class Op:
    __slots__ = ("eng", "fn", "deps", "needs_inc", "tok", "sem", "is_dma", "chan", "n")


class Prog:
    ENG = ("pe", "act", "dve", "pool", "sp")

    def __init__(self, nc):
        self.nc = nc
        self.ops = {e: [] for e in self.ENG}
        self.res = {}
        self.stack = ExitStack()
        self.chan_sems = {}
        self.chan_cnt = {}
        self.last = {e: None for e in self.ENG}
        self.last_dma = {}
        self.strict_default = False

    def sb(self, name, shape, dt):
        return self.stack.enter_context(self.nc.sbuf_tensor(name, list(shape), dt))

    def ps(self, name, shape, dt):
        return self.stack.enter_context(self.nc.psum_tensor(name, list(shape), dt))

    def _add(self, eng, fn, reads, writes, chan=None, n=1, strict=False):
        op = Op()
        op.eng, op.fn, op.needs_inc, op.is_dma, op.chan, op.n = eng, fn, False, chan is not None, chan, n
        deps = set()
        raw = set()
        for r in reads:
            st = self.res.get(r)
            if st is not None and st[0] is not None:
                deps.add(st[0])
                raw.add(st[0])
            if st is not None and r.startswith("pb"):
                deps.update(st[1])
        for w in writes:
            st = self.res.get(w)
            if st is not None:
                if st[0] is not None:
                    deps.add(st[0])
                deps.update(st[1])
        keep = []
        for d in deps:
            if d is op or d.fn is None:
                continue
            if (not d.is_dma) and d.eng == eng and eng == "pe":
                continue
            d.needs_inc = True
            keep.append(d)
        op.deps = keep
        for r in reads:
            lst = self.res.setdefault(r, [None, []])[1]
            if not op.is_dma:
                lst[:] = [o for o in lst if o.is_dma or o.eng != eng]
            lst.append(op)
        for w in writes:
            self.res[w] = [op, []]
        self.ops[eng].append(op)
        if op.is_dma:
            self.last_dma[chan] = op
        else:
            self.last[eng] = op
        return op

    def op(self, eng, fn, reads=(), writes=(), strict=None):
        if strict is None:
            strict = self.strict_default
        return self._add(eng, fn, reads, writes, strict=strict)

    def dma(self, eng, chan, fn, reads=(), writes=(), n=1):
        return self._add(eng, fn, reads, writes, chan=chan, n=n)

    def strict(self):
        P = self

        class _S:
            def __enter__(self_):
                self_.old = P.strict_default
                P.strict_default = True

            def __exit__(self_, *a):
                P.strict_default = self_.old
        return _S()

    def barrier(self):
        lasts = [o for o in self.last.values() if o is not None]
        dmas = list(self.last_dma.values())
        for e in self.ENG:
            op = Op()
            op.eng, op.fn, op.needs_inc, op.is_dma, op.chan, op.n = e, None, False, False, None, 0
            op.deps = []
            for d in lasts:
                if d.eng != e:
                    d.needs_inc = True
                    op.deps.append(d)
            for d in dmas:
                op.deps.append(d)
            self.ops[e].append(op)
        self.res = {}

    def emit(self):
        nc = self.nc
        st = self.stack
        esem = {e: st.enter_context(nc.semaphore("sem_" + e)) for e in self.ENG}
        for e in self.ENG:
            for op in self.ops[e]:
                if op.is_dma and op.chan not in self.chan_sems:
                    self.chan_sems[op.chan] = st.enter_context(nc.semaphore("ch_" + op.chan))
                    self.chan_cnt[op.chan] = 0
        for e in self.ENG:
            c = 0
            for op in self.ops[e]:
                if op.is_dma:
                    self.chan_cnt[op.chan] += 16 * op.n
                    op.sem = self.chan_sems[op.chan]
                    op.tok = self.chan_cnt[op.chan]
                else:
                    if op.needs_inc:
                        c += 1
                    op.sem = esem[e]
                    op.tok = c
        finals = [(s, self.chan_cnt[ch]) for ch, s in self.chan_sems.items()]
        block = st.enter_context(nc.Block())

        def body_for(e):
            def body(eng):
                known = {}
                for op in self.ops[e]:
                    need = {}
                    for d in op.deps:
                        k = id(d.sem)
                        if k not in need or need[k][1] < d.tok:
                            need[k] = (d.sem, d.tok)
                    for k, (s, v) in need.items():
                        if known.get(k, 0) >= v:
                            continue
                        eng.wait_ge(s, v)
                        known[k] = v
                    if op.fn is None:
                        continue
                    ins = op.fn(eng)
                    if op.is_dma:
                        if op.n == 1:
                            ins.then_inc(op.sem, 16)
                        else:
                            for i_ in ins:
                                i_.then_inc(op.sem, 16)
                    elif op.needs_inc:
                        ins.then_inc(op.sem, 1)
                if e == "sp":
                    for s, v in finals:
                        eng.wait_ge(s, v)
            return body

        block.tensor(body_for("pe"))
        block.scalar(body_for("act"))
        block.vector(body_for("dve"))
        block.gpsimd(body_for("pool"))
        block.sync(body_for("sp"))
        st.close()


class Arena:
    def __init__(self, P, kbytes):
        self.t = P.sb("arena", [128, kbytes * 256], F32)
        self.cap = kbytes * 1024
        self.off = 0

    def alloc(self, free_shape, dt):
        n = 1
        for s in free_shape:
            n *= s
        nbytes = n * mybir.dt.size(dt)
        nbytes = (nbytes + 63) // 64 * 64
        assert self.off + nbytes <= self.cap, ("arena overflow", self.off, nbytes, self.cap)
        a = self.t[:, self.off // 4:(self.off + nbytes) // 4]
        self.off += nbytes
        if dt != F32:
            a = a.bitcast(dt)
        a = a[:, 0:n]
        if len(free_shape) == 2:
            a = a.rearrange("p (a b) -> p a b", a=free_shape[0])
        elif len(free_shape) == 3:
            a = a.rearrange("p (a b c) -> p a b c", a=free_shape[0], b=free_shape[1])
        return a


def bcast_last(ap2d, n):
    return bass.AP(ap2d.tensor, ap2d.offset, [list(ap2d.ap[0]), list(ap2d.ap[1]), [0, n]])


def MM(P, out, lhsT, rhs, start, stop, r, w):
    P.op("pe", lambda e: e.matmul(out, lhsT=lhsT, rhs=rhs, start=start, stop=stop), reads=r, writes=w)


def TR(P, out, in_, ident, r, w):
    P.op("pe", lambda e: e.transpose(out, in_, ident), reads=r, writes=w)


def ACT(P, out, in_, func, r, w, scale=None, bias=None, accum=None):
    kw = {}
    if scale is not None:
        kw["scale"] = scale
    if bias is not None:
        kw["bias"] = bias
    if accum is not None:
        kw["accum_out"] = accum
    P.op("act", lambda e: e.activation(out=out, in_=in_, func=func, **kw), reads=r, writes=w)


def TS(P, eng, out, in0, s1, s2, op0, op1, r, w, accum=None):
    kw = {}
    if op1 is not None:
        kw["op1"] = op1
    if accum is not None:
        kw["accum_out"] = accum
    P.op(eng, lambda e: e.tensor_scalar(out=out, in0=in0, scalar1=s1, scalar2=s2, op0=op0, **kw), reads=r, writes=w)


def TT(P, eng, out, in0, in1, op, r, w):
    P.op(eng, lambda e: e.tensor_tensor(out=out, in0=in0, in1=in1, op=op), reads=r, writes=w)


def STT(P, out, in0, scalar, in1, op0, op1, r, w):
    P.op("dve", lambda e: e.scalar_tensor_tensor(out=out, in0=in0, scalar=scalar, in1=in1, op0=op0, op1=op1), reads=r, writes=w)


SKIP_RED = [False]


def RED(P, out, in_, op, r, w):
    if SKIP_RED[0]:
        return
    P.op("dve", lambda e: e.tensor_reduce(out=out, in_=in_, axis=AX.X, op=op), reads=r, writes=w)


def COPY(P, eng, out, in_, r, w):
    if eng == "act":
        P.op("act", lambda e: e.copy(out=out, in_=in_), reads=r, writes=w)
    else:
        P.op(eng, lambda e: e.tensor_copy(out=out, in_=in_), reads=r, writes=w)


SKIP_CH = set()


def DMA(P, eng, chan, out, in_, r, w):
    if chan in SKIP_CH:
        return
    P.dma(eng, chan, lambda e: e.dma_start(out=out, in_=in_), reads=r, writes=w)


def DMAN(P, eng, chan, pairs, r, w):
    pairs = list(pairs)
    P.dma(eng, chan, lambda e: [e.dma_start(out=o, in_=i) for (o, i) in pairs], reads=r, writes=w, n=len(pairs))


class Ctx:
    pass


def build(stages=("a0", "m0", "a1", "m1"), S=4096, debug=None):
    NT = S // 128
    NG = S // 512
    nc = bass.Bass("TRN2", target_bir_lowering=False)
    P = Prog(nc)
    C = Ctx()
    C.S, C.NT, C.NG = S, NT, NG

    def din(name, shape, dt=F32):
        return nc.dram_tensor(name, list(shape), dt, kind="ExternalInput").ap()

    def dscr(name, shape, dt):
        return nc.dram_tensor(name, list(shape), dt, kind="Internal").ap()

    dr = {}
    dr["x"] = din("x", [S, D])
    dr["y"] = nc.dram_tensor("y", [S, D], F32, kind="ExternalOutput").ap()
    dr["gb"] = din("gb", [5, 128, D])
    dr["gqkv"] = din("gqkv", [128, 512])
    dr["tb"] = din("tb", [128, H, 256])
    dr["b31"] = din("b31", [128, H])
    dr["wqkv"] = din("wqkv", [D, 3 * D])
    dr["wo0"] = din("wo0", [D, D])
    dr["win"] = din("win", [D, 584])
    dr["wuq"] = din("wuq", [256, D])
    dr["wqi"] = din("wqi", [256, 512])
    dr["wukT"] = din("wukT", [256, D])
    dr["wuv"] = din("wuv", [256, D])
    dr["wo1"] = din("wo1", [D, D])
    dr["wup0"] = din("wup0", [D, DFF])
    dr["wup1"] = din("wup1", [D, DFF])
    dr["wdn0"] = din("wdn0", [DFF, D])
    dr["wdn1"] = din("wdn1", [DFF, D])
    dr["qT"] = dscr("qT", [8, 128, S], BF16)
    dr["kT"] = dscr("kT", [8, 128, S], BF16)
    dr["va"] = dscr("va", [H, 128, NT, 128], BF16)
    dr["qiT"] = dscr("qiT", [4, 128, S], BF16)
    xs = [dr["x"]]
    for i, stg in enumerate(stages):
        if i == len(stages) - 1:
            xs.append(dr["y"])
        else:
            xs.append(dscr("xs%d" % i, [S, D], F32))

    C.pb = [P.ps("pb%d" % i, [128, 512], F32) for i in range(8)]
    A = Arena(P, 200)
    C.A = A
    C.ident = A.alloc([128], BF16)
    C.identB = A.alloc([128], BF16)
    C.io = A.alloc([128], F32)
    C.ssb = A.alloc([64], F32)
    C.ksum = A.alloc([8, 16], F32)
    P.op("pool", lambda e: e.iota(C.io, pattern=[[1, 128]], base=0, channel_multiplier=-1,
                                  allow_small_or_imprecise_dtypes=True), writes=["io"])
    TS(P, "dve", C.ident, C.io, 0.0, None, ALU.is_equal, None, ["io"], ["ident"])
    TS(P, "dve", C.identB, C.io, 0.0, BIG, ALU.is_equal, ALU.mult, ["io"], ["identB"])
    base_off = A.off
    P.barrier()

    for i, stg in enumerate(stages):
        A.off = base_off
        last = (i == len(stages) - 1)
        if stg == "a0":
            attn_phaseA(P, C, dr, xs[i], 0)
            P.barrier()
            A.off = base_off
            if debug is not None and debug.startswith("A"):
                dbg_copy(P, C, dr, xs[i], xs[i + 1])
            else:
                attn_phaseB(P, C, dr, xs[i], xs[i + 1], 0, debug)
        elif stg == "a1":
            C.kiTz = [A.alloc([S], BF16) for _ in range(2)]
            C.wabs = A.alloc([NT, 8], F32)
            C.wsgn = A.alloc([NT, 8], F32)
            base1 = A.off
            attn_phaseA(P, C, dr, xs[i], 1)
            P.barrier()
            A.off = base1
            attn_phaseB(P, C, dr, xs[i], xs[i + 1], 1)
        elif stg == "m0":
            mlp_phase(P, C, dr, xs[i], xs[i + 1], 0, False)
        elif stg == "m1":
            mlp_phase(P, C, dr, xs[i], xs[i + 1], 1, True)
        P.barrier()
    P.emit()
    return nc


def load_w_cast(P, dst3, src, nk, ncols, name, c0=0):
    pairs = []
    step = 2048
    for k in range(nk):
        for cc in range(0, ncols, step):
            w = min(step, ncols - cc)
            pairs.append((dst3[:, k, cc:cc + w], src[k * 128:(k + 1) * 128, c0 + cc:c0 + cc + w]))
    DMAN(P, "pool", "w_" + name, pairs, [], [name])


class Normer:
    def __init__(self, P, C, gb_dram_row, nbuf=3, tag="n"):
        A = C.A
        self.P, self.C = P, C
        self.gb = A.alloc([D], F32)
        DMA(P, "sp", "gbld", self.gb, gb_dram_row, [], ["gb"])
        self.nbuf = nbuf
        self.xt = [A.alloc([D], F32) for _ in range(nbuf)]
        self.xn = [A.alloc([D], BF16) for _ in range(2)]
        self.junk = A.alloc([D], BF16)
        self.cnt = 0

    def load(self, src_tile):
        i = self.cnt % self.nbuf
        DMA(self.P, "sp", "xt%d" % i, self.xt[i], src_tile, [], ["xt%d" % i])
        return i

    def norm_T(self, i, dstT, c0, dst_name, evac_eng="act"):
        P, C = self.P, self.C
        k = self.cnt
        self.cnt += 1
        xt = self.xt[i]
        xn = self.xn[k % 2]
        xnn = "xn%d" % (k % 2)
        ss = C.ssb[:, (k % 8) * 2:(k % 8) * 2 + 1]
        rs = C.ssb[:, (k % 8) * 2 + 1:(k % 8) * 2 + 2]
        sn = "ss%d" % (k % 8)
        ACT(P, self.junk, xt, AF.Square, ["xt%d" % i], ["junk", sn], accum=ss)
        with P.strict():
            ACT(P, rs, ss, AF.Ln, [sn], [sn + "r"], scale=1.0 / D, bias=EPS)
            ACT(P, rs, rs, AF.Exp, [sn + "r"], [sn + "r"], scale=-0.5)
        STT(P, xn, xt, rs, self.gb, ALU.mult, ALU.mult, ["xt%d" % i, sn + "r", "gb"], [xnn])
        pbi = k % 2
        pbv = C.pb[pbi][:].bitcast(BF16)
        for kc in range(8):
            TR(P, pbv[:, kc * 128:(kc + 1) * 128], xn[:, kc * 128:(kc + 1) * 128], C.ident, [xnn, "ident"], ["pb%d" % pbi])
        COPY(P, evac_eng, dstT[:, :, c0:c0 + 128], pbv.rearrange("p (a b) -> p a b", a=8), ["pb%d" % pbi], [dst_name])


def attn_phaseA(P, C, dr, xin, layer):
    A = C.A
    S, NT, NG = C.S, C.NT, C.NG
    nm = Normer(P, C, dr["gb"][2 * layer], nbuf=3)
    hnT = [A.alloc([8, 512], BF16) for _ in range(2)]
    qst = A.alloc([8, 512], BF16)
    kst = A.alloc([8, 512], BF16)
    vst = [A.alloc([8, 2, 128], BF16) for _ in range(2)]
    for b in range(2):
        P.op("pool", (lambda e, b=b: e.memset(vst[b], 1.0)), writes=["vst%d" % b])
    if layer == 0:
        wq = A.alloc([8, D], BF16)
        wk = A.alloc([8, D], BF16)
        wv = A.alloc([8, D], BF16)
        load_w_cast(P, wq, dr["wqkv"], 8, D, "wq", 0)
        load_w_cast(P, wk, dr["wqkv"], 8, D, "wk", D)
        load_w_cast(P, wv, dr["wqkv"], 8, D, "wv", 2 * D)
        P.op("dve", lambda e: e.memset(C.ksum, 0.0), writes=["ksum"])
    else:
        win = A.alloc([8, 584], BF16)
        wuq = A.alloc([2, D], BF16)
        wqi = A.alloc([2, 512], BF16)
        wuk = A.alloc([2, D], BF16)
        wuv = A.alloc([2, D], BF16)
        load_w_cast(P, win, dr["win"], 8, 584, "win")
        load_w_cast(P, wuq, dr["wuq"], 2, D, "wuq")
        load_w_cast(P, wqi, dr["wqi"], 2, 512, "wqi")
        load_w_cast(P, wuk, dr["wukT"], 2, D, "wuk")
        load_w_cast(P, wuv, dr["wuv"], 2, D, "wuv")
        gq = A.alloc([512], F32)
        DMA(P, "sp", "gqld", gq, dr["gqkv"], [], ["gq"])
        cn = [A.alloc([512], BF16) for _ in range(2)]
        ki2 = [A.alloc([2, 128], BF16) for _ in range(2)]
        for b in range(2):
            P.op("pool", (lambda e, b=b: e.memset(ki2[b], 0.0)), writes=["ki2%d" % b])
        cnT = [A.alloc([4, 512], BF16) for _ in range(2)]
        qist = A.alloc([4, 512], BF16)
        junk2 = A.alloc([256], BF16)
    rot = [2, 3, 4, 5, 6, 7]
    rc = [0]

    def nextbank():
        b = rot[rc[0] % len(rot)]
        rc[0] += 1
        return b

    xtiles = xin.rearrange("(t p) d -> t p d", p=128)
    for g in range(NG):
        hb = g % 2
        hT = hnT[hb]
        hname = "hnT%d" % hb
        for j in range(4):
            t = g * 4 + j
            i = nm.load(xtiles[t])
            nm.norm_T(i, hT, j * 128, hname, evac_eng="act" if layer == 0 else "dve")
        if layer == 0:
            for which, wmat, stg, sname in (("q", wq, qst, "qst"), ("k", wk, kst, "kst")):
                for hp in range(8):
                    b = nextbank()
                    for kc in range(8):
                        MM(P, C.pb[b][:, :], wmat[:, kc, hp * 128:(hp + 1) * 128], hT[:, kc, :], kc == 0, kc == 7,
                           ["w" + which, hname], ["pb%d" % b])
                    if which == "q":
                        ACT(P, stg[:, hp, :], C.pb[b][:, :], AF.Identity, ["pb%d" % b], [sname], scale=0.125)
                    else:
                        COPY(P, "act", stg[:, hp, :], C.pb[b][:, :], ["pb%d" % b], [sname])
                        RED(P, C.ksum[:, hp, 2 * g:2 * g + 2], C.pb[b][:, :].rearrange("p (a b) -> p a b", a=2), ALU.add,
                            ["pb%d" % b], ["ksum"])
                dst = dr["qT" if which == "q" else "kT"][:, :, g * 512:(g + 1) * 512].rearrange("h p t -> p h t")
                DMA(P, "sp", sname, dst, stg, [sname], [])
        else:
            for j in range(4):
                t = g * 4 + j
                tc = slice(j * 128, (j + 1) * 128)
                ba = nextbank()
                bb = nextbank()
                for kc in range(8):
                    MM(P, C.pb[ba][:, :], hT[:, kc, tc], win[:, kc, 0:512], kc == 0, kc == 7, [hname, "win"], ["pb%d" % ba])
                for kc in range(8):
                    MM(P, C.pb[bb][:, 0:72], hT[:, kc, tc], win[:, kc, 512:584], kc == 0, kc == 7, [hname, "win"], ["pb%d" % bb])
                cb = t % 2
                ssq = C.ssb[:, 32 + cb * 4:32 + cb * 4 + 1]
                ssk = C.ssb[:, 32 + cb * 4 + 1:32 + cb * 4 + 2]
                rsq = C.ssb[:, 32 + cb * 4 + 2:32 + cb * 4 + 3]
                rsk = C.ssb[:, 32 + cb * 4 + 3:32 + cb * 4 + 4]
                sn = "cs%d" % cb
                ACT(P, junk2, C.pb[ba][:, 0:256], AF.Square, ["pb%d" % ba], ["junk2", sn], accum=ssq)
                ACT(P, junk2, C.pb[ba][:, 256:512], AF.Square, ["pb%d" % ba], ["junk2", sn], accum=ssk)
                with P.strict():
                    ACT(P, rsq, ssq, AF.Ln, [sn], [sn + "q"], scale=1.0 / 256, bias=EPS)
                    ACT(P, rsq, rsq, AF.Exp, [sn + "q"], [sn + "q"], scale=-0.5)
                    ACT(P, rsk, ssk, AF.Ln, [sn], [sn + "k"], scale=1.0 / 256, bias=EPS)
                    ACT(P, rsk, rsk, AF.Exp, [sn + "k"], [sn + "k"], scale=-0.5)
                cname = "cn%d" % cb
                STT(P, cn[cb][:, 0:256], C.pb[ba][:, 0:256], rsq, gq[:, 0:256], ALU.mult, ALU.mult,
                    ["pb%d" % ba, sn + "q", "gq"], [cname])
                STT(P, cn[cb][:, 256:512], C.pb[ba][:, 256:512], rsk, gq[:, 256:512], ALU.mult, ALU.mult,
                    ["pb%d" % ba, sn + "k", "gq"], [cname])
                kname = "ki2%d" % cb
                COPY(P, "act", ki2[cb][:, 0, 0:64], C.pb[bb][:, 0:64], ["pb%d" % bb], [kname])
                COPY(P, "act", ki2[cb][:, 1, 64:128], C.pb[bb][:, 0:64], ["pb%d" % bb], [kname])
                TS(P, "dve", C.wabs[:, t, :], C.pb[bb][:, 64:72], (8.0 ** -0.5) * (64.0 ** -0.5), None, ALU.mult, None,
                   ["pb%d" % bb], ["wabs"])
                pbv = C.pb[cb][:].bitcast(BF16)
                for q4 in range(4):
                    TR(P, pbv[:, q4 * 128:(q4 + 1) * 128], cn[cb][:, q4 * 128:(q4 + 1) * 128], C.ident, [cname, "ident"], ["pb%d" % cb])
                TR(P, pbv[:, 512:640], ki2[cb][:, 0, :], C.ident, [kname, "ident"], ["pb%d" % cb])
                TR(P, pbv[:, 640:768], ki2[cb][:, 1, :], C.ident, [kname, "ident"], ["pb%d" % cb])
                COPY(P, "act", cnT[hb][:, :, tc], pbv[:, 0:512].rearrange("p (a b) -> p a b", a=4), ["pb%d" % cb], ["cnT%d" % hb])
                COPY(P, "act", C.kiTz[0][:, t * 128:(t + 1) * 128], pbv[:, 512:640], ["pb%d" % cb], ["kiT"])
                COPY(P, "act", C.kiTz[1][:, t * 128:(t + 1) * 128], pbv[:, 640:768], ["pb%d" % cb], ["kiT"])
            cT = cnT[hb]
            cTn = "cnT%d" % hb
            for hp in range(8):
                b = nextbank()
                for kc in range(2):
                    MM(P, C.pb[b][:, :], wuq[:, kc, hp * 128:(hp + 1) * 128], cT[:, kc, :], kc == 0, kc == 1, ["wuq", cTn], ["pb%d" % b])
                ACT(P, qst[:, hp, :], C.pb[b][:, :], AF.Identity, ["pb%d" % b], ["qst"], scale=0.125)
            DMA(P, "sp", "qst", dr["qT"][:, :, g * 512:(g + 1) * 512].rearrange("h p t -> p h t"), qst, ["qst"], [])
            for ch in range(4):
                b = nextbank()
                for kc in range(2):
                    MM(P, C.pb[b][:, :], wqi[:, kc, ch * 128:(ch + 1) * 128], cT[:, kc, :], kc == 0, kc == 1, ["wqi", cTn], ["pb%d" % b])
                COPY(P, "dve", qist[:, ch, :], C.pb[b][:, :], ["pb%d" % b], ["qist"])
            DMA(P, "sp", "qist", dr["qiT"][:, :, g * 512:(g + 1) * 512].rearrange("h p t -> p h t"), qist, ["qist"], [])
            for hp in range(8):
                b = nextbank()
                for kc in range(2):
                    MM(P, C.pb[b][:, :], wuk[:, kc, hp * 128:(hp + 1) * 128], cT[:, 2 + kc, :], kc == 0, kc == 1, ["wuk", cTn], ["pb%d" % b])
                COPY(P, "act", kst[:, hp, :], C.pb[b][:, :], ["pb%d" % b], ["kst"])
            DMA(P, "sp", "kst", dr["kT"][:, :, g * 512:(g + 1) * 512].rearrange("h p t -> p h t"), kst, ["kst"], [])
        for j in range(4):
            t = g * 4 + j
            tc = slice(j * 128, (j + 1) * 128)
            vb = t % 2
            vname = "vst%d" % vb
            for half in range(2):
                b = nextbank()
                if layer == 0:
                    for kc in range(8):
                        MM(P, C.pb[b][:, :], hT[:, kc, tc], wv[:, kc, half * 512:(half + 1) * 512], kc == 0, kc == 7, [hname, "wv"], ["pb%d" % b])
                else:
                    for kc in range(2):
                        MM(P, C.pb[b][:, :], cT[:, 2 + kc, tc], wuv[:, kc, half * 512:(half + 1) * 512], kc == 0, kc == 1, [cTn, "wuv"], ["pb%d" % b])
                psv = C.pb[b][:, :].rearrange("p (a b c) -> p a b c", a=4, b=2)
                COPY(P, "dve", vst[vb][:, half * 4:(half + 1) * 4, 0, 0:64], psv[:, :, 0, :], ["pb%d" % b], [vname])
                COPY(P, "dve", vst[vb][:, half * 4:(half + 1) * 4, 1, 64:128], psv[:, :, 1, :], ["pb%d" % b], [vname])
            dst = dr["va"][:, :, t, :].rearrange("(a b) p d -> p a b d", b=2)
            DMA(P, "sp", vname, dst, vst[vb], [vname], [])


def dbg_copy(P, C, dr, xin, xout):
    A = C.A
    xt = A.alloc([D], F32)
    xin_t = xin.rearrange("(t p) d -> t p d", p=128)
    xout_t = xout.rearrange("(t p) d -> t p d", p=128)
    for t in range(C.NT):
        DMA(P, "sp", "dbgl", xt, xin_t[t], [], ["dbgx"])
        DMA(P, "pool", "dbgs", xout_t[t], xt, ["dbgx"], [])


SALL = ["score%d" % i_ for i_ in range(8)]


def attn_phaseB(P, C, dr, xin, xout, layer, debug=None):
    A = C.A
    S, NT, NG = C.S, C.NT, C.NG
    wo = A.alloc([8, D], BF16)
    load_w_cast(P, wo, dr["wo0" if layer == 0 else "wo1"], 8, D, "wo")
    if layer == 1:
        score = A.alloc([max(S, H * 256)], F32)
        tbf = score[:, 0:H * 256].rearrange("p (a b) -> p a b", a=H)
        tbn = "score0"
    else:
        tbf = A.alloc([H, 256], F32)
        tbn = "tbf"
    b31 = A.alloc([H], F32)
    cm0 = A.alloc([128], F32)
    TBp = A.alloc([H, 256], BF16)
    DMA(P, "sp", "tbld", tbf, dr["tb"], [], SALL if layer == 1 else [tbn])
    DMA(P, "sp", "b31ld", b31, dr["b31"], [], ["b31"])
    TS(P, "dve", cm0, C.io, 0.0, -BIG, ALU.is_lt, ALU.mult, ["io"], ["cm0"])
    for h in range(H):
        STT(P, TBp[:, h, 0:128], tbf[:, h, 0:128], b31[:, h:h + 1], cm0, ALU.subtract, ALU.add, (SALL if layer == 1 else [tbn]) + ["b31", "cm0"], ["TBp"])
        TS(P, "dve", TBp[:, h, 128:256], tbf[:, h, 128:256], b31[:, h:h + 1], None, ALU.subtract, None, (SALL if layer == 1 else [tbn]) + ["b31"], ["TBp"])
    nqb = 2
    qz = [[A.alloc([8, 512], BF16) for _ in range(2)] for _ in range(nqb)]
    for b in range(2):
        P.op("pool", (lambda e, b=b: e.memset(qz[b][0][64:128], 0.0)), writes=["qg%d" % b])
        P.op("pool", (lambda e, b=b: e.memset(qz[b][1][0:64], 0.0)), writes=["qg%d" % b])
    kbuf = [A.alloc([S], BF16) for _ in range(2)]
    vbuf = [A.alloc([NT, 128], BF16) for _ in range(2)]
    pbuf = [A.alloc([512], BF16) for _ in range(3)]
    oT = A.alloc([8, 512], BF16)
    rec = A.alloc([512], F32)
    xt = [A.alloc([D], F32) for _ in range(2)]
    if layer == 0:
        kbd = A.alloc([8, 32], BF16)
        nidx = A.alloc([256], F32)
        pneg = A.alloc([256], F32)
        ownm1 = A.alloc([256], F32)
        gm = A.alloc([256], F32)
        gm2 = A.alloc([256], F32)
        tmpg = A.alloc([256], F32)
        mx = A.alloc([16], F32)
        selv = A.alloc([256], BF16)
        selT2 = [A.alloc([H, 512], BF16) for _ in range(2)]
        Esel = A.alloc([16, 128], BF16)
        iot = A.alloc([16, 128], F32)
        for b_ in range(2):
            P.op("pool", (lambda e, b_=b_: e.memset(selT2[b_], 0.0)), writes=["selT%d" % b_])
        P.op("dve", lambda e: e.memset(kbd, 0.0), writes=["kbd"])
        COPY(P, "dve", kbd[0:64, :, 0:16], C.ksum[0:64, :, :], ["ksum", "kbd"], ["kbd"])
        COPY(P, "dve", kbd[64:128, :, 16:32], C.ksum[64:128, :, :], ["ksum", "kbd"], ["kbd"])
        P.op("pool", lambda e: e.iota(nidx.rearrange("p (a b) -> p a b", a=16), pattern=[[0, 16], [1, 16]], base=0,
                                      channel_multiplier=0, allow_small_or_imprecise_dtypes=True), writes=["nidx"])
        P.op("pool", lambda e: e.iota(iot, pattern=[[1, 16], [0, 128]], base=0, channel_multiplier=-1,
                                      allow_small_or_imprecise_dtypes=True), writes=["iot"])
        TS(P, "dve", Esel, iot, 0.0, BIG, ALU.is_equal, ALU.mult, ["iot"], ["Esel"])
    else:
        qig = A.alloc([4, 512], BF16)
        madd = A.alloc([S], BF16)
        junkb = madd
        maskT = A.alloc([NT, 512], BF16)
        cneg = A.alloc([128], F32)
        cpos = A.alloc([128], F32)
        dtmp = A.alloc([128], F32)
        bs = A.alloc([16], F32)
        bsi = A.alloc([4], I32)
        pw = A.alloc([NBIS + 1], F32)
        sk = A.alloc([NBIS + 1], F32)
        for k_ in range(NBIS + 1):
            P.op("dve", (lambda e, k_=k_: e.memset(pw[:, k_:k_ + 1], 2.0 ** -(k_ + 1))), writes=["pw"])
        TS(P, "dve", cneg, C.io, 0.0, NEGF, ALU.is_gt, ALU.mult, ["io"], ["cneg"])
        TS(P, "dve", cpos, C.io, 0.0, -NEGF, ALU.is_gt, ALU.mult, ["io"], ["cpos"])
    xin_t = xin.rearrange("(t p) d -> t p d", p=128)
    xout_t = xout.rearrange("(t p) d -> t p d", p=128)
    lrot = [2, 3, 4]
    lc = [0]
    ucnt = [0]
    def load_q(gq):
        qb_ = gq % nqb
        DMAN(P, "sp", "qg%d" % qb_, [(qz[qb_][0][0:64], dr["qT"][:, 0:64, gq * 512:(gq + 1) * 512].rearrange("h p t -> p h t")),
                                    (qz[qb_][1][64:128], dr["qT"][:, 64:128, gq * 512:(gq + 1) * 512].rearrange("h p t -> p h t"))],
             [], ["qg%d" % qb_])

    def gate_a(gq, j):
        t = gq * 4 + j
        blk = t // 2
        QG = qz[gq % nqb]
        qn_ = "qg%d" % (gq % nqb)
        if t % 2 == 0:
            TS(P, "dve", pneg, nidx, float(blk), NEGF, ALU.is_ge, ALU.mult, ["nidx"], ["pneg"])
            TS(P, "dve", ownm1, nidx, float(blk), -1.0, ALU.is_ge, ALU.add, ["nidx"], ["ownm1"])
        for hp in range(8):
            for h2 in range(2):
                MM(P, C.pb[0][:, hp * 32:(hp + 1) * 32], QG[h2][:, hp, j * 128:(j + 1) * 128], kbd[:, hp, :], h2 == 0, h2 == 1,
                   [qn_, "kbd"], ["pb0"])
        TT(P, "dve", gm, C.pb[0][:, 0:256], pneg, ALU.add, ["pb0", "pneg"], ["gm"])
        g3 = gm.rearrange("p (a b) -> p a b", a=16)
        g23 = gm2.rearrange("p (a b) -> p a b", a=16)
        t3 = tmpg.rearrange("p (a b) -> p a b", a=16)
        mb = bcast_last(mx, 16)
        RED(P, mx, g3, ALU.max, ["gm"], ["mx"])
        TT(P, "dve", t3, g3, mb, ALU.is_ge, ["gm", "mx"], ["tmpg"])
        STT(P, gm2, tmpg, NEGF, gm, ALU.mult, ALU.add, ["tmpg", "gm"], ["gm2"])
        RED(P, mx, g23, ALU.max, ["gm2"], ["mx"])
        TT(P, "dve", t3, g23, mb, ALU.is_ge, ["gm2", "mx"], ["tmpg"])
        STT(P, gm2, tmpg, NEGF, gm2, ALU.mult, ALU.add, ["tmpg", "gm2"], ["gm2"])
        RED(P, mx, g23, ALU.max, ["gm2"], ["mx"])
        TT(P, "dve", t3, g3, mb, ALU.is_ge, ["gm", "mx"], ["tmpg"])
        STT(P, selv, tmpg, -1.0, ownm1, ALU.add, ALU.max, ["tmpg", "ownm1"], ["selv"])

    def gate_b(gq, j):
        sT = selT2[gq % 2]
        pv1 = C.pb[1][:].bitcast(BF16)
        for rnd in range(2):
            for hh in range(8):
                h = rnd * 8 + hh
                TR(P, pv1[0:16, hh * 128:(hh + 1) * 128], selv[:, h * 16:(h + 1) * 16], C.ident, ["selv", "ident"], ["pb1"])
            COPY(P, "act", sT[0:16, rnd * 8:(rnd + 1) * 8, j * 128:(j + 1) * 128],
                 pv1[0:16, :].rearrange("p (a b) -> p a b", a=8), ["pb1"], ["selT%d" % (gq % 2)])

    load_q(0)
    if layer == 0:
        for j in range(4):
            gate_a(0, j)
            gate_b(0, j)
    for g in range(NG):
        qb = g % nqb
        qname = "qg%d" % qb
        QZ = qz[qb]
        nch = 4 * (g + 1)
        if g + 1 < NG:
            load_q(g + 1)
        if layer == 0:
            selT = selT2[g % 2]
            sTn = "selT%d" % (g % 2)
        else:
            DMA(P, "sp", "qig", qig, dr["qiT"][:, :, g * 512:(g + 1) * 512].rearrange("h p t -> p h t"), [], ["qig"])
            for j in range(4):
                t = g * 4 + j
                nk = 128 * (t + 1)
                qc = slice(j * 128, (j + 1) * 128)
                nsg = (nk + 511) // 512
                for hi in range(8):
                    ch = hi // 2
                    for sg in range(nsg):
                        ncol = min(512, nk - sg * 512)
                        sc = slice(sg * 512, sg * 512 + ncol)
                        bm = (0, 1, 2)[ucnt[0] % 3]
                        br = (3, 4, 6, 7)[ucnt[0] % 4]
                        ucnt[0] += 1
                        sn_ = "score%d" % sg
                        MM(P, C.pb[bm][:, 0:ncol], qig[:, ch, qc], C.kiTz[hi % 2][:, sc], True, True, ["qig", "kiT"], ["pb%d" % bm])
                        ACT(P, C.pb[br][:, 0:ncol], C.pb[bm][:, 0:ncol], AF.Relu, ["pb%d" % bm], ["pb%d" % br])
                        if hi == 0:
                            TS(P, "dve", score[:, sc], C.pb[br][:, 0:ncol], C.wabs[:, t, 0:1], None, ALU.mult, None,
                               ["pb%d" % br, "wabs"], [sn_])
                        else:
                            STT(P, score[:, sc], C.pb[br][:, 0:ncol], C.wabs[:, t, hi:hi + 1], score[:, sc], ALU.mult, ALU.add,
                                ["pb%d" % br, "wabs", sn_], [sn_])
                dg = slice(128 * t, 128 * t + 128)
                lo, hi_, rg, mid, cnt, m1 = (bs[:, k:k + 1] for k in range(6))
                fl = bsi[:, 0:1]
                P.strict_default = True
                if t >= 2:
                    RED(P, lo, score[:, 0:256], ALU.min, SALL, ["bs"])
                else:
                    TT(P, "dve", dtmp, score[:, dg], cpos, ALU.add, SALL + ["cpos"], ["dtmp"])
                    RED(P, lo, dtmp, ALU.min, ["dtmp"], ["bs"])
                    if t > 0:
                        RED(P, m1, score[:, 0:128 * t], ALU.min, SALL, ["bs"])
                        TT(P, "dve", lo, lo, m1, ALU.min, ["bs"], ["bs"])
                TT(P, "dve", score[:, dg], score[:, dg], cneg, ALU.add, SALL + ["cneg"], SALL)
                if nk > 256:
                    negS, tq = bs[:, 6:7], bs[:, 7:8]
                    RED(P, hi_, score[:, 0:nk], ALU.max, SALL, ["bs"])
                    TT(P, "dve", rg, hi_, lo, ALU.subtract, ["bs"], ["bs"])
                    TS(P, "dve", sk, pw, rg, None, ALU.mult, None, ["pw", "bs"], ["sk"])
                    TT(P, "dve", mid, lo, sk[:, 0:1], ALU.add, ["bs", "sk"], ["mid"])
                    split = nk >= 1536
                    n1 = (int(nk * 0.49) // 128) * 128 if split else nk
                    thr_c = 256.0 - (nk - n1) / 2.0 - (0.25 if split else 0.0)
                    for it in range(NBIS):
                        if split:
                            ACT(P, madd[:, n1:nk], score[:, n1:nk], AF.Sign, SALL + ["mid"], ["maddA", "negS"],
                                scale=-1.0, bias=mid, accum=negS)
                        TS(P, "dve", madd[:, 0:n1], score[:, 0:n1], mid, None, ALU.is_ge, ALU.add, SALL + ["mid"], ["maddD", "cnt"], accum=cnt)
                        if split:
                            TS(P, "dve", cnt, negS, -0.5, cnt, ALU.mult, ALU.add, ["negS", "cnt"], ["cnt"])
                        STT(P, tq, cnt, thr_c, sk[:, it:it + 1], ALU.is_ge, ALU.mult, ["cnt", "sk"], ["tq"])
                        STT(P, mid, tq, sk[:, it + 1:it + 2], mid, ALU.subtract, ALU.add, ["tq", "sk", "mid"], ["mid"])
                    TT(P, "dve", lo, mid, sk[:, NBIS:NBIS + 1], ALU.subtract, ["mid", "sk"], ["bs"])
                TS(P, "dve", madd[:, 0:nk], score[:, 0:nk], lo, 1.0, ALU.is_ge, ALU.subtract, SALL + ["bs"], ["madd", "maddA", "maddD"])
                P.strict_default = False
                for c0 in range(0, t + 1, 8):
                    cn_ = min(8, t + 1 - c0)
                    bk_ = 5 + ((c0 // 8) % 2)
                    pv7 = C.pb[bk_][:].bitcast(BF16)
                    for cc in range(cn_):
                        c = c0 + cc
                        TR(P, pv7[:, cc * 128:(cc + 1) * 128], madd[:, c * 128:(c + 1) * 128], C.ident, ["madd", "maddA", "maddD", "ident"], ["pb%d" % bk_])
                    COPY(P, "act", maskT[:, c0:c0 + cn_, qc], pv7[:, 0:cn_ * 128].rearrange("p (a b) -> p a b", a=cn_), ["pb%d" % bk_], ["maskT"])
        items = [(h, c) for h in range(H) for c in range(nch)]
        LOOK = 3
        lbanks = [2, 3, 4, 7] if layer == 0 else [0, 1, 2, 5]

        def hinfo(h):
            hp, h2 = h // 2, h % 2
            r0 = 64 * h2
            return hp, h2, slice(r0, r0 + 64), slice(64 - r0, 128 - r0), hp % 2, h % 2

        def issue_qk(idx):
            h, c = items[idx]
            hp, h2, rs_, so, kb, vb = hinfo(h)
            kname = "kbuf%d" % kb
            if c == 0:
                if h2 == 0:
                    DMA(P, "sp", kname, kbuf[kb][:, 0:nch * 128], dr["kT"][hp, :, 0:nch * 128], [], [kname])
                DMA(P, "sp", "vbuf%d" % vb, vbuf[vb][:, 0:nch, :], dr["va"][h, :, 0:nch, :], [], ["vbuf%d" % vb])
            dc = c - 4 * g
            col0 = max(0, dc * 128)
            cs = slice(col0, 512)
            lb = lbanks[idx % 4]
            lbn = "pb%d" % lb
            psl = C.pb[lb]
            n_blk = c // 2
            need_sel = (layer == 1) or (n_blk < 2 * g + 1)
            need_tb = dc >= -1
            MM(P, psl[:, cs], kbuf[kb][:, c * 128:(c + 1) * 128], QZ[h2][:, hp, cs], True, not (need_sel or need_tb), [kname, qname], [lbn])
            if need_sel:
                if layer == 0:
                    MM(P, psl[:, cs], Esel[:, n_blk, :], selT[:, h, cs], False, not need_tb, ["Esel", sTn], [lbn])
                else:
                    MM(P, psl[:, cs], C.identB, maskT[:, c, cs], False, not need_tb, ["identB", "maskT"], [lbn])
            if need_tb:
                if dc == -1:
                    MM(P, psl[:, 0:128], C.ident, TBp[:, h, 128:256], False, True, ["ident", "TBp"], [lbn])
                elif dc == 3:
                    MM(P, psl[:, 384:512], C.ident, TBp[:, h, 0:128], False, True, ["ident", "TBp"], [lbn])
                else:
                    MM(P, psl[:, col0:col0 + 256], C.ident, TBp[:, h, 0:256], False, True, ["ident", "TBp"], [lbn])

        for idx in range(min(LOOK, len(items))):
            issue_qk(idx)
        for idx in range(len(items)):
            if idx + LOOK < len(items):
                issue_qk(idx + LOOK)
            h, c = items[idx]
            if layer == 0 and g + 1 < NG and c == 0:
                if h % 4 == 0:
                    gate_a(g + 1, h // 4)
                elif h % 4 == 2:
                    gate_b(g + 1, h // 4)
            hp, h2, rs_, so, kb, vb = hinfo(h)
            ob = 5 + (h % 2) if layer == 0 else 3 + (h % 2)
            obn = "pb%d" % ob
            pso = C.pb[ob]
            dc = c - 4 * g
            col0 = max(0, dc * 128)
            cs = slice(col0, 512)
            lb = lbanks[idx % 4]
            lbn = "pb%d" % lb
            pi = idx % 3
            pn = "pbuf%d" % pi
            ACT(P, pbuf[pi][:, cs], C.pb[lb][:, cs], AF.Exp, [lbn], [pn])
            MM(P, pso[:, cs], vbuf[vb][:, c, :], pbuf[pi][:, cs], c == 0, c == nch - 1, ["vbuf%d" % vb, pn], [obn])
            if c == nch - 1:
                P.op("dve", (lambda e, o_=rec[so, :], i_=pso[so, :]: e.reciprocal(out=o_, in_=i_)), reads=[obn], writes=["rec"])
                TT(P, "dve", oT[rs_, hp, :], pso[rs_, :], rec[so, :], ALU.mult, [obn, "rec"], ["oT"])
        for j in range(4):
            t = g * 4 + j
            xb = t % 2
            xn_ = "xta%d" % xb
            DMA(P, "sp", xn_, xt[xb], xin_t[t], [], [xn_])
            for half in range(2):
                b = 6 + half if layer == 1 else (0, 1)[half]
                bn = "pb%d" % b
                for hp in range(8):
                    MM(P, C.pb[b][:, :], oT[:, hp, j * 128:(j + 1) * 128], wo[:, hp, half * 512:(half + 1) * 512], hp == 0, hp == 7, ["oT", "wo"], [bn])
                hs = slice(half * 512, (half + 1) * 512)
                TT(P, "dve", xt[xb][:, hs], C.pb[b][:, :], xt[xb][:, hs], ALU.add, [bn, xn_], [xn_])
            DMA(P, "pool", "xst%d" % xb, xout_t[t], xt[xb], [xn_], [])


def mlp_phase(P, C, dr, xin, xout, layer, final):
    A = C.A
    S, NT = C.S, C.NT
    wup = A.alloc([8, DFF], BF16)
    wdn = A.alloc([32, D], BF16)
    load_w_cast(P, wup, dr["wup%d" % layer], 8, DFF, "wup")
    load_w_cast(P, wdn, dr["wdn%d" % layer], 32, D, "wdn")
    nm = Normer(P, C, dr["gb"][2 * layer + 1], nbuf=4)
    if final:
        fnb = A.alloc([D], F32)
        DMA(P, "sp", "fnld", fnb, dr["gb"][4], [], ["fnb"])
        junkf = A.alloc([D], BF16)
    hnT = [A.alloc([8, 256], BF16) for _ in range(2)]
    hTt = A.alloc([32, 256], BF16)
    rbuf = [A.alloc([512], BF16) for _ in range(3)]
    xin_t = xin.rearrange("(t p) d -> t p d", p=128)
    xout_t = xout.rearrange("(t p) d -> t p d", p=128)
    rcnt = [0]
    for g2 in range(NT // 2):
        hb = g2 % 2
        hname = "mhT%d" % hb
        xi = []
        for j in range(2):
            t = g2 * 2 + j
            i = nm.load(xin_t[t])
            xi.append(i)
            nm.norm_T(i, hnT[hb], j * 128, hname, evac_eng="dve")
        for f2 in range(16):
            b = 2 + (f2 % 2)
            bn = "pb%d" % b
            for q in range(2):
                ffc = f2 * 2 + q
                for kc in range(8):
                    MM(P, C.pb[b][:, q * 256:(q + 1) * 256], wup[:, kc, ffc * 128:(ffc + 1) * 128], hnT[hb][:, kc, :], kc == 0, kc == 7,
                       ["wup", hname], [bn])
            ri = rcnt[0] % 3
            rcnt[0] += 1
            rn = "rbuf%d" % ri
            ACT(P, rbuf[ri], C.pb[b][:, :], AF.Relu, [bn], [rn])
            TT(P, "dve", hTt[:, f2 * 2:f2 * 2 + 2, :], C.pb[b][:, :].rearrange("p (a b) -> p a b", a=2),
               rbuf[ri].rearrange("p (a b) -> p a b", a=2), ALU.mult, [bn, rn], ["hTt"])
        for j in range(2):
            t = g2 * 2 + j
            i = xi[j]
            xn_ = "xt%d" % i
            for half in range(2):
                b = 4 + j * 2 + half
                bn = "pb%d" % b
                for ffc in range(32):
                    MM(P, C.pb[b][:, :], hTt[:, ffc, j * 128:(j + 1) * 128], wdn[:, ffc, half * 512:(half + 1) * 512], ffc == 0, ffc == 31,
                       ["hTt", "wdn"], [bn])
                hs = slice(half * 512, (half + 1) * 512)
                TT(P, "dve", nm.xt[i][:, hs], C.pb[b][:, :], nm.xt[i][:, hs], ALU.add, [bn, xn_], [xn_])
            if final:
                ss = C.ssb[:, 48 + (t % 4) * 2:48 + (t % 4) * 2 + 1]
                rs = C.ssb[:, 48 + (t % 4) * 2 + 1:48 + (t % 4) * 2 + 2]
                sn = "fs%d" % (t % 4)
                ACT(P, junkf, nm.xt[i], AF.Square, [xn_], ["junkf", sn], accum=ss)
                with P.strict():
                    ACT(P, rs, ss, AF.Ln, [sn], [sn + "r"], scale=1.0 / D, bias=EPS)
                    ACT(P, rs, rs, AF.Exp, [sn + "r"], [sn + "r"], scale=-0.5)
                STT(P, nm.xt[i], nm.xt[i], rs, fnb, ALU.mult, ALU.mult, [xn_, sn + "r", "fnb"], [xn_])
            DMA(P, "pool", "xst%d" % i, xout_t[t], nm.xt[i], [xn_], [])


def _rel_bucket_np(dist):
    n = np.maximum(dist, 0)
    exact = 16
    nf = np.maximum(n, 1).astype(np.float32)
    large = exact + (np.log(nf / np.float32(exact)) / np.float32(math.log(128 / exact)) * np.float32(32 - exact)).astype(np.int32)
    large = np.minimum(large, 31)
    return np.where(n < exact, n, large)


def host_layout(inp):
    f = lambda a: np.ascontiguousarray(np.asarray(a, dtype=np.float32))
    rb = f(inp["rel_bias"])
    s_idx = np.arange(128)[:, None]
    q_idx = np.arange(128)[None, :]
    bk0 = _rel_bucket_np(q_idx - s_idx)
    bk1 = _rel_bucket_np(q_idx - s_idx + 128)
    tb = np.empty((128, H, 256), np.float32)
    tb[:, :, 0:128] = rb[bk0].transpose(0, 2, 1)
    tb[:, :, 128:256] = rb[bk1].transpose(0, 2, 1)
    gb = np.stack([inp["ln_attn"][0], inp["ln_mlp"][0], inp["ln_attn"][1], inp["ln_mlp"][1], inp["final_norm"]])
    gb = f(np.broadcast_to(gb[:, None, :], (5, 128, D)))
    gqkv = f(np.broadcast_to(np.concatenate([inp["dsa_g_q"][0], inp["dsa_g_kv"][0]])[None, :], (128, 512)))
    m = {
        "gb": gb, "gqkv": gqkv, "tb": f(tb), "b31": f(np.broadcast_to(rb[31][None, :], (128, H))),
        "wqkv": f(inp["moba_w_qkv"][0]), "wo0": f(inp["moba_w_o"][0]), "win": f(inp["dsa_w_in"][0]),
        "wuq": f(inp["dsa_w_uq"][0]), "wqi": f(inp["dsa_w_qi"][0]),
        "wukT": f(np.transpose(inp["dsa_w_uk"][0], (2, 0, 1)).reshape(256, D)),
        "wuv": f(np.transpose(inp["dsa_w_uv"][0], (1, 0, 2)).reshape(256, D)),
        "wo1": f(inp["dsa_w_o"][0]),
        "wup0": f(inp["mlp_w_up"][0]), "wup1": f(inp["mlp_w_up"][1]),
        "wdn0": f(inp["mlp_w_down"][0]), "wdn1": f(inp["mlp_w_down"][1]),
    }
    return m


_NC_CACHE = {}


def kernel(**inputs):
    x = np.asarray(inputs["x"], dtype=np.float32)
    B = x.shape[0]
    shared = host_layout(inputs)
    key = ("full", x.shape[1])
    if key not in _NC_CACHE:
        _NC_CACHE[key] = build(("a0", "m0", "a1", "m1"), S=x.shape[1])
    nc = _NC_CACHE[key]
    in_maps = []
    for b in range(B):
        m = dict(shared)
        m["x"] = np.ascontiguousarray(x[b])
        in_maps.append(m)
    res = run_bass_kernel_spmd(nc, in_maps, core_ids=list(range(B)))
    return np.stack([np.asarray(r["y"], dtype=np.float32) for r in res.results], axis=0)
```
